# Optimizing a Trainium2 kernel written in Bass

```python
import math
import jax, jax.numpy as jnp
from jax import lax
import numpy as np

D_MODEL = 1024
BATCH = 8
SEQ = 4096
DEPTH = 4

HEAD_DIM = 64
WIDTH_A = D_MODEL // 2
WIDTH_B = D_MODEL - WIDTH_A
N_HEADS_A = WIDTH_A // HEAD_DIM
N_HEADS_B = WIDTH_B // (2 * HEAD_DIM)
N_HEADS = N_HEADS_A + N_HEADS_B
IN_WIDTH = 3 * WIDTH_A + 3 * WIDTH_B
DILATED_PATTERNS = ((128, 1), (512, 4), (2048, 16))
Q_BLOCK = 128
NUM_BUCKETS = 32
MAX_DISTANCE = 2048
N_EXPERTS = 16
N_GROUPS = 4
EXPERTS_PER_GROUP = N_EXPERTS // N_GROUPS
TOP_K = 2
D_FF = D_MODEL // 2
ALPHA = (2.0 * DEPTH) ** 0.25
BETA = (8.0 * DEPTH) ** -0.25
LN_EPS = 1e-5

kernel_name = "hybrid_dilated_diffattn_grouped_moe_deepnorm"


def t5_bucket(dist):
    n = jnp.maximum(dist, 0)
    max_exact = NUM_BUCKETS // 2
    nf = jnp.maximum(n, max_exact).astype(jnp.float32)
    large = max_exact + (jnp.log(nf / max_exact) / math.log(MAX_DISTANCE / max_exact)
                         * (NUM_BUCKETS - max_exact)).astype(jnp.int32)
    large = jnp.minimum(large, NUM_BUCKETS - 1)
    return jnp.where(n < max_exact, n, large)


def layer_norm(x, g, b):
    xf = x.astype(jnp.float32)
    mu = jnp.mean(xf, -1, keepdims=True)
    var = jnp.mean(jnp.square(xf - mu), -1, keepdims=True)
    return ((xf - mu) * lax.rsqrt(var + LN_EPS)).astype(x.dtype) * g + b


def rms_norm(x, g):
    xf = x.astype(jnp.float32)
    y = xf * lax.rsqrt(jnp.mean(jnp.square(xf), -1, keepdims=True) + LN_EPS)
    return y.astype(x.dtype) * g


def dilated_branch(q, k, v, bias_table, window, dil):
    bsz, seq, nh, hd = q.shape
    blk = window // dil
    sub_len = seq // dil
    nb = -(-sub_len // blk)
    pad = nb * blk - sub_len

    def to_blocks(t):
        t = t.reshape(bsz, sub_len, dil, nh, t.shape[-1])
        t = jnp.pad(t, ((0, 0), (0, pad), (0, 0), (0, 0), (0, 0)))
        return t.reshape(bsz, nb, blk, dil, nh, t.shape[-1])

    def band(t):
        prev = jnp.pad(t[:, :-1], ((0, 0), (1, 0), (0, 0), (0, 0), (0, 0), (0, 0)))
        return jnp.concatenate([prev, t], axis=2)

    qb = to_blocks(q)
    kw = band(to_blocks(k))
    vw = band(to_blocks(v))
    logits = jnp.einsum('bnqrhe,bnkrhe->bnrhqk', qb, kw,
                        preferred_element_type=jnp.float32)
    qi = jnp.arange(blk)[:, None]
    kj = jnp.arange(2 * blk)[None, :]
    steps = qi + blk - kj
    in_band = (steps >= 0) & (steps <= blk)
    bias = bias_table[t5_bucket(steps * dil)].astype(jnp.float32).transpose(2, 0, 1)
    key_exists = (jnp.arange(nb)[:, None, None] > 0) | (kj[None] >= blk)
    mask = in_band[None] & key_exists
    logits = jnp.where(mask[None, :, None, None], logits + bias, -jnp.inf)
    m = jnp.max(logits, -1, keepdims=True)
    p = jnp.exp(logits - m)
    s = jnp.sum(p, -1)
    o = jnp.einsum('bnrhqk,bnkrhe->bnqrhe', p.astype(v.dtype), vw,
                   preferred_element_type=jnp.float32)
    o = o / s.transpose(0, 1, 4, 2, 3)[..., None]
    lse = (m[..., 0] + jnp.log(s)).transpose(0, 1, 4, 2, 3)
    o = o.reshape(bsz, nb * blk, dil, nh, hd)[:, :sub_len].reshape(bsz, seq, nh, hd)
    lse = lse.reshape(bsz, nb * blk, dil, nh)[:, :sub_len].reshape(bsz, seq, nh)
    return o, lse


def dilated_attention(q, k, v, bias_table):
    outs, lses = [], []
    for window, dil in DILATED_PATTERNS:
        o, lse = dilated_branch(q, k, v, bias_table, window, dil)
        outs.append(o)
        lses.append(lse)
    w = jax.nn.softmax(jnp.stack(lses, 0), axis=0)
    return jnp.sum(w[..., None] * jnp.stack(outs, 0), axis=0)


def diff_attention(q1, q2, k1, k2, v, lam, bias_table):
    bsz, seq, nh, hd = q1.shape
    nq = seq // Q_BLOCK
    kpos = jnp.arange(seq)

    def block(args):
        i, q1b, q2b = args
        qpos = i * Q_BLOCK + jnp.arange(Q_BLOCK)
        dist = qpos[:, None] - kpos[None, :]
        bias = bias_table[t5_bucket(dist)].astype(jnp.float32).transpose(2, 0, 1)
        causal = dist >= 0

        def probs(qb, kk):
            l = jnp.einsum('bqhe,bkhe->bhqk', qb, kk,
                           preferred_element_type=jnp.float32) + bias
            return jax.nn.softmax(jnp.where(causal, l, -jnp.inf), axis=-1)

        a = probs(q1b, k1) - lam * probs(q2b, k2)
        return jnp.einsum('bhqk,bkhe->bqhe', a.astype(v.dtype), v,
                          preferred_element_type=jnp.float32)

    def split_blocks(t):
        return t.reshape(bsz, nq, Q_BLOCK, nh, hd).transpose(1, 0, 2, 3, 4)

    out = lax.map(block, (jnp.arange(nq), split_blocks(q1), split_blocks(q2)))
    return out.transpose(1, 0, 2, 3, 4).reshape(bsz, seq, nh, v.shape[-1])


def grouped_moe(h, w_router, b_router, w_gate, w_up, w_down):
    bsz, seq, d = h.shape
    hf = h.reshape(-1, d)
    n_tok = hf.shape[0]
    scores = jax.nn.sigmoid(jnp.matmul(hf, w_router, preferred_element_type=jnp.float32))
    sel = scores + b_router.astype(jnp.float32)
    grp = sel.reshape(n_tok, N_GROUPS, EXPERTS_PER_GROUP)
    group_score = jnp.sum(lax.top_k(grp, 2)[0], -1)
    best_group = jnp.argmax(group_score, -1)
    in_group = (jnp.arange(N_EXPERTS) // EXPERTS_PER_GROUP)[None, :] == best_group[:, None]
    _, idx = lax.top_k(jnp.where(in_group, sel, -jnp.inf), TOP_K)
    w = jnp.take_along_axis(scores, idx, -1)
    w = w / jnp.sum(w, -1, keepdims=True)
    gates = jnp.sum(jax.nn.one_hot(idx, N_EXPERTS, dtype=jnp.float32) * w[..., None], 1)

    def expert(acc, xs):
        g, wg, wu, wd = xs
        y = (jax.nn.silu(hf @ wg) * (hf @ wu)) @ wd
        return acc + g[:, None].astype(hf.dtype) * y, None

    out, _ = lax.scan(expert, jnp.zeros_like(hf), (gates.T, w_gate, w_up, w_down))
    return out.reshape(bsz, seq, d)


def setup_inputs(seed: int = 0) -> dict:
    key = jax.random.key(seed)
    ks = jax.random.split(key, 24)
    nrm = jax.random.normal
    f32 = jnp.float32
    d = D_MODEL
    col_scale = jnp.concatenate([
        jnp.ones((2 * WIDTH_A,), f32), jnp.full((WIDTH_A,), BETA, f32),
        jnp.ones((2 * WIDTH_B,), f32), jnp.full((WIDTH_B,), BETA, f32)])
    return {
        "x": nrm(ks[0], (BATCH, SEQ, d), f32),
        "c": nrm(ks[1], (BATCH, d), f32),
        "rel_bias": 0.5 * nrm(ks[2], (NUM_BUCKETS, N_HEADS), f32),
        "w_in": nrm(ks[3], (DEPTH, d, IN_WIDTH), f32) * d ** -0.5 * col_scale,
        "w_o": nrm(ks[4], (DEPTH, WIDTH_A + WIDTH_B, d), f32) * (WIDTH_A + WIDTH_B) ** -0.5 * BETA,
        "lam_q1": 0.1 * nrm(ks[5], (DEPTH, HEAD_DIM), f32),
        "lam_k1": 0.1 * nrm(ks[6], (DEPTH, HEAD_DIM), f32),
        "lam_q2": 0.1 * nrm(ks[7], (DEPTH, HEAD_DIM), f32),
        "lam_k2": 0.1 * nrm(ks[8], (DEPTH, HEAD_DIM), f32),
        "subln_g": 1.0 + 0.01 * nrm(ks[9], (DEPTH, 2 * HEAD_DIM), f32),
        "w_ada": 0.5 * nrm(ks[10], (DEPTH, d, 6 * d), f32) * d ** -0.5,
        "b_ada": 0.02 * nrm(ks[11], (DEPTH, 6 * d), f32),
        "ln1_g": 1.0 + 0.01 * nrm(ks[12], (DEPTH, d), f32),
        "ln1_b": 0.01 * nrm(ks[13], (DEPTH, d), f32),
        "ln2_g": 1.0 + 0.01 * nrm(ks[14], (DEPTH, d), f32),
        "ln2_b": 0.01 * nrm(ks[15], (DEPTH, d), f32),
        "w_router": nrm(ks[16], (d, N_EXPERTS), f32) * d ** -0.5,
        "b_router": 0.01 * nrm(ks[17], (N_EXPERTS,), f32),
        "w_gate": nrm(ks[18], (DEPTH, N_EXPERTS, d, D_FF), f32) * d ** -0.5,
        "w_up": nrm(ks[19], (DEPTH, N_EXPERTS, d, D_FF), f32) * d ** -0.5 * BETA,
        "w_down": nrm(ks[20], (DEPTH, N_EXPERTS, D_FF, d), f32) * D_FF ** -0.5 * BETA,
    }


def reference(x, c, rel_bias, w_in, w_o, lam_q1, lam_k1, lam_q2, lam_k2, subln_g,
              w_ada, b_ada, ln1_g, ln1_b, ln2_g, ln2_b, w_router, b_router,
              w_gate, w_up, w_down):
    bsz, seq, d = x.shape
    bias_a = rel_bias[:, :N_HEADS_A]
    bias_b = rel_bias[:, N_HEADS_A:]
    c_act = jax.nn.silu(c)
    q_scale = HEAD_DIM ** -0.5
    splits = [WIDTH_A, 2 * WIDTH_A, 3 * WIDTH_A,
              3 * WIDTH_A + WIDTH_B, 3 * WIDTH_A + 2 * WIDTH_B]
    for l in range(DEPTH):
        mod = (c_act @ w_ada[l] + b_ada[l])[:, None, :]
        shift1, scale1, gate1, shift2, scale2, gate2 = jnp.split(mod, 6, axis=-1)

        h = x * (1 + scale1) + shift1
        qkv = h @ w_in[l]
        qa, ka, va, qb, kb, vb = jnp.split(qkv, splits, axis=-1)
        qa = qa.reshape(bsz, seq, N_HEADS_A, HEAD_DIM) * q_scale
        ka = ka.reshape(bsz, seq, N_HEADS_A, HEAD_DIM)
        va = va.reshape(bsz, seq, N_HEADS_A, HEAD_DIM)
        out_a = dilated_attention(qa, ka, va, bias_a).astype(x.dtype)
        out_a = out_a.reshape(bsz, seq, WIDTH_A)

        qb = qb.reshape(bsz, seq, N_HEADS_B, 2, HEAD_DIM) * q_scale
        kb = kb.reshape(bsz, seq, N_HEADS_B, 2, HEAD_DIM)
        vb = vb.reshape(bsz, seq, N_HEADS_B, 2 * HEAD_DIM)
        lam_init = 0.8 - 0.6 * math.exp(-0.3 * l)
        lam = (jnp.exp(jnp.sum(lam_q1[l].astype(jnp.float32) * lam_k1[l].astype(jnp.float32)))
               - jnp.exp(jnp.sum(lam_q2[l].astype(jnp.float32) * lam_k2[l].astype(jnp.float32)))
               + lam_init)
        out_b = diff_attention(qb[..., 0, :], qb[..., 1, :], kb[..., 0, :], kb[..., 1, :],
                               vb, lam, bias_b).astype(x.dtype)
        out_b = rms_norm(out_b, subln_g[l]) * (1.0 - lam_init)
        out_b = out_b.reshape(bsz, seq, WIDTH_B)

        mix = jnp.concatenate([out_a, out_b], axis=-1) @ w_o[l]
        x = layer_norm(ALPHA * x + gate1 * mix, ln1_g[l], ln1_b[l])

        h = x * (1 + scale2) + shift2
        ffn = grouped_moe(h, w_router, b_router, w_gate[l], w_up[l], w_down[l])
        x = layer_norm(ALPHA * x + gate2 * ffn, ln2_g[l], ln2_b[l])
    return x
```

```python
import math
import numpy as np
import concourse.bass as bass
import concourse.mybir as mybir
from concourse.bass_utils import run_bass_kernel_spmd

F32 = mybir.dt.float32
BF16 = mybir.dt.bfloat16
AF = mybir.ActivationFunctionType
ALU = mybir.AluOpType
AX = mybir.AxisListType

D = 1024
SEQ = 4096
DEPTH = 4
NT = SEQ // 128
NW = SEQ // 512
NE = 16
DFF = 512
ALPHA = (2.0 * DEPTH) ** 0.25
LN_EPS = 1e-5
W_STRIP = 2176
WF = 2304
NEG = -30000.0
BIG = 100.0
SEM_LIMIT = 30000


def _t5_bucket_np(d):
    n = np.maximum(d, 0)
    nf = np.maximum(n, 16).astype(np.float32)
    large = 16 + (np.log(nf / np.float32(16)) / np.float32(math.log(2048 / 16)) * np.float32(16)).astype(np.int32)
    large = np.minimum(large, 31)
    return np.where(n < 16, n, large)


def _host_consts():
    m = np.arange(WF)
    d = m - 127
    bucket = _t5_bucket_np(d)
    valid_c = d >= 0
    mult = ((d <= 128).astype(np.int32) + ((d % 4 == 0) & (d <= 512)).astype(np.int32)
            + ((d % 16 == 0) & (d <= 2048)).astype(np.int32))
    mult = np.where(valid_c, mult, 0)
    oh = np.zeros((2, 32, WF), np.float32)
    add = np.zeros((12, WF), np.float32)
    for b in range(32):
        oh[0, b] = ((bucket == b) & (mult > 0)).astype(np.float32)
        oh[1, b] = ((bucket == b) & valid_c).astype(np.float32)
    add_dil = np.where(mult > 0, np.log(np.maximum(mult, 1)).astype(np.float32), np.float32(NEG))
    add_dif = np.where(valid_c, np.float32(0), np.float32(NEG))
    add[:8] = add_dil[None]
    add[8:] = add_dif[None]
    ident = np.eye(128, dtype=np.float32)
    flip = ident[::-1].copy()
    return {"c_oh": oh, "c_add": add, "c_ident": ident, "c_flip": flip}


class Buf:
    __slots__ = ("w", "rd")
    EPOCH = None
    REG = []

    def __init__(self):
        self.w = Buf.EPOCH
        self.rd = {}
        Buf.REG.append(self)


class _Eng:
    def __init__(self, S, name, h, compute):
        self.S, self.name, self.h, self.compute = S, name, h, compute
        self.sem = None
        self.count = 0
        self.own = set()
        self.waited = {}
        self.nsem = 0
        self.new_sem()

    def new_sem(self):
        self.sem = self.S.nc.alloc_semaphore(f"s_{self.name}_{self.nsem}")
        self.nsem += 1
        self.count = 0
        self.own.add(id(self.sem))


class Chan:
    def __init__(self, S, name):
        self.S, self.name = S, name
        self.n = 0
        self.sem = S.nc.alloc_semaphore(f"c_{name}_0")
        self.count = 0
        self.last = None


class Sched:
    def __init__(self, nc):
        self.nc = nc
        self.e = {
            "pe": _Eng(self, "pe", nc.tensor, True),
            "act": _Eng(self, "act", nc.scalar, True),
            "dve": _Eng(self, "dve", nc.vector, True),
            "pool": _Eng(self, "pool", nc.gpsimd, True),
            "sp": _Eng(self, "sp", nc.sync, False),
        }
        self.sems = {}
        self.ninst = 0
        self.log = {k: [] for k in self.e}

    def _wait(self, X, evs):
        best = {}
        for sem, v in evs:
            k = id(sem)
            self.sems[k] = sem
            if v > best.get(k, 0):
                best[k] = v
        for k, v in best.items():
            if X.waited.get(k, 0) < v:
                X.h.wait_ge(self.sems[k], v)
                X.waited[k] = v
                self.log[X.name].append(("w", k, v, self.ninst))

    def _deps(self, X, rd, wr, is_dma):
        evs = []
        for b in rd:
            if b.w is not None:
                sem, v = b.w
                if is_dma or not (X.name == "pe" and id(sem) in X.own):
                    evs.append(b.w)
        for b in wr:
            if b.w is not None:
                sem, v = b.w
                if is_dma or id(sem) not in X.own or (b in rd and X.name != "pe"):
                    evs.append(b.w)
            for k, v in b.rd.items():
                if is_dma or k not in X.own:
                    evs.append((self.sems[k], v))
        return evs

    def _commit(self, ev, rd, wr):
        sem, v = ev
        k = id(sem)
        self.sems[k] = sem
        for b in rd:
            if b.rd.get(k, 0) < v:
                b.rd[k] = v
        for b in wr:
            b.w = ev
            b.rd = {}

    def op(self, eng, fn, rd=(), wr=(), sig=True):
        X = self.e[eng]
        self._wait(X, self._deps(X, rd, wr, False))
        ins = fn(X.h)
        self.ninst += 1
        if sig:
            ins.then_inc(X.sem, 1)
            X.count += 1
            ev = (X.sem, X.count)
            self.log[X.name].append(("i", id(X.sem), 1, self.ninst))
        else:
            ev = (X.sem, X.count + 1)
        self._commit(ev, rd, wr)
        self.last_ev = ev
        if sig and X.count >= SEM_LIMIT:
            X.new_sem()
        return ins

    def dma(self, q, out, in_, rd=(), wr=(), chan=None, **kw):
        X = self.e[q]
        evs = self._deps(X, rd, wr, True)
        if chan.last is not None:
            evs.append(chan.last)
        self._wait(X, evs)
        if chan.count >= SEM_LIMIT:
            chan.n += 1
            chan.sem = self.nc.alloc_semaphore(f"c_{chan.name}_{chan.n}")
            chan.count = 0
        ins = X.h.dma_start(out=out, in_=in_, **kw)
        ins.then_inc(chan.sem, 16)
        chan.count += 16
        self.log[X.name].append(("i", id(chan.sem), 16, self.ninst))
        ev = (chan.sem, chan.count)
        chan.last = ev
        self._commit(ev, rd, wr)
        self.ninst += 1
        return ev

    def fence(self, bufs, scratch):
        bufs = [b for b in bufs]
        self.op("dve", lambda e: e.memset(scratch, 0.0), rd=tuple(bufs), wr=tuple(bufs))
        Buf.EPOCH = self.last_ev

    def check_deadlock(self):
        val = {}
        pos = {k: 0 for k in self.log}
        progress = True
        while progress:
            progress = False
            for k, lg in self.log.items():
                while pos[k] < len(lg):
                    t, sem, v, n = lg[pos[k]]
                    if t == "w":
                        if val.get(sem, 0) >= v:
                            pos[k] += 1
                            progress = True
                        else:
                            break
                    else:
                        val[sem] = val.get(sem, 0) + v
                        pos[k] += 1
                        progress = True
        stuck = {k: (pos[k], len(lg), lg[pos[k]] if pos[k] < len(lg) else None) for k, lg in self.log.items()}
        ok = all(pos[k] == len(lg) for k, lg in self.log.items())
        return ok, stuck, val

    def wait_event(self, eng, ev):
        self._wait(self.e[eng], [ev])


def build(layers=(0, 1, 2, 3), dbg=None, skip=""):
    nc = bass.Bass("TRN2", target_bir_lowering=False)
    S = Sched(nc)
    Buf.EPOCH = None
    Buf.REG = []
    dt_in = lambda name, shape: nc.dram_tensor(name, list(shape), F32, kind="ExternalInput").ap()
    x_in = dt_in("x", (SEQ, D))
    c_in = dt_in("c", (1, D))
    rel_bias = dt_in("rel_bias", (32, 12))
    w_in = dt_in("w_in", (DEPTH, D, 3072))
    w_o = dt_in("w_o", (DEPTH, D, D))
    lam_q1 = dt_in("lam_q1", (DEPTH, 64))
    lam_k1 = dt_in("lam_k1", (DEPTH, 64))
    lam_q2 = dt_in("lam_q2", (DEPTH, 64))
    lam_k2 = dt_in("lam_k2", (DEPTH, 64))
    subln_g = dt_in("subln_g", (DEPTH, 128))
    w_ada = dt_in("w_ada", (DEPTH, D, 6 * D))
    b_ada = dt_in("b_ada", (DEPTH, 6 * D))
    ln1_g = dt_in("ln1_g", (DEPTH, D))
    ln1_b = dt_in("ln1_b", (DEPTH, D))
    ln2_g = dt_in("ln2_g", (DEPTH, D))
    ln2_b = dt_in("ln2_b", (DEPTH, D))
    w_router = dt_in("w_router", (D, NE))
    b_router = dt_in("b_router", (1, NE))
    w_gate = dt_in("w_gate", (DEPTH, NE, D, DFF))
    w_up = dt_in("w_up", (DEPTH, NE, D, DFF))
    w_down = dt_in("w_down", (DEPTH, NE, DFF, D))
    c_oh = dt_in("c_oh", (2, 32, WF))
    c_add = dt_in("c_add", (12, WF))
    c_ident = dt_in("c_ident", (128, 128))
    c_flip = dt_in("c_flip", (128, 128))
    out = nc.dram_tensor("out", [SEQ, D], F32, kind="ExternalOutput").ap()
    xs_a = nc.dram_tensor("xs_a", [SEQ, D], F32, kind=("ExternalOutput" if dbg else "Internal")).ap()
    xs_b = nc.dram_tensor("xs_b", [SEQ, D], F32, kind="Internal").ap()
    f_dram = nc.dram_tensor("f_dram", [12, WF], F32, kind="Internal")

    sb = lambda name, shape, dt: nc.alloc_sbuf_tensor(name, list(shape), dt).ap()
    PS = [nc.alloc_psum_tensor(f"ps{i}", [128, 512], F32).ap() for i in range(8)]
    PSB = [Buf() for _ in range(8)]

    ident_f = sb("ident_f", (128, 128), F32)
    ident_b = sb("ident_b", (128, 128), BF16)
    flip_b = sb("flip_b", (128, 128), BF16)
    ones_f = sb("ones_f", (128, 128), F32)
    zeros_b = sb("zeros_b", (128, 512), BF16)
    modT = sb("modT", (128, DEPTH, 48), F32)
    sc1p = sb("sc1p", (128, DEPTH, 8), F32)
    sc2p = sb("sc2p", (128, DEPTH, 8), F32)
    neg_lam = sb("neg_lam", (128, DEPTH), F32)
    gsub = sb("gsub", (128, DEPTH, 128), F32)
    wr_b = sb("wr_b", (128, 8, NE), BF16)
    br_b = sb("br_b", (128, NE), F32)
    fscr = sb("fscr", (128, 1), F32)
    B_const = Buf()

    ch_const = Chan(S, "const")
    ch_constp = Chan(S, "constp")

    def cdma(out_ap, in_ap, q="sp", **kw):
        return S.dma(q, out_ap, in_ap, rd=(), wr=(B_const,), chan=(ch_const if q == "sp" else ch_constp), **kw)

    with nc.sbuf_tensor("pro_region", [128, 128 * 1024 // 4], F32) as pro_h, \
            nc.sbuf_tensor("cTb", [128, 8], BF16) as cTb_h, \
            nc.sbuf_tensor("wblk", [128, 4, 8, 512], BF16) as wblk_h:
        pro = pro_h.ap()
        off = [0]

        def carve(n_f32):
            a = pro[:, off[0]:off[0] + n_f32]
            off[0] += n_f32
            return a

        cdma(ident_f, c_ident)
        cdma(ident_b, c_ident, q="pool")
        cdma(flip_b, c_flip, q="pool")
        S.op("dve", lambda e: e.memset(ones_f, 1.0), wr=(B_const,))
        S.op("dve", lambda e: e.memset(zeros_b, 0.0), wr=(B_const,))
        cdma(wr_b, w_router.rearrange("(c p) n -> p c n", p=128), q="pool")
        cdma(br_b, b_router.partition_broadcast(128))
        c_rows = carve(128)[0:8, :]
        cdma(c_rows, c_in.rearrange("o (j p) -> (o j) p", p=128))
        ca_rows = carve(128)[0:8, :]
        S.op("act", lambda e: e.activation(out=ca_rows, in_=c_rows, func=AF.Silu), rd=(B_const,), wr=(B_const,))
        cT = carve(8)
        S.op("pe", lambda e: e.matmul(PS[0][:, 0:8], lhsT=ca_rows, rhs=ident_f[0:8, 0:8], start=True, stop=True),
             rd=(B_const,), wr=(PSB[0],))
        S.op("dve", lambda e: e.tensor_copy(out=cT, in_=PS[0][:, 0:8]), rd=(PSB[0],), wr=(B_const,))
        cTb_t = cTb_h.ap()
        S.op("dve", lambda e: e.tensor_copy(out=cTb_t, in_=cT), rd=(B_const,), wr=(B_const,))
        brow = carve(6 * D)[0:1, :]
        mrow = carve(6 * D)[0:1, :]
        rowB = Buf()
        wblk_t = wblk_h.ap()
        wblkB = [Buf() for _ in range(4)]
        wch = [Chan(S, f"wb{i}") for i in range(4)]
        it = 0
        for l in range(DEPTH):
            cdma(brow, b_ada[l:l + 1, :])
            for cb in range(12):
                s_ = it % 4
                it += 1
                S.dma("pool", wblk_t[:, s_], w_ada[l, :, cb * 512:(cb + 1) * 512].rearrange("(c p) n -> p c n", p=128),
                      wr=(wblkB[s_],), chan=wch[s_])
                bk = 3 + (cb % 2)
                for kc in range(8):
                    S.op("pe", lambda e, s_=s_, kc=kc, bk=bk: e.matmul(
                        PS[bk][0:1, :], lhsT=cTb_t[:, kc:kc + 1], rhs=wblk_t[:, s_, kc, :], start=(kc == 0), stop=(kc == 7)),
                        rd=(wblkB[s_], B_const), wr=(PSB[bk],), sig=(kc == 7))
                S.op("dve", lambda e, cb=cb, bk=bk: e.tensor_tensor(
                    out=mrow[:, cb * 512:(cb + 1) * 512], in0=PS[bk][0:1, :], in1=brow[:, cb * 512:(cb + 1) * 512], op=ALU.add),
                    rd=(PSB[bk], B_const), wr=(rowB,))
            for j in range(48):
                S.op("pe", lambda e, j=j, l=l: e.matmul(
                    PS[2][:, l * 48 + j:l * 48 + j + 1], lhsT=mrow[:, j * 128:(j + 1) * 128], rhs=ones_f[0:1, 0:1],
                    start=True, stop=True), rd=(rowB, B_const), wr=(PSB[2],), sig=(j == 47))
        modT_flat = modT.rearrange("p l j -> p (l j)")
        S.op("dve", lambda e: e.tensor_copy(out=modT_flat, in_=PS[2][:, 0:192]), rd=(PSB[2],), wr=(B_const,))
        S.op("dve", lambda e: e.tensor_scalar(out=sc1p, in0=modT[:, :, 8:16], scalar1=1.0, scalar2=None, op0=ALU.add),
             rd=(B_const,), wr=(B_const,))
        S.op("dve", lambda e: e.tensor_scalar(out=sc2p, in0=modT[:, :, 32:40], scalar1=1.0, scalar2=None, op0=ALU.add),
             rd=(B_const,), wr=(B_const,))
        lam_t = [carve(DEPTH * 64) for _ in range(4)]
        for t_, src in zip(lam_t, (lam_q1, lam_k1, lam_q2, lam_k2)):
            cdma(t_, src.rearrange("l e -> (l e)").partition_broadcast(128))
        prod = carve(DEPTH * 64)
        ssum = [carve(DEPTH), carve(DEPTH)]
        for i in range(2):
            S.op("dve", lambda e, i=i: e.tensor_tensor(out=prod, in0=lam_t[2 * i], in1=lam_t[2 * i + 1], op=ALU.mult),
                 rd=(B_const,), wr=(B_const,))
            S.op("dve", lambda e, i=i: e.tensor_reduce(out=ssum[i], in_=prod.rearrange("p (l e) -> p l e", l=DEPTH),
                                                    axis=AX.X, op=ALU.add), rd=(B_const,), wr=(B_const,))
            S.op("act", lambda e, i=i: e.activation(out=ssum[i], in_=ssum[i], func=AF.Exp), rd=(B_const,), wr=(B_const,))
        S.op("dve", lambda e: e.tensor_tensor(out=neg_lam, in0=ssum[1], in1=ssum[0], op=ALU.subtract),
             rd=(B_const,), wr=(B_const,))
        cdma(gsub, subln_g.rearrange("l e -> (l e)").partition_broadcast(128))
        for l in range(DEPTH):
            lam_init = 0.8 - 0.6 * math.exp(-0.3 * l)
            S.op("dve", lambda e, l=l, li=lam_init: e.tensor_scalar(out=neg_lam[:, l:l + 1], in0=neg_lam[:, l:l + 1],
                                                                  scalar1=-li, scalar2=None, op0=ALU.add),
                 rd=(B_const,), wr=(B_const,))
            S.op("dve", lambda e, l=l, li=lam_init: e.tensor_scalar(out=gsub[:, l, :], in0=gsub[:, l, :],
                                                                  scalar1=1.0 - li, scalar2=None, op0=ALU.mult),
                 rd=(B_const,), wr=(B_const,))
        rb = carve(12)[0:32, :]
        cdma(rb, rel_bias)
        oh_sb = carve(2 * WF).rearrange("p (k m) -> p k m", k=2)[0:32]
        cdma(oh_sb, c_oh.rearrange("k b m -> b k m"))
        add_sb = carve(WF)[0:12, :]
        cdma(add_sb, c_add)
        f_sb = carve(WF)[0:12, :]
        fd_sb = carve(WF)[0:12, :]
        for kind, dst in ((0, f_sb), (1, fd_sb)):
            for cc in range(0, WF, 512):
                n = min(512, WF - cc)
                bk = 3 + (cc // 512) % 2
                S.op("pe", lambda e, kind=kind, cc=cc, n=n, bk=bk: e.matmul(
                    PS[bk][0:12, 0:n], lhsT=rb, rhs=oh_sb[:, kind, cc:cc + n], start=True, stop=True),
                    rd=(B_const,), wr=(PSB[bk],))
                S.op("dve", lambda e, dst=dst, cc=cc, n=n, bk=bk: e.tensor_tensor(
                    out=dst[:, cc:cc + n], in0=PS[bk][0:12, 0:n], in1=add_sb[:, cc:cc + n], op=ALU.add),
                    rd=(PSB[bk], B_const), wr=(B_const,))
        S.op("act", lambda e: e.activation(out=f_sb, in_=f_sb, func=AF.Exp), rd=(B_const,), wr=(B_const,))
        S.op("act", lambda e: e.activation(out=fd_sb, in_=fd_sb, func=AF.Exp), rd=(B_const,), wr=(B_const,))
        B_f = Buf()
        ch_f = Chan(S, "fdram")
        fd_ap = f_dram.ap()
        S.dma("sp", fd_ap[0:8, :], f_sb[0:8, :], rd=(B_const,), wr=(B_f,), chan=ch_f)
        S.dma("sp", fd_ap[8:12, :], fd_sb[8:12, :], rd=(B_const,), wr=(B_f,), chan=ch_f)
        S.fence(list(Buf.REG), fscr)

    ch_xl = [Chan(S, f"xl{i}") for i in range(4)]
    ch_xs = [Chan(S, f"xs{i}") for i in range(4)]
    ch_w = [Chan(S, f"w{i}") for i in range(8)]
    ch_bc = Chan(S, "bc")

    def bcast_rows(dst, l, col0, tmp, tmpB):
        for j in range(8):
            bk = 6 + (j % 2)
            S.op("dve", lambda e, j=j: e.tensor_scalar(out=tmp, in0=ident_f, scalar1=modT[:, l, col0 + j:col0 + j + 1],
                                                     scalar2=None, op0=ALU.mult), rd=(B_const,), wr=(tmpB,))
            S.op("pe", lambda e, bk=bk: e.matmul(PS[bk][:, 0:128], lhsT=ones_f, rhs=tmp, start=True, stop=True),
                 rd=(tmpB, B_const), wr=(PSB[bk],))
            S.op("dve", lambda e, j=j, bk=bk: e.tensor_copy(out=dst[:, j * 128:(j + 1) * 128], in_=PS[bk][:, 0:128]),
                 rd=(PSB[bk],), wr=(tmpB,))

    def ln_tail(y, stats, mv, rstd, g_b, b_b, yB, dst_dram, ch, eps_t, dstB, lnB, use_pool=False, act_norm=True):
        for hh in range(2):
            S.op("dve", lambda e, hh=hh: e.bn_stats(out=stats[:, hh, :], in_=y[:, hh * 512:(hh + 1) * 512]),
                 rd=(yB,), wr=(yB,))
        S.op("dve", lambda e: e.bn_aggr(out=mv, in_=stats.rearrange("p a b -> p (a b)")), rd=(yB,), wr=(yB,))
        S.op("act", lambda e: e.activation(out=rstd, in_=mv[:, 1:2], func=AF.Ln, bias=eps_t, scale=1.0), rd=(yB, B_const), wr=(yB,))
        S.op("act", lambda e: e.activation(out=rstd, in_=rstd, func=AF.Exp, scale=-0.5), rd=(yB,), wr=(yB,))
        if act_norm:
            S.op("dve", lambda e: e.tensor_scalar(out=mv[:, 1:2], in0=mv[:, 0:1], scalar1=rstd, scalar2=-1.0, op0=ALU.mult, op1=ALU.mult),
                 rd=(yB,), wr=(yB,))
            S.op("act", lambda e: e.activation(out=y, in_=y, func=AF.Identity, bias=mv[:, 1:2], scale=rstd), rd=(yB,), wr=(yB,))
        else:
            S.op("dve", lambda e: e.tensor_scalar(out=y, in0=y, scalar1=mv[:, 0:1], scalar2=rstd, op0=ALU.subtract, op1=ALU.mult),
                 rd=(yB,), wr=(yB,))
        eng = "pool" if use_pool else "dve"
        S.op(eng, lambda e: e.tensor_tensor(out=y, in0=y, in1=g_b, op=ALU.mult), rd=(yB, lnB), wr=(yB,))
        S.op(eng, lambda e: e.tensor_tensor(out=y, in0=y, in1=b_b, op=ALU.add), rd=(yB, lnB), wr=(yB,))
        S.dma("sp", dst_dram, y, rd=(yB,), wr=(dstB,), chan=ch)

    eps_t = sb("eps_t", (128, 1), F32)
    S.op("dve", lambda e: e.memset(eps_t, LN_EPS), wr=(B_const,))
    neghalf = sb("neghalf", (128, 8), F32)
    S.op("dve", lambda e: e.memset(neghalf, -0.5), wr=(B_const,))

    def stream_bufs(li, n_layers):
        src = x_in if li == 0 else xs_b
        dst = out if li == n_layers - 1 else xs_b
        return src, xs_a, dst

    B_xmid = [Buf() for _ in range(NT)]
    B_xdst = [Buf() for _ in range(NT)]

    for li, l in enumerate(layers):
        x_src, x_mid, x_dst = stream_bufs(li, len(layers))
        reg0 = len(Buf.REG)
        if "A" in skip:
            pass
        else:
          with nc.sbuf_tensor(f"hT{li}", [128, 8, SEQ], BF16) as hT_h, \
                nc.sbuf_tensor(f"rOT{li}", [128, 8, SEQ], BF16) as OT_h:
            hT = hT_h.ap()
            OT = OT_h.ap()
            hTB = [[Buf() for _ in range(NW)] for _ in range(8)]
            OTB = [[Buf() for _ in range(NW)] for _ in range(8)]
            with nc.sbuf_tensor(f"xst{li}", [128, 2, 4, D], F32) as xst_h:
                otf = OT.rearrange("p c t -> p (c t)")
                xbf = [otf[:, i * 4096:(i + 1) * 4096].rearrange("p (s d) -> p s d", s=4) for i in range(2)]
                xstage = [xst_h.ap()[:, i] for i in range(2)]
                xsB = [Buf(), Buf()]
                xbB = [Buf(), Buf()]
                for tw in range(NW):
                    s = tw % 2
                    S.dma("sp", xstage[s], x_src[tw * 512:(tw + 1) * 512, :].rearrange("(s p) d -> p s d", p=128),
                          rd=tuple(B_xdst[tw * 4:tw * 4 + 4]), wr=(xsB[s],), chan=ch_xl[s])
                    for ss in range(4):
                        S.op("dve", lambda e, s=s, ss=ss: e.tensor_copy(out=xbf[s][:, ss, :], in_=xstage[s][:, ss, :]),
                             rd=(xsB[s],), wr=(xbB[s],))
                    for c in range(8):
                        bk = 6 + (c % 2)
                        for ss in range(4):
                            S.op("pe", lambda e, s=s, ss=ss, c=c, bk=bk: e.matmul(
                                PS[bk][:, ss * 128:(ss + 1) * 128], lhsT=xbf[s][:, ss, c * 128:(c + 1) * 128],
                                rhs=ident_b, start=True, stop=True), rd=(xbB[s], B_const), wr=(PSB[bk],), sig=(ss == 3))
                        S.op("act", lambda e, c=c, tw=tw, bk=bk: e.activation(
                            out=hT[:, c, tw * 512:(tw + 1) * 512], in_=PS[bk], func=AF.Identity,
                            bias=modT[:, l, c:c + 1], scale=sc1p[:, l, c:c + 1]),
                            rd=(PSB[bk], B_const), wr=(hTB[c][tw],))
                S.fence(Buf.REG[reg0:], fscr)
            with nc.sbuf_tensor(f"grp{li}", [128, 4096 * 3 + 32 * 130 + 4 * W_STRIP + 2 * 8 * 384 + 6 * 512 + 4 * 128], BF16) as G_h, \
                    nc.sbuf_tensor(f"accS{li}", [128, 1040], F32) as accS_h, \
                    nc.sbuf_tensor(f"sml{li}", [128, 64], F32) as sml_h:
                G = G_h.ap()
                go = [0]

                def gcarve(n):
                    a = G[:, go[0]:go[0] + n]
                    go[0] += n
                    return a

                KT = gcarve(4096)
                QZ = [gcarve(4096), gcarve(4096)]
                Vt = gcarve(32 * 130)
                strip = [gcarve(W_STRIP), gcarve(W_STRIP)]
                stripH = [gcarve(W_STRIP), gcarve(W_STRIP)]
                stripHB = [Buf(), Buf()]
                Wg = [gcarve(8 * 384).rearrange("p (c n) -> p c n", c=8) for _ in range(2)]
                PT = [gcarve(512) for _ in range(6)]
                Otok = gcarve(4 * 128).rearrange("p (s n) -> p s n", s=4)
                KTB = [Buf() for _ in range(NW)]
                QZB = [[Buf() for _ in range(NW)] for _ in range(2)]
                VB = [Buf() for _ in range(NW)]
                stripB = [Buf(), Buf()]
                WgB = [Buf(), Buf()]
                PTB = [Buf() for _ in range(6)]
                accS = accS_h.ap()
                accSB = Buf()
                OtokB = Buf()
                small = sml_h.ap()
                smallB = Buf()
                S.op("dve", lambda e: e.memset(QZ[0][64:128, :], 0.0), wr=tuple(QZB[0]))
                S.op("dve", lambda e: e.memset(QZ[1][0:64, :], 0.0), wr=tuple(QZB[1]))

                def load_group_weights(g, slot):
                    if g < 4:
                        cols = (g * 128, 512 + g * 128, 1024 + g * 128)
                    else:
                        hh = g - 4
                        cols = (1536 + hh * 128, 2048 + hh * 128, 2560 + hh * 128)
                    for i, c0 in enumerate(cols):
                        S.dma("pool", Wg[slot][:, :, i * 128:(i + 1) * 128],
                              w_in[l, :, c0:c0 + 128].rearrange("(c p) n -> p c n", p=128),
                              wr=(WgB[slot],), chan=ch_w[slot * 3 + i] if slot == 0 else ch_w[3 + i])

                load_group_weights(0, 0)
                pt_i = [0]
                st_i = [0]
                pending_tr = []
                bg = []
                for g in range(8):
                    slot = g % 2
                    dil = g < 4
                    if g + 1 < 8:
                        load_group_weights(g + 1, 1 - slot)
                    heads = (2 * g, 2 * g + 1) if dil else (8 + g - 4,)
                    for u, hd in enumerate(heads):
                        S.dma("pool", stripH[u], bass.AP(f_dram, hd * WF, [[1, 128], [1, W_STRIP]]),
                              rd=(B_f,), wr=(stripHB[u],), chan=ch_w[6 + u], max_dma_last_dim=4352)
                    EV = 65 if dil else 129
                    Vv = Vt[:, 0:32 * 130].rearrange("p (t n) -> p t n", t=32) if dil else \
                        Vt[:, 0:32 * 129].rearrange("p (t n) -> p t n", t=32)
                    if dil:
                        S.op("dve", lambda e, Vv=Vv: e.memset(Vv[:, :, 64:65], 1.0), wr=tuple(VB))
                        S.op("dve", lambda e, Vv=Vv: e.memset(Vv[:, :, 129:130], 1.0), wr=tuple(VB))
                    else:
                        S.op("dve", lambda e, Vv=Vv: e.memset(Vv[:, :, 128:129], 1.0), wr=tuple(VB))
                    for tw in range(NW):
                        tsl = slice(tw * 512, (tw + 1) * 512)
                        for _ in range(3):
                            if bg:
                                bg.pop(0)()
                        bk = 6
                        for kc in range(8):
                            S.op("pe", lambda e, kc=kc, tsl=tsl, bk=bk: e.matmul(
                                PS[bk], lhsT=Wg[slot][:, kc, 0:128], rhs=hT[:, kc, tsl], start=(kc == 0), stop=(kc == 7)),
                                rd=(WgB[slot], hTB[kc][tw]), wr=(PSB[bk],), sig=(kc == 7))
                        S.op("dve", lambda e, tsl=tsl, bk=bk: e.tensor_scalar(
                            out=QZ[0][0:64, tsl], in0=PS[bk][0:64, :], scalar1=0.125, scalar2=None, op0=ALU.mult),
                            rd=(PSB[bk],), wr=(QZB[0][tw],))
                        S.op("act", lambda e, tsl=tsl, bk=bk: e.activation(
                            out=QZ[1][64:128, tsl], in_=PS[bk][64:128, :], func=AF.Copy, scale=0.125),
                            rd=(PSB[bk],), wr=(QZB[1][tw],))
                        bk = 7
                        for kc in range(8):
                            S.op("pe", lambda e, kc=kc, tsl=tsl, bk=bk: e.matmul(
                                PS[bk], lhsT=Wg[slot][:, kc, 128:256], rhs=hT[:, kc, tsl], start=(kc == 0), stop=(kc == 7)),
                                rd=(WgB[slot], hTB[kc][tw]), wr=(PSB[bk],), sig=(kc == 7))
                        S.op("dve", lambda e, tsl=tsl, bk=bk: e.tensor_copy(out=KT[:, tsl], in_=PS[bk]),
                             rd=(PSB[bk],), wr=(KTB[tw],))
                        bk = 3 + (tw % 2)
                        for ss in range(4):
                            t = tw * 4 + ss
                            for kc in range(8):
                                S.op("pe", lambda e, kc=kc, t=t, ss=ss, bk=bk: e.matmul(
                                    PS[bk][:, ss * 128:(ss + 1) * 128], lhsT=hT[:, kc, t * 128:(t + 1) * 128],
                                    rhs=Wg[slot][:, kc, 256:384], start=(kc == 0), stop=(kc == 7)),
                                    rd=(WgB[slot], hTB[kc][tw]), wr=(PSB[bk],), sig=(kc == 7 and ss == 3))
                        pv = PS[bk].rearrange("p (s n) -> p s n", s=4)
                        if dil:
                            S.op("dve", lambda e, tw=tw, pv=pv, Vv=Vv: e.tensor_copy(
                                out=Vv[:, tw * 4:(tw + 1) * 4, 0:64], in_=pv[:, :, 0:64]), rd=(PSB[bk],), wr=(VB[tw],))
                            S.op("act", lambda e, tw=tw, pv=pv, Vv=Vv: e.activation(
                                out=Vv[:, tw * 4:(tw + 1) * 4, 65:129], in_=pv[:, :, 64:128], func=AF.Copy),
                                rd=(PSB[bk],), wr=(VB[tw],))
                        else:
                            S.op("dve", lambda e, tw=tw, pv=pv, Vv=Vv: e.tensor_copy(
                                out=Vv[:, tw * 4:(tw + 1) * 4, 0:128], in_=pv), rd=(PSB[bk],), wr=(VB[tw],))
                    for u, hd in enumerate(heads):
                        for ci, cc in enumerate(range(0, W_STRIP, 512)):
                            n = min(512, W_STRIP - cc)
                            bk = 6 + ci % 2
                            S.op("pe", lambda e, cc=cc, n=n, bk=bk, u=u: e.matmul(PS[bk][:, 0:n], lhsT=flip_b, rhs=stripH[u][:, cc:cc + n],
                                                                               start=True, stop=True),
                                 rd=(stripHB[u], B_const), wr=(PSB[bk],))
                            S.op("dve", lambda e, cc=cc, n=n, bk=bk, u=u: e.tensor_copy(out=strip[u][:, cc:cc + n], in_=PS[bk][:, 0:n]),
                                 rd=(PSB[bk],), wr=(stripB[u],))
                    if True:
                        if dil:
                            accb = [3, 4]
                            reg = lambda u, i: PS[3 + u][:, i * 65:(i + 1) * 65]
                            regb = lambda u, i: PSB[3 + u]
                        else:
                            accb = [3, 4, 5]
                            reg = lambda u, i: PS[3 + (u * 4 + i) // 3][:, ((u * 4 + i) % 3) * 129:((u * 4 + i) % 3 + 1) * 129]
                            regb = lambda u, i: PSB[3 + (u * 4 + i) // 3]
                        items = []
                        for qw in range(NW):
                            kts = list(range(max(0, 4 * qw - 16), 4 * qw + 4)) if dil else list(range(0, 4 * qw + 4))
                            w_items = []
                            for kt in kts:
                                for u in range(2):
                                    q_lo = max(kt, 4 * qw)
                                    q_hi = min(4 * qw + 3, kt + 16) if dil else 4 * qw + 3
                                    if q_hi - q_lo + 1 > 0:
                                        w_items.append([qw, kt, u, q_lo, q_hi, False, False])
                            w_items[0][5] = True
                            w_items[-1][6] = True
                            items += w_items

                        def emit_st(it):
                            qw, kt, u, q_lo, q_hi, _, _ = it
                            nv = q_hi - q_lo + 1
                            N = nv * 128
                            offq = (q_lo - kt) * 128
                            offs = min(offq, W_STRIP - N)
                            sb_ = st_i[0] % 3
                            st_i[0] += 1
                            su = u if dil else 0
                            S.op("pe", lambda e: e.matmul(
                                PS[sb_][:, 0:N], lhsT=KT[:, kt * 128:(kt + 1) * 128],
                                rhs=QZ[u][:, q_lo * 128:q_lo * 128 + N], start=True, stop=True),
                                rd=(KTB[kt // 4],) + tuple(QZB[u][q_lo // 4:q_hi // 4 + 1]), wr=(PSB[sb_],))
                            pi = pt_i[0] % 6
                            pt_i[0] += 1
                            S.op("act", lambda e: e.activation(
                                out=PT[pi][:, 0:N], in_=PS[sb_][:, 0:N], func=AF.Exp), rd=(PSB[sb_],), wr=(PTB[pi],))
                            S.op("dve", lambda e: e.tensor_tensor(
                                out=PT[pi][:, 0:N], in0=PT[pi][:, 0:N], in1=strip[su][:, offs:offs + N], op=ALU.mult),
                                rd=(PTB[pi], stripB[su]), wr=(PTB[pi],))
                            return pi

                        def emit_pv(it, pi):
                            qw, kt, u, q_lo, q_hi, first, last = it
                            if first:
                                for b_ in accb:
                                    S.op("pe", lambda e, b_=b_: e.matmul(PS[b_], lhsT=zeros_b[:, 0:128], rhs=zeros_b,
                                                                        start=True, stop=True, skip_group_check=True),
                                         rd=(B_const,), wr=(PSB[b_],))
                            nv = q_hi - q_lo + 1
                            for i in range(nv):
                                qt = q_lo + i
                                qi = qt - 4 * qw
                                vr = Vv[:, kt, u * 65:(u + 1) * 65] if dil else Vv[:, kt, 0:129]
                                S.op("pe", lambda e, i=i, qi=qi, vr=vr, qt=qt: e.matmul(
                                    reg(u, qi), lhsT=PT[pi][:, i * 128:(i + 1) * 128], rhs=vr,
                                    start=False, stop=(kt == qt), skip_group_check=True),
                                    rd=(PTB[pi], VB[kt // 4]), wr=(regb(u, qi),), sig=(i == nv - 1))
                            if last:
                                win_end(qw)

                        def win_end(qw):
                            while bg:
                                bg.pop(0)()
                            if dil:
                                for u in range(2):
                                    if u == 0:
                                        S.op("dve", lambda e, u=u: e.tensor_copy(out=accS[:, u * 260:(u + 1) * 260], in_=PS[3 + u][:, 0:260]),
                                             rd=(PSB[3 + u],), wr=(accSB,))
                                    else:
                                        S.op("act", lambda e, u=u: e.activation(out=accS[:, u * 260:(u + 1) * 260], in_=PS[3 + u][:, 0:260],
                                                                                func=AF.Copy), rd=(PSB[3 + u],), wr=(accSB,))
                                a3 = accS[:, 0:520].rearrange("p (r n) -> p r n", r=8)
                                rr = small[:, 0:8]
                                bg.append(lambda: S.op("dve", lambda e: e.reciprocal(out=rr.unsqueeze(2), in_=a3[:, :, 64:65]),
                                                       rd=(accSB,), wr=(smallB,)))
                                for u in range(2):
                                    bg.append(lambda u=u: S.op("dve", lambda e: e.tensor_tensor(
                                        out=Otok[:, :, u * 64:(u + 1) * 64], in0=a3[:, u * 4:(u + 1) * 4, 0:64],
                                        in1=rr[:, u * 4:(u + 1) * 4].unsqueeze(2).to_broadcast([128, 4, 64]), op=ALU.mult),
                                        rd=(accSB, smallB), wr=(OtokB,)))
                            else:
                                a3 = accS[:, 0:1032].rearrange("p (r n) -> p r n", r=8)
                                for b_ in range(3):
                                    nr = 3 if b_ < 2 else 2
                                    src = PS[3 + b_][:, 0:nr * 129].rearrange("p (r n) -> p r n", r=nr)
                                    dst = a3[:, 3 * b_:3 * b_ + nr, :]
                                    if b_ == 1:
                                        S.op("act", lambda e, src=src, dst=dst: e.activation(out=dst, in_=src, func=AF.Copy),
                                             rd=(PSB[3 + b_],), wr=(accSB,))
                                    else:
                                        S.op("dve", lambda e, src=src, dst=dst: e.tensor_copy(out=dst, in_=src),
                                             rd=(PSB[3 + b_],), wr=(accSB,))
                                rr = small[:, 0:8]
                                r2n = small[:, 8:12]
                                ss_ = small[:, 12:16]
                                o1 = a3[:, 0:4, 0:128]
                                o2 = a3[:, 4:8, 0:128]
                                A = lambda fn, extra=(): S.op("dve", fn, rd=(accSB, smallB) + extra, wr=(accSB, smallB))
                                bg.append(lambda: A(lambda e: e.reciprocal(out=rr.unsqueeze(2), in_=a3[:, :, 128:129])))
                                bg.append(lambda: A(lambda e: e.tensor_scalar(out=r2n, in0=rr[:, 4:8], scalar1=neg_lam[:, l:l + 1], scalar2=None,
                                                                              op0=ALU.mult), (B_const,)))
                                bg.append(lambda: A(lambda e: e.tensor_tensor(out=o2, in0=o2, in1=r2n.unsqueeze(2).to_broadcast([128, 4, 128]), op=ALU.mult)))
                                bg.append(lambda: A(lambda e: e.tensor_tensor(out=o1, in0=o1, in1=rr[:, 0:4].unsqueeze(2).to_broadcast([128, 4, 128]), op=ALU.mult)))
                                bg.append(lambda: A(lambda e: e.tensor_tensor(out=o1, in0=o1, in1=o2, op=ALU.add)))
                                bg.append(lambda: A(lambda e: e.tensor_tensor(out=o2, in0=o1, in1=o1, op=ALU.mult)))
                                bg.append(lambda: A(lambda e: e.tensor_reduce(out=ss_, in_=o2, axis=AX.X, op=ALU.add)))

                                def rstd_pool():
                                    S.op("pool", lambda e: e.tensor_scalar(out=ss_, in0=ss_, scalar1=1.0 / 128, scalar2=LN_EPS, op0=ALU.mult, op1=ALU.add),
                                         rd=(smallB,), wr=(smallB,))
                                    S.op("pool", lambda e: e.tensor_tensor(out=ss_, in0=ss_, in1=neghalf[:, 0:4], op=ALU.pow),
                                         rd=(smallB, B_const), wr=(smallB,))
                                bg.append(rstd_pool)
                                bg.append(lambda: None)
                                bg.append(lambda: None)
                                bg.append(lambda: A(lambda e: e.tensor_tensor(out=o1, in0=o1, in1=ss_.unsqueeze(2).to_broadcast([128, 4, 128]), op=ALU.mult)))
                                bg.append(lambda: S.op("dve", lambda e: e.tensor_tensor(
                                    out=Otok, in0=o1, in1=gsub[:, l, :].unsqueeze(1).to_broadcast([128, 4, 128]), op=ALU.mult),
                                    rd=(accSB, B_const), wr=(OtokB,)))

                            def tr_out(g=g, qw=qw):
                                bk = 6 + (qw % 2)
                                for qi in range(4):
                                    S.op("pe", lambda e, qi=qi, bk=bk: e.matmul(
                                        PS[bk][:, qi * 128:(qi + 1) * 128], lhsT=Otok[:, qi, :], rhs=ident_b, start=True, stop=True),
                                        rd=(OtokB, B_const), wr=(PSB[bk],), sig=(qi == 3))
                                S.op("act", lambda e, bk=bk: e.activation(out=OT[:, g, qw * 512:(qw + 1) * 512], in_=PS[bk], func=AF.Copy),
                                     rd=(PSB[bk],), wr=(OTB[g][qw], xbB[0], xbB[1]))
                            bg.append(lambda: None)
                            bg.append(lambda: None)
                            bg.append(tr_out)

                        LOOK = 3
                        pis = []
                        for idx in range(len(items) + LOOK):
                            if idx < len(items):
                                pis.append(emit_st(items[idx]))
                                if bg:
                                    bg.pop(0)()
                            if idx - LOOK >= 0:
                                emit_pv(items[idx - LOOK], pis[idx - LOOK])
                    if g == 7:
                        while bg:
                            bg.pop(0)()
                    while pending_tr:
                        pending_tr.pop(0)()
                S.fence(Buf.REG[reg0:], fscr)
            with nc.sbuf_tensor(f"wo{li}", [128, 8, D], BF16) as Wo_h, \
                    nc.sbuf_tensor(f"wk4{li}", [128, 11264], F32) as wk4_h:
                Wo = Wo_h.ap()
                work = wk4_h.ap()
                WoB = Buf()
                for kc in range(8):
                    S.dma("pool", Wo[:, kc, :], w_o[l, kc * 128:(kc + 1) * 128, :], wr=(WoB,), chan=ch_w[kc % 6])
                g_b = work[:, 0:1024]
                lg_b = work[:, 1024:2048]
                lb_b = work[:, 2048:3072]
                tmpd = work[:, 3072:3200]
                bcB = Buf()
                bcast_rows(g_b, l, 16, tmpd, bcB)
                S.dma("sp", lg_b, ln1_g[l:l + 1, :].rearrange("o d -> (o d)").partition_broadcast(128), wr=(bcB,), chan=ch_bc)
                S.dma("sp", lb_b, ln1_b[l:l + 1, :].rearrange("o d -> (o d)").partition_broadcast(128), wr=(bcB,), chan=ch_bc)
                ys = [work[:, 4096 + i * 1024:4096 + (i + 1) * 1024] for i in range(3)]
                ysB = [Buf() for _ in range(3)]
                t1s = [work[:, 8192 + i * 1024:8192 + (i + 1) * 1024] for i in range(2)]
                t1B = [Buf() for _ in range(2)]
                st_ = [work[:, 10240 + i * 32:10240 + i * 32 + 12].rearrange("p (a b) -> p a b", a=2) for i in range(3)]
                mv_ = [work[:, 10400 + i * 8:10400 + i * 8 + 2] for i in range(3)]
                rs_ = [work[:, 10440 + i * 8:10440 + i * 8 + 1] for i in range(3)]
                def ld_x(t):
                    S.dma("sp", ys[t % 3], x_src[t * 128:(t + 1) * 128, :], rd=(B_xdst[t],), wr=(ysB[t % 3],), chan=ch_xl[t % 3])
                ld_x(0)
                ld_x(1)
                for t in range(NT):
                    s3 = t % 3
                    s2 = t % 2
                    if t + 2 < NT:
                        ld_x(t + 2)
                    for hh in range(2):
                        bk = (t % 2) * 2 + hh
                        for kc in range(8):
                            S.op("pe", lambda e, kc=kc, t=t, hh=hh, bk=bk: e.matmul(
                                PS[bk], lhsT=OT[:, kc, t * 128:(t + 1) * 128], rhs=Wo[:, kc, hh * 512:(hh + 1) * 512],
                                start=(kc == 0), stop=(kc == 7)), rd=(OTB[kc][t // 4], WoB), wr=(PSB[bk],), sig=(kc == 7))
                        S.op("dve", lambda e, hh=hh, bk=bk, s2=s2: e.tensor_tensor(
                            out=t1s[s2][:, hh * 512:(hh + 1) * 512], in0=PS[bk], in1=g_b[:, hh * 512:(hh + 1) * 512], op=ALU.mult),
                            rd=(PSB[bk], bcB), wr=(t1B[s2],))
                    S.op("dve", lambda e, s3=s3, s2=s2: e.scalar_tensor_tensor(
                        out=ys[s3], in0=ys[s3], scalar=ALPHA, in1=t1s[s2], op0=ALU.mult, op1=ALU.add),
                        rd=(ysB[s3], t1B[s2]), wr=(ysB[s3],))
                    ln_tail(ys[s3], st_[s3], mv_[s3], rs_[s3], lg_b, lb_b, ysB[s3], x_mid[t * 128:(t + 1) * 128, :], ch_xs[s3], eps_t,
                            B_xmid[t], bcB, use_pool=True)
                S.fence(Buf.REG[reg0:], fscr)
        reg0 = len(Buf.REG)
        if "B" in skip:
            pass
        else:
          with nc.sbuf_tensor(f"h2T{li}", [128, 2, 8, 1024], BF16) as h2_h, \
                nc.sbuf_tensor(f"acc{li}", [128, 2, 8, D], F32) as acc_h, \
                nc.sbuf_tensor(f"ew{li}", [128, 2, 3, 4096], BF16) as ew_h, \
                nc.sbuf_tensor(f"actb{li}", [128, 2, 4, 512], BF16) as act_h, \
                nc.sbuf_tensor(f"xb2{li}", [128, 2, 1024], BF16) as xb2_h, \
                nc.sbuf_tensor(f"bw{li}", [128, 10624], F32) as bw_h:
            h2Tall = h2_h.ap()
            accall = acc_h.ap()
            ew = ew_h.ap()
            actb = act_h.ap()
            bw = bw_h.ap()
            xb2 = xb2_h.ap()
            g_b = bw[:, 0:1024]
            lg_b = bw[:, 1024:2048]
            lb_b = bw[:, 2048:3072]
            tmpd = bw[:, 3072:3200]
            bcB = Buf()
            bcast_rows(g_b, l, 40, tmpd, bcB)
            S.dma("sp", lg_b, ln2_g[l:l + 1, :].rearrange("o d -> (o d)").partition_broadcast(128), wr=(bcB,), chan=ch_bc)
            S.dma("sp", lb_b, ln2_b[l:l + 1, :].rearrange("o d -> (o d)").partition_broadcast(128), wr=(bcB,), chan=ch_bc)
            xstage = [bw[:, 4096 + i * 1024:4096 + (i + 1) * 1024] for i in range(3)]
            xsB = [Buf() for _ in range(3)]
            sgs = [bw[:, 7168 + i * 512:7168 + (i + 1) * 512] for i in range(2)]
            sgB = [Buf(), Buf()]
            rt = bw[:, 8192:8192 + 1024]
            rtB = Buf()
            st_ = [bw[:, 9216 + i * 32:9216 + i * 32 + 12].rearrange("p (a b) -> p a b", a=2) for i in range(3)]
            mv_ = [bw[:, 9344 + i * 8:9344 + i * 8 + 2] for i in range(3)]
            rs_ = [bw[:, 9376 + i * 8:9376 + i * 8 + 1] for i in range(3)]
            gates2 = [bw[:, 9472 + i * 128:9472 + (i + 1) * 128].rearrange("p (s n) -> p s n", s=8) for i in range(2)]
            gatesB = [Buf(), Buf()]
            h2B = [[Buf() for _ in range(2)] for _ in range(2)]
            accB = [[Buf() for _ in range(8)] for _ in range(2)]
            ewB = [Buf(), Buf()]
            actB = [Buf(), Buf()]
            xb2B = [Buf(), Buf()]
            xctr = [0]

            def load_expert(e_, slot):
                S.dma("pool", ew[:, slot, 0, :].rearrange("p (c n) -> p c n", c=8),
                      w_gate[l, e_].rearrange("(c p) n -> p c n", p=128), wr=(ewB[slot],), chan=ch_w[slot * 3 + 0])
                S.dma("pool", ew[:, slot, 1, :].rearrange("p (c n) -> p c n", c=8),
                      w_up[l, e_].rearrange("(c p) n -> p c n", p=128), wr=(ewB[slot],), chan=ch_w[slot * 3 + 1])
                S.dma("pool", ew[:, slot, 2, :].rearrange("p (c n) -> p c n", c=4),
                      w_down[l, e_].rearrange("(c p) n -> p c n", p=128), wr=(ewB[slot],), chan=ch_w[slot * 3 + 2])

            def b1_sub(T, s):
                par = T % 2
                h2T = h2Tall[:, par]
                t = T * 8 + s
                s3 = xctr[0] % 3
                s2 = xctr[0] % 2
                xctr[0] += 1
                S.dma("sp", xstage[s3], x_mid[t * 128:(t + 1) * 128, :], rd=(B_xmid[t],), wr=(xsB[s3],), chan=ch_xl[s3])
                S.op("dve", lambda e: e.tensor_copy(out=xb2[:, s2, :], in_=xstage[s3]), rd=(xsB[s3],), wr=(xb2B[s2],))
                for c in range(8):
                    bk = 6 + (c // 4) % 2
                    S.op("pe", lambda e, c=c, bk=bk: e.matmul(
                        PS[bk][:, (c % 4) * 128:(c % 4 + 1) * 128], lhsT=xb2[:, s2, c * 128:(c + 1) * 128],
                        rhs=ident_b, start=True, stop=True), rd=(xb2B[s2], B_const), wr=(PSB[bk],), sig=(c % 4 == 3))
                    if c % 4 == 3:
                        c0 = c - 3
                        for cc in range(4):
                            S.op("act", lambda e, c0=c0, cc=cc, bk=bk: e.activation(
                                out=h2T[:, c0 + cc, s * 128:(s + 1) * 128], in_=PS[bk][:, cc * 128:(cc + 1) * 128],
                                func=AF.Identity, bias=modT[:, l, 24 + c0 + cc:24 + c0 + cc + 1],
                                scale=sc2p[:, l, c0 + cc:c0 + cc + 1]),
                                rd=(PSB[bk], B_const), wr=(h2B[par][s // 4],))

            def router(T):
                par = T % 2
                h2T = h2Tall[:, par]
                gates = gates2[par]
                RB = 7
                for s in range(8):
                    for kc in range(8):
                        S.op("pe", lambda e, s=s, kc=kc: e.matmul(
                            PS[RB][:, s * 16:(s + 1) * 16], lhsT=h2T[:, kc, s * 128:(s + 1) * 128], rhs=wr_b[:, kc, :],
                            start=(kc == 0), stop=(kc == 7)), rd=(h2B[par][s // 4], B_const), wr=(PSB[RB],), sig=(kc == 7 and s == 7))
                v3 = lambda a: a.rearrange("p (s n) -> p s n", s=8)
                sc = v3(rt[:, 0:128])
                sel = v3(rt[:, 128:256])
                sel2 = v3(rt[:, 256:384])
                eq = v3(rt[:, 384:512])
                eq2 = v3(rt[:, 512:640])
                m1 = rt[:, 640:672]
                m2 = rt[:, 672:704]
                gs = rt[:, 704:736]
                gm = rt[:, 736:744]
                pen = rt[:, 744:776]
                t1_ = rt[:, 776:784]
                t2_ = rt[:, 784:792]
                ws_ = rt[:, 792:800]
                g4 = lambda a: a.rearrange("p s (g k) -> p (s g) k", g=4)
                R = lambda fn, **kw: S.op("dve", fn, rd=(rtB,) + kw.get("rd", ()), wr=(rtB,) + kw.get("wr", ()))
                S.op("act", lambda e: e.activation(out=sc, in_=v3(PS[RB][:, 0:128]), func=AF.Sigmoid), rd=(PSB[RB],), wr=(rtB,))
                R(lambda e: e.tensor_tensor(out=sel, in0=sc, in1=br_b.unsqueeze(1).to_broadcast([128, 8, 16]), op=ALU.add), rd=(B_const,))
                R(lambda e: e.tensor_reduce(out=m1, in_=g4(sel), axis=AX.X, op=ALU.max))
                R(lambda e: e.tensor_tensor(out=g4(eq), in0=g4(sel), in1=m1.unsqueeze(2).to_broadcast([128, 32, 4]), op=ALU.is_equal))
                R(lambda e: e.scalar_tensor_tensor(out=sel2, in0=eq, scalar=-BIG, in1=sel, op0=ALU.mult, op1=ALU.add))
                R(lambda e: e.tensor_reduce(out=m2, in_=g4(sel2), axis=AX.X, op=ALU.max))
                R(lambda e: e.tensor_tensor(out=gs, in0=m1, in1=m2, op=ALU.add))
                R(lambda e: e.tensor_reduce(out=gm, in_=gs.rearrange("p (s g) -> p s g", g=4), axis=AX.X, op=ALU.max))
                R(lambda e: e.tensor_tensor(out=pen.rearrange("p (s g) -> p s g", g=4), in0=gs.rearrange("p (s g) -> p s g", g=4),
                                            in1=gm.unsqueeze(2).to_broadcast([128, 8, 4]), op=ALU.is_equal))
                R(lambda e: e.tensor_scalar(out=pen, in0=pen, scalar1=-1.0, scalar2=BIG, op0=ALU.add, op1=ALU.mult))
                R(lambda e: e.tensor_tensor(out=g4(sel2), in0=g4(sel), in1=pen.unsqueeze(2).to_broadcast([128, 32, 4]), op=ALU.add))
                R(lambda e: e.tensor_reduce(out=t1_, in_=sel2, axis=AX.X, op=ALU.max))
                R(lambda e: e.tensor_tensor(out=eq, in0=sel2, in1=t1_.unsqueeze(2).to_broadcast([128, 8, 16]), op=ALU.is_equal))
                R(lambda e: e.scalar_tensor_tensor(out=sel, in0=eq, scalar=-BIG, in1=sel2, op0=ALU.mult, op1=ALU.add))
                R(lambda e: e.tensor_reduce(out=t2_, in_=sel, axis=AX.X, op=ALU.max))
                R(lambda e: e.tensor_tensor(out=eq2, in0=sel, in1=t2_.unsqueeze(2).to_broadcast([128, 8, 16]), op=ALU.is_equal))
                R(lambda e: e.tensor_tensor(out=eq, in0=eq, in1=eq2, op=ALU.add))
                R(lambda e: e.tensor_tensor(out=eq, in0=eq, in1=sc, op=ALU.mult))
                R(lambda e: e.tensor_reduce(out=ws_, in_=eq, axis=AX.X, op=ALU.add))
                R(lambda e: e.reciprocal(out=ws_, in_=ws_))
                S.op("dve", lambda e: e.tensor_tensor(out=gates, in0=eq, in1=ws_.unsqueeze(2).to_broadcast([128, 8, 16]), op=ALU.mult),
                     rd=(rtB,), wr=(gatesB[par],))

            def b4_load(T, s):
                t = T * 8 + s
                s3 = xctr[0] % 3
                xctr[0] += 1
                S.dma("sp", xstage[s3], x_mid[t * 128:(t + 1) * 128, :], rd=(B_xmid[t],), wr=(xsB[s3],), chan=ch_xl[s3])
                return s3

            def b4_sub(T, s, s3=None):
                par = T % 2
                acc = accall[:, par]
                t = T * 8 + s
                if s3 is None:
                    s3 = b4_load(T, s)
                S.op("dve", lambda e: e.tensor_tensor(out=acc[:, s, :], in0=acc[:, s, :], in1=g_b, op=ALU.mult),
                     rd=(accB[par][s], bcB), wr=(accB[par][s],))
                S.op("dve", lambda e: e.scalar_tensor_tensor(
                    out=xstage[s3], in0=xstage[s3], scalar=ALPHA, in1=acc[:, s, :], op0=ALU.mult, op1=ALU.add),
                    rd=(xsB[s3], accB[par][s]), wr=(xsB[s3],))
                ln_tail(xstage[s3], st_[s3], mv_[s3], rs_[s3], lg_b, lb_b, xsB[s3], x_dst[t * 128:(t + 1) * 128, :], ch_xs[s3], eps_t,
                        B_xdst[t], bcB, act_norm=False)

            def gu(T, e_, w_, slot):
                par = T % 2
                h2T = h2Tall[:, par]
                wg = ew[:, slot, 0, :].rearrange("p (c n) -> p c n", c=8)
                wu = ew[:, slot, 1, :].rearrange("p (c n) -> p c n", c=8)
                tsl = slice(w_ * 512, (w_ + 1) * 512)
                for fc in range(4):
                    pb = (fc % 2) * 2
                    for kc in range(8):
                        S.op("pe", lambda e, kc=kc, fc=fc, pb=pb: e.matmul(
                            PS[pb], lhsT=wg[:, kc, fc * 128:(fc + 1) * 128], rhs=h2T[:, kc, tsl],
                            start=(kc == 0), stop=(kc == 7)), rd=(ewB[slot], h2B[par][w_]), wr=(PSB[pb],), sig=(kc == 7))
                    for kc in range(8):
                        S.op("pe", lambda e, kc=kc, fc=fc, pb=pb: e.matmul(
                            PS[pb + 1], lhsT=wu[:, kc, fc * 128:(fc + 1) * 128], rhs=h2T[:, kc, tsl],
                            start=(kc == 0), stop=(kc == 7)), rd=(ewB[slot], h2B[par][w_]), wr=(PSB[pb + 1],), sig=(kc == 7))
                    si = fc % 2
                    S.op("act", lambda e, si=si, pb=pb: e.activation(out=sgs[si], in_=PS[pb], func=AF.Silu),
                         rd=(PSB[pb],), wr=(sgB[si],))
                    S.op("dve", lambda e, si=si, pb=pb, fc=fc: e.tensor_tensor(
                        out=actb[:, w_, fc, :], in0=sgs[si], in1=PS[pb + 1], op=ALU.mult),
                        rd=(sgB[si], PSB[pb + 1]), wr=(actB[w_],))

            def down(T, e_, w_, slot):
                par = T % 2
                acc = accall[:, par]
                gates = gates2[par]
                wd = ew[:, slot, 2, :].rearrange("p (c n) -> p c n", c=4)
                for ss in range(4):
                    s = w_ * 4 + ss
                    for hh in range(2):
                        yb = 4 + ((ss * 2 + hh) % 2)
                        for fc in range(4):
                            S.op("pe", lambda e, fc=fc, ss=ss, hh=hh, yb=yb: e.matmul(
                                PS[yb], lhsT=actb[:, w_, fc, ss * 128:(ss + 1) * 128], rhs=wd[:, fc, hh * 512:(hh + 1) * 512],
                                start=(fc == 0), stop=(fc == 3)), rd=(actB[w_], ewB[slot]), wr=(PSB[yb],), sig=(fc == 3))
                        if e_ == 0:
                            S.op("dve", lambda e, s=s, hh=hh, yb=yb: e.tensor_scalar(
                                out=acc[:, s, hh * 512:(hh + 1) * 512], in0=PS[yb], scalar1=gates[:, s, e_:e_ + 1],
                                scalar2=None, op0=ALU.mult), rd=(PSB[yb], gatesB[par]), wr=(accB[par][s],))
                        else:
                            S.op("dve", lambda e, s=s, hh=hh, yb=yb: e.scalar_tensor_tensor(
                                out=acc[:, s, hh * 512:(hh + 1) * 512], in0=PS[yb], scalar=gates[:, s, e_:e_ + 1],
                                in1=acc[:, s, hh * 512:(hh + 1) * 512], op0=ALU.mult, op1=ALU.add),
                                rd=(PSB[yb], gatesB[par], accB[par][s]), wr=(accB[par][s],))

            load_expert(0, 0)
            for s in range(8):
                b1_sub(0, s)
            router(0)
            eidx = 0
            pendD = None
            NTT = 4
            for T in range(NTT):
                extras = []
                if T > 0:
                    extras += [(lambda T=T, s=s: b4_sub(T - 1, s)) for s in range(8)]
                extras_late = []
                if T + 1 < NTT:
                    extras_late += [(lambda T=T, s=s: b1_sub(T + 1, s)) for s in range(8)]
                    extras_late.append(lambda T=T: router(T + 1))
                step = 0
                for e_ in range(NE):
                    slot = eidx % 2
                    eidx += 1
                    nxt = (T, e_ + 1) if e_ + 1 < NE else ((T + 1, 0) if T + 1 < NTT else None)
                    for w_ in range(2):
                        gu(T, e_, w_, slot)
                        if pendD is not None:
                            pendD()
                            pendD = None
                        if w_ == 0 and nxt is not None:
                            load_expert(nxt[1], 1 - slot)
                        pendD = (lambda T=T, e_=e_, w_=w_, slot=slot: down(T, e_, w_, slot))
                        if extras:
                            extras.pop(0)()
                        elif step >= 14 and extras_late:
                            extras_late.pop(0)()
                        step += 1
                pendD()
                pendD = None
                while extras:
                    extras.pop(0)()
                while extras_late:
                    extras_late.pop(0)()
            slots = [b4_load(NTT - 1, 0), b4_load(NTT - 1, 1)]
            for s in range(8):
                if s + 2 < 8:
                    slots.append(b4_load(NTT - 1, s + 2))
                b4_sub(NTT - 1, s, slots[s])
            S.fence(Buf.REG[reg0:], fscr)
    for ch in ch_xs:
        if ch.last is not None:
            S.wait_event("sp", ch.last)
    return nc, S


_CACHE = {}


def kernel(**inputs):
    if "nc" not in _CACHE:
        _CACHE["nc"] = build()[0]
    nc = _CACHE["nc"]
    consts = _host_consts()
    shared = {}
    for k, v in inputs.items():
        if k in ("x", "c"):
            continue
        a = np.ascontiguousarray(np.asarray(v, dtype=np.float32))
        if k == "b_router":
            a = a.reshape(1, NE)
        shared[k] = a
    shared.update(consts)
    x = np.asarray(inputs["x"], dtype=np.float32)
    c = np.asarray(inputs["c"], dtype=np.float32)
    in_maps = []
    for b in range(8):
        m = dict(shared)
        m["x"] = np.ascontiguousarray(x[b])
        m["c"] = np.ascontiguousarray(c[b:b + 1])
        in_maps.append(m)
    res = run_bass_kernel_spmd(nc, in_maps, core_ids=list(range(8)))
    return np.stack([np.asarray(r["out"]) for r in res.results], axis=0).astype(np.float32)
```

```python
import math
import numpy as np
import concourse.bass as bass
import concourse.mybir as mybir
from concourse.bass_utils import run_bass_kernel_spmd

F32 = mybir.dt.float32
BF16 = mybir.dt.bfloat16
AF = mybir.ActivationFunctionType
ALU = mybir.AluOpType
AX = mybir.AxisListType

D = 1024
SEQ = 4096
DEPTH = 4
NT = SEQ // 128
NW = SEQ // 512
NE = 16
DFF = 512
ALPHA = (2.0 * DEPTH) ** 0.25
LN_EPS = 1e-5
W_STRIP = 2176
WF = 2304
NEG = -30000.0
BIG = 100.0
SEM_LIMIT = 30000


def _t5_bucket_np(d):
    n = np.maximum(d, 0)
    nf = np.maximum(n, 16).astype(np.float32)
    large = 16 + (np.log(nf / np.float32(16)) / np.float32(math.log(2048 / 16)) * np.float32(16)).astype(np.int32)
    large = np.minimum(large, 31)
    return np.where(n < 16, n, large)


def _host_consts():
    m = np.arange(WF)
    d = m - 127
    bucket = _t5_bucket_np(d)
    valid_c = d >= 0
    mult = ((d <= 128).astype(np.int32) + ((d % 4 == 0) & (d <= 512)).astype(np.int32)
            + ((d % 16 == 0) & (d <= 2048)).astype(np.int32))
    mult = np.where(valid_c, mult, 0)
    oh = np.zeros((2, 32, WF), np.float32)
    add = np.zeros((12, WF), np.float32)
    for b in range(32):
        oh[0, b] = ((bucket == b) & (mult > 0)).astype(np.float32)
        oh[1, b] = ((bucket == b) & valid_c).astype(np.float32)
    add_dil = np.where(mult > 0, np.log(np.maximum(mult, 1)).astype(np.float32), np.float32(NEG))
    add_dif = np.where(valid_c, np.float32(0), np.float32(NEG))
    add[:8] = add_dil[None]
    add[8:] = add_dif[None]
    ident = np.eye(128, dtype=np.float32)
    flip = ident[::-1].copy()
    return {"c_oh": oh, "c_add": add, "c_ident": ident, "c_flip": flip}


class Buf:
    __slots__ = ("w", "rd")
    EPOCH = None
    REG = []

    def __init__(self):
        self.w = Buf.EPOCH
        self.rd = {}
        Buf.REG.append(self)


class _Eng:
    def __init__(self, S, name, h, compute):
        self.S, self.name, self.h, self.compute = S, name, h, compute
        self.sem = None
        self.count = 0
        self.own = set()
        self.waited = {}
        self.nsem = 0
        self.new_sem()

    def new_sem(self):
        self.sem = self.S.nc.alloc_semaphore(f"s_{self.name}_{self.nsem}")
        self.nsem += 1
        self.count = 0
        self.own.add(id(self.sem))


class Chan:
    def __init__(self, S, name):
        self.S, self.name = S, name
        self.n = 0
        self.sem = S.nc.alloc_semaphore(f"c_{name}_0")
        self.count = 0
        self.last = None


class Sched:
    def __init__(self, nc):
        self.nc = nc
        self.e = {
            "pe": _Eng(self, "pe", nc.tensor, True),
            "act": _Eng(self, "act", nc.scalar, True),
            "dve": _Eng(self, "dve", nc.vector, True),
            "pool": _Eng(self, "pool", nc.gpsimd, True),
            "sp": _Eng(self, "sp", nc.sync, False),
        }
        self.sems = {}
        self.ninst = 0
        self.log = {k: [] for k in self.e}

    def _wait(self, X, evs):
        best = {}
        for sem, v in evs:
            k = id(sem)
            self.sems[k] = sem
            if v > best.get(k, 0):
                best[k] = v
        for k, v in best.items():
            if X.waited.get(k, 0) < v:
                X.h.wait_ge(self.sems[k], v)
                X.waited[k] = v
                self.log[X.name].append(("w", k, v, self.ninst))

    def _deps(self, X, rd, wr, is_dma):
        evs = []
        for b in rd:
            if b.w is not None:
                sem, v = b.w
                if is_dma or not (X.name == "pe" and id(sem) in X.own):
                    evs.append(b.w)
        for b in wr:
            if b.w is not None:
                sem, v = b.w
                if is_dma or id(sem) not in X.own or (b in rd and X.name != "pe"):
                    evs.append(b.w)
            for k, v in b.rd.items():
                if is_dma or k not in X.own:
                    evs.append((self.sems[k], v))
        return evs

    def _commit(self, ev, rd, wr):
        sem, v = ev
        k = id(sem)
        self.sems[k] = sem
        for b in rd:
            if b.rd.get(k, 0) < v:
                b.rd[k] = v
        for b in wr:
            b.w = ev
            b.rd = {}

    def op(self, eng, fn, rd=(), wr=(), sig=True):
        X = self.e[eng]
        self._wait(X, self._deps(X, rd, wr, False))
        ins = fn(X.h)
        self.ninst += 1
        if sig:
            ins.then_inc(X.sem, 1)
            X.count += 1
            ev = (X.sem, X.count)
            self.log[X.name].append(("i", id(X.sem), 1, self.ninst))
        else:
            ev = (X.sem, X.count + 1)
        self._commit(ev, rd, wr)
        self.last_ev = ev
        if sig and X.count >= SEM_LIMIT:
            X.new_sem()
        return ins

    def dma(self, q, out, in_, rd=(), wr=(), chan=None, **kw):
        X = self.e[q]
        evs = self._deps(X, rd, wr, True)
        if chan.last is not None:
            evs.append(chan.last)
        self._wait(X, evs)
        if chan.count >= SEM_LIMIT:
            chan.n += 1
            chan.sem = self.nc.alloc_semaphore(f"c_{chan.name}_{chan.n}")
            chan.count = 0
        ins = X.h.dma_start(out=out, in_=in_, **kw)
        ins.then_inc(chan.sem, 16)
        chan.count += 16
        self.log[X.name].append(("i", id(chan.sem), 16, self.ninst))
        ev = (chan.sem, chan.count)
        chan.last = ev
        self._commit(ev, rd, wr)
        self.ninst += 1
        return ev

    def fence(self, bufs, scratch):
        bufs = [b for b in bufs]
        self.op("dve", lambda e: e.memset(scratch, 0.0), rd=tuple(bufs), wr=tuple(bufs))
        Buf.EPOCH = self.last_ev

    def check_deadlock(self):
        val = {}
        pos = {k: 0 for k in self.log}
        progress = True
        while progress:
            progress = False
            for k, lg in self.log.items():
                while pos[k] < len(lg):
                    t, sem, v, n = lg[pos[k]]
                    if t == "w":
                        if val.get(sem, 0) >= v:
                            pos[k] += 1
                            progress = True
                        else:
                            break
                    else:
                        val[sem] = val.get(sem, 0) + v
                        pos[k] += 1
                        progress = True
        stuck = {k: (pos[k], len(lg), lg[pos[k]] if pos[k] < len(lg) else None) for k, lg in self.log.items()}
        ok = all(pos[k] == len(lg) for k, lg in self.log.items())
        return ok, stuck, val

    def wait_event(self, eng, ev):
        self._wait(self.e[eng], [ev])


def build(layers=(0, 1, 2, 3), dbg=None, skip=""):
    nc = bass.Bass("TRN2", target_bir_lowering=False)
    S = Sched(nc)
    Buf.EPOCH = None
    Buf.REG = []
    dt_in = lambda name, shape: nc.dram_tensor(name, list(shape), F32, kind="ExternalInput").ap()
    x_in = dt_in("x", (SEQ, D))
    c_in = dt_in("c", (1, D))
    rel_bias = dt_in("rel_bias", (32, 12))
    w_in = dt_in("w_in", (DEPTH, D, 3072))
    w_o = dt_in("w_o", (DEPTH, D, D))
    lam_q1 = dt_in("lam_q1", (DEPTH, 64))
    lam_k1 = dt_in("lam_k1", (DEPTH, 64))
    lam_q2 = dt_in("lam_q2", (DEPTH, 64))
    lam_k2 = dt_in("lam_k2", (DEPTH, 64))
    subln_g = dt_in("subln_g", (DEPTH, 128))
    w_ada = dt_in("w_ada", (DEPTH, D, 6 * D))
    b_ada = dt_in("b_ada", (DEPTH, 6 * D))
    ln1_g = dt_in("ln1_g", (DEPTH, D))
    ln1_b = dt_in("ln1_b", (DEPTH, D))
    ln2_g = dt_in("ln2_g", (DEPTH, D))
    ln2_b = dt_in("ln2_b", (DEPTH, D))
    w_router = dt_in("w_router", (D, NE))
    b_router = dt_in("b_router", (1, NE))
    w_gate = dt_in("w_gate", (DEPTH, NE, D, DFF))
    w_up = dt_in("w_up", (DEPTH, NE, D, DFF))
    w_down = dt_in("w_down", (DEPTH, NE, DFF, D))
    c_oh = dt_in("c_oh", (2, 32, WF))
    c_add = dt_in("c_add", (12, WF))
    c_ident = dt_in("c_ident", (128, 128))
    c_flip = dt_in("c_flip", (128, 128))
    out = nc.dram_tensor("out", [SEQ, D], F32, kind="ExternalOutput").ap()
    xs_a = nc.dram_tensor("xs_a", [SEQ, D], F32, kind=("ExternalOutput" if dbg else "Internal")).ap()
    xs_b = nc.dram_tensor("xs_b", [SEQ, D], F32, kind="Internal").ap()
    f_dram = nc.dram_tensor("f_dram", [12, WF], F32, kind="Internal")

    sb = lambda name, shape, dt: nc.alloc_sbuf_tensor(name, list(shape), dt).ap()
    PS = [nc.alloc_psum_tensor(f"ps{i}", [128, 512], F32).ap() for i in range(8)]
    PSB = [Buf() for _ in range(8)]

    ident_f = sb("ident_f", (128, 128), F32)
    ident_b = sb("ident_b", (128, 128), BF16)
    flip_b = sb("flip_b", (128, 128), BF16)
    ones_f = sb("ones_f", (128, 128), F32)
    zeros_b = sb("zeros_b", (128, 512), BF16)
    modT = sb("modT", (128, DEPTH, 48), F32)
    sc1p = sb("sc1p", (128, DEPTH, 8), F32)
    sc2p = sb("sc2p", (128, DEPTH, 8), F32)
    neg_lam = sb("neg_lam", (128, DEPTH), F32)
    gsub = sb("gsub", (128, DEPTH, 128), F32)
    wr_b = sb("wr_b", (128, 8, NE), BF16)
    br_b = sb("br_b", (128, NE), F32)
    fscr = sb("fscr", (128, 1), F32)
    B_const = Buf()

    ch_const = Chan(S, "const")
    ch_constp = Chan(S, "constp")

    def cdma(out_ap, in_ap, q="sp", **kw):
        return S.dma(q, out_ap, in_ap, rd=(), wr=(B_const,), chan=(ch_const if q == "sp" else ch_constp), **kw)

    with nc.sbuf_tensor("pro_region", [128, 128 * 1024 // 4], F32) as pro_h, \
            nc.sbuf_tensor("cTb", [128, 8], BF16) as cTb_h, \
            nc.sbuf_tensor("wblk", [128, 4, 8, 512], BF16) as wblk_h:
        pro = pro_h.ap()
        off = [0]

        def carve(n_f32):
            a = pro[:, off[0]:off[0] + n_f32]
            off[0] += n_f32
            return a

        cdma(ident_f, c_ident)
        cdma(ident_b, c_ident, q="pool")
        cdma(flip_b, c_flip, q="pool")
        S.op("dve", lambda e: e.memset(ones_f, 1.0), wr=(B_const,))
        S.op("dve", lambda e: e.memset(zeros_b, 0.0), wr=(B_const,))
        cdma(wr_b, w_router.rearrange("(c p) n -> p c n", p=128), q="pool")
        cdma(br_b, b_router.partition_broadcast(128))
        c_rows = carve(128)[0:8, :]
        cdma(c_rows, c_in.rearrange("o (j p) -> (o j) p", p=128))
        ca_rows = carve(128)[0:8, :]
        S.op("act", lambda e: e.activation(out=ca_rows, in_=c_rows, func=AF.Silu), rd=(B_const,), wr=(B_const,))
        cT = carve(8)
        S.op("pe", lambda e: e.matmul(PS[0][:, 0:8], lhsT=ca_rows, rhs=ident_f[0:8, 0:8], start=True, stop=True),
             rd=(B_const,), wr=(PSB[0],))
        S.op("dve", lambda e: e.tensor_copy(out=cT, in_=PS[0][:, 0:8]), rd=(PSB[0],), wr=(B_const,))
        cTb_t = cTb_h.ap()
        S.op("dve", lambda e: e.tensor_copy(out=cTb_t, in_=cT), rd=(B_const,), wr=(B_const,))
        brow = carve(6 * D)[0:1, :]
        mrow = carve(6 * D)[0:1, :]
        rowB = Buf()
        wblk_t = wblk_h.ap()
        wblkB = [Buf() for _ in range(4)]
        wch = [Chan(S, f"wb{i}") for i in range(4)]
        it = 0
        for l in range(DEPTH):
            cdma(brow, b_ada[l:l + 1, :])
            for cb in range(12):
                s_ = it % 4
                it += 1
                S.dma("pool", wblk_t[:, s_], w_ada[l, :, cb * 512:(cb + 1) * 512].rearrange("(c p) n -> p c n", p=128),
                      wr=(wblkB[s_],), chan=wch[s_])
                bk = 3 + (cb % 2)
                for kc in range(8):
                    S.op("pe", lambda e, s_=s_, kc=kc, bk=bk: e.matmul(
                        PS[bk][0:1, :], lhsT=cTb_t[:, kc:kc + 1], rhs=wblk_t[:, s_, kc, :], start=(kc == 0), stop=(kc == 7)),
                        rd=(wblkB[s_], B_const), wr=(PSB[bk],), sig=(kc == 7))
                S.op("dve", lambda e, cb=cb, bk=bk: e.tensor_tensor(
                    out=mrow[:, cb * 512:(cb + 1) * 512], in0=PS[bk][0:1, :], in1=brow[:, cb * 512:(cb + 1) * 512], op=ALU.add),
                    rd=(PSB[bk], B_const), wr=(rowB,))
            for j in range(48):
                S.op("pe", lambda e, j=j, l=l: e.matmul(
                    PS[2][:, l * 48 + j:l * 48 + j + 1], lhsT=mrow[:, j * 128:(j + 1) * 128], rhs=ones_f[0:1, 0:1],
                    start=True, stop=True), rd=(rowB, B_const), wr=(PSB[2],), sig=(j == 47))
        modT_flat = modT.rearrange("p l j -> p (l j)")
        S.op("dve", lambda e: e.tensor_copy(out=modT_flat, in_=PS[2][:, 0:192]), rd=(PSB[2],), wr=(B_const,))
        S.op("dve", lambda e: e.tensor_scalar(out=sc1p, in0=modT[:, :, 8:16], scalar1=1.0, scalar2=None, op0=ALU.add),
             rd=(B_const,), wr=(B_const,))
        S.op("dve", lambda e: e.tensor_scalar(out=sc2p, in0=modT[:, :, 32:40], scalar1=1.0, scalar2=None, op0=ALU.add),
             rd=(B_const,), wr=(B_const,))
        lam_t = [carve(DEPTH * 64) for _ in range(4)]
        for t_, src in zip(lam_t, (lam_q1, lam_k1, lam_q2, lam_k2)):
            cdma(t_, src.rearrange("l e -> (l e)").partition_broadcast(128))
        prod = carve(DEPTH * 64)
        ssum = [carve(DEPTH), carve(DEPTH)]
        for i in range(2):
            S.op("dve", lambda e, i=i: e.tensor_tensor(out=prod, in0=lam_t[2 * i], in1=lam_t[2 * i + 1], op=ALU.mult),
                 rd=(B_const,), wr=(B_const,))
            S.op("dve", lambda e, i=i: e.tensor_reduce(out=ssum[i], in_=prod.rearrange("p (l e) -> p l e", l=DEPTH),
                                                    axis=AX.X, op=ALU.add), rd=(B_const,), wr=(B_const,))
            S.op("act", lambda e, i=i: e.activation(out=ssum[i], in_=ssum[i], func=AF.Exp), rd=(B_const,), wr=(B_const,))
        S.op("dve", lambda e: e.tensor_tensor(out=neg_lam, in0=ssum[1], in1=ssum[0], op=ALU.subtract),
             rd=(B_const,), wr=(B_const,))
        cdma(gsub, subln_g.rearrange("l e -> (l e)").partition_broadcast(128))
        for l in range(DEPTH):
            lam_init = 0.8 - 0.6 * math.exp(-0.3 * l)
            S.op("dve", lambda e, l=l, li=lam_init: e.tensor_scalar(out=neg_lam[:, l:l + 1], in0=neg_lam[:, l:l + 1],
                                                                  scalar1=-li, scalar2=None, op0=ALU.add),
                 rd=(B_const,), wr=(B_const,))
            S.op("dve", lambda e, l=l, li=lam_init: e.tensor_scalar(out=gsub[:, l, :], in0=gsub[:, l, :],
                                                                  scalar1=1.0 - li, scalar2=None, op0=ALU.mult),
                 rd=(B_const,), wr=(B_const,))
        rb = carve(12)[0:32, :]
        cdma(rb, rel_bias)
        oh_sb = carve(2 * WF).rearrange("p (k m) -> p k m", k=2)[0:32]
        cdma(oh_sb, c_oh.rearrange("k b m -> b k m"))
        add_sb = carve(WF)[0:12, :]
        cdma(add_sb, c_add)
        f_sb = carve(WF)[0:12, :]
        fd_sb = carve(WF)[0:12, :]
        for kind, dst in ((0, f_sb), (1, fd_sb)):
            for cc in range(0, WF, 512):
                n = min(512, WF - cc)
                bk = 3 + (cc // 512) % 2
                S.op("pe", lambda e, kind=kind, cc=cc, n=n, bk=bk: e.matmul(
                    PS[bk][0:12, 0:n], lhsT=rb, rhs=oh_sb[:, kind, cc:cc + n], start=True, stop=True),
                    rd=(B_const,), wr=(PSB[bk],))
                S.op("dve", lambda e, dst=dst, cc=cc, n=n, bk=bk: e.tensor_tensor(
                    out=dst[:, cc:cc + n], in0=PS[bk][0:12, 0:n], in1=add_sb[:, cc:cc + n], op=ALU.add),
                    rd=(PSB[bk], B_const), wr=(B_const,))
        S.op("act", lambda e: e.activation(out=f_sb, in_=f_sb, func=AF.Exp), rd=(B_const,), wr=(B_const,))
        S.op("act", lambda e: e.activation(out=fd_sb, in_=fd_sb, func=AF.Exp), rd=(B_const,), wr=(B_const,))
        B_f = Buf()
        ch_f = Chan(S, "fdram")
        fd_ap = f_dram.ap()
        S.dma("sp", fd_ap[0:8, :], f_sb[0:8, :], rd=(B_const,), wr=(B_f,), chan=ch_f)
        S.dma("sp", fd_ap[8:12, :], fd_sb[8:12, :], rd=(B_const,), wr=(B_f,), chan=ch_f)
        S.fence(list(Buf.REG), fscr)

    ch_xl = [Chan(S, f"xl{i}") for i in range(4)]
    ch_xs = [Chan(S, f"xs{i}") for i in range(4)]
    ch_w = [Chan(S, f"w{i}") for i in range(8)]
    ch_bc = Chan(S, "bc")

    def bcast_rows(dst, l, col0, tmp, tmpB):
        for j in range(8):
            bk = 6 + (j % 2)
            S.op("dve", lambda e, j=j: e.tensor_scalar(out=tmp, in0=ident_f, scalar1=modT[:, l, col0 + j:col0 + j + 1],
                                                     scalar2=None, op0=ALU.mult), rd=(B_const,), wr=(tmpB,))
            S.op("pe", lambda e, bk=bk: e.matmul(PS[bk][:, 0:128], lhsT=ones_f, rhs=tmp, start=True, stop=True),
                 rd=(tmpB, B_const), wr=(PSB[bk],))
            S.op("dve", lambda e, j=j, bk=bk: e.tensor_copy(out=dst[:, j * 128:(j + 1) * 128], in_=PS[bk][:, 0:128]),
                 rd=(PSB[bk],), wr=(tmpB,))

    def ln_tail(y, stats, mv, rstd, g_b, b_b, yB, dst_dram, ch, eps_t, dstB, lnB, use_pool=False, act_norm=True):
        for hh in range(2):
            S.op("dve", lambda e, hh=hh: e.bn_stats(out=stats[:, hh, :], in_=y[:, hh * 512:(hh + 1) * 512]),
                 rd=(yB,), wr=(yB,))
        S.op("dve", lambda e: e.bn_aggr(out=mv, in_=stats.rearrange("p a b -> p (a b)")), rd=(yB,), wr=(yB,))
        S.op("act", lambda e: e.activation(out=rstd, in_=mv[:, 1:2], func=AF.Ln, bias=eps_t, scale=1.0), rd=(yB, B_const), wr=(yB,))
        S.op("act", lambda e: e.activation(out=rstd, in_=rstd, func=AF.Exp, scale=-0.5), rd=(yB,), wr=(yB,))
        if act_norm:
            S.op("dve", lambda e: e.tensor_scalar(out=mv[:, 1:2], in0=mv[:, 0:1], scalar1=rstd, scalar2=-1.0, op0=ALU.mult, op1=ALU.mult),
                 rd=(yB,), wr=(yB,))
            S.op("act", lambda e: e.activation(out=y, in_=y, func=AF.Identity, bias=mv[:, 1:2], scale=rstd), rd=(yB,), wr=(yB,))
        else:
            S.op("dve", lambda e: e.tensor_scalar(out=y, in0=y, scalar1=mv[:, 0:1], scalar2=rstd, op0=ALU.subtract, op1=ALU.mult),
                 rd=(yB,), wr=(yB,))
        eng = "pool" if use_pool else "dve"
        S.op(eng, lambda e: e.tensor_tensor(out=y, in0=y, in1=g_b, op=ALU.mult), rd=(yB, lnB), wr=(yB,))
        S.op(eng, lambda e: e.tensor_tensor(out=y, in0=y, in1=b_b, op=ALU.add), rd=(yB, lnB), wr=(yB,))
        S.dma("sp", dst_dram, y, rd=(yB,), wr=(dstB,), chan=ch)

    eps_t = sb("eps_t", (128, 1), F32)
    S.op("dve", lambda e: e.memset(eps_t, LN_EPS), wr=(B_const,))
    neghalf = sb("neghalf", (128, 8), F32)
    S.op("dve", lambda e: e.memset(neghalf, -0.5), wr=(B_const,))

    def stream_bufs(li, n_layers):
        src = x_in if li == 0 else xs_b
        dst = out if li == n_layers - 1 else xs_b
        return src, xs_a, dst

    B_xmid = [Buf() for _ in range(NT)]
    B_xdst = [Buf() for _ in range(NT)]

    for li, l in enumerate(layers):
        x_src, x_mid, x_dst = stream_bufs(li, len(layers))
        reg0 = len(Buf.REG)
        if "A" in skip:
            pass
        else:
          with nc.sbuf_tensor(f"hT{li}", [128, 8, SEQ], BF16) as hT_h, \
                nc.sbuf_tensor(f"rOT{li}", [128, 8, SEQ], BF16) as OT_h:
            hT = hT_h.ap()
            OT = OT_h.ap()
            hTB = [[Buf() for _ in range(NW)] for _ in range(8)]
            OTB = [[Buf() for _ in range(NW)] for _ in range(8)]
            with nc.sbuf_tensor(f"xst{li}", [128, 2, 4, D], F32) as xst_h:
                otf = OT.rearrange("p c t -> p (c t)")
                xbf = [otf[:, i * 4096:(i + 1) * 4096].rearrange("p (s d) -> p s d", s=4) for i in range(2)]
                xstage = [xst_h.ap()[:, i] for i in range(2)]
                xsB = [Buf(), Buf()]
                xbB = [Buf(), Buf()]
                for tw in range(NW):
                    s = tw % 2
                    S.dma("sp", xstage[s], x_src[tw * 512:(tw + 1) * 512, :].rearrange("(s p) d -> p s d", p=128),
                          rd=tuple(B_xdst[tw * 4:tw * 4 + 4]), wr=(xsB[s],), chan=ch_xl[s])
                    for ss in range(4):
                        S.op("dve", lambda e, s=s, ss=ss: e.tensor_copy(out=xbf[s][:, ss, :], in_=xstage[s][:, ss, :]),
                             rd=(xsB[s],), wr=(xbB[s],))
                    for c in range(8):
                        bk = 6 + (c % 2)
                        for ss in range(4):
                            S.op("pe", lambda e, s=s, ss=ss, c=c, bk=bk: e.matmul(
                                PS[bk][:, ss * 128:(ss + 1) * 128], lhsT=xbf[s][:, ss, c * 128:(c + 1) * 128],
                                rhs=ident_b, start=True, stop=True), rd=(xbB[s], B_const), wr=(PSB[bk],), sig=(ss == 3))
                        S.op("act", lambda e, c=c, tw=tw, bk=bk: e.activation(
                            out=hT[:, c, tw * 512:(tw + 1) * 512], in_=PS[bk], func=AF.Identity,
                            bias=modT[:, l, c:c + 1], scale=sc1p[:, l, c:c + 1]),
                            rd=(PSB[bk], B_const), wr=(hTB[c][tw],))
                S.fence(Buf.REG[reg0:], fscr)
            with nc.sbuf_tensor(f"grp{li}", [128, 4096 * 3 + 32 * 130 + 4 * W_STRIP + 2 * 8 * 384 + 6 * 512 + 4 * 128], BF16) as G_h, \
                    nc.sbuf_tensor(f"accS{li}", [128, 1040], F32) as accS_h, \
                    nc.sbuf_tensor(f"sml{li}", [128, 64], F32) as sml_h:
                G = G_h.ap()
                go = [0]

                def gcarve(n):
                    a = G[:, go[0]:go[0] + n]
                    go[0] += n
                    return a

                KT = gcarve(4096)
                QZ = [gcarve(4096), gcarve(4096)]
                Vt = gcarve(32 * 130)
                strip = [gcarve(W_STRIP), gcarve(W_STRIP)]
                stripH = [gcarve(W_STRIP), gcarve(W_STRIP)]
                stripHB = [Buf(), Buf()]
                Wg = [gcarve(8 * 384).rearrange("p (c n) -> p c n", c=8) for _ in range(2)]
                PT = [gcarve(512) for _ in range(6)]
                Otok = gcarve(4 * 128).rearrange("p (s n) -> p s n", s=4)
                KTB = [Buf() for _ in range(NW)]
                QZB = [[Buf() for _ in range(NW)] for _ in range(2)]
                VB = [Buf() for _ in range(NW)]
                stripB = [Buf(), Buf()]
                WgB = [Buf(), Buf()]
                PTB = [Buf() for _ in range(6)]
                accS = accS_h.ap()
                accSB = Buf()
                OtokB = Buf()
                small = sml_h.ap()
                smallB = Buf()
                S.op("dve", lambda e: e.memset(QZ[0][64:128, :], 0.0), wr=tuple(QZB[0]))
                S.op("dve", lambda e: e.memset(QZ[1][0:64, :], 0.0), wr=tuple(QZB[1]))

                def load_group_weights(g, slot):
                    if g < 4:
                        cols = (g * 128, 512 + g * 128, 1024 + g * 128)
                    else:
                        hh = g - 4
                        cols = (1536 + hh * 128, 2048 + hh * 128, 2560 + hh * 128)
                    for i, c0 in enumerate(cols):
                        S.dma("pool", Wg[slot][:, :, i * 128:(i + 1) * 128],
                              w_in[l, :, c0:c0 + 128].rearrange("(c p) n -> p c n", p=128),
                              wr=(WgB[slot],), chan=ch_w[slot * 3 + i] if slot == 0 else ch_w[3 + i])

                load_group_weights(0, 0)
                pt_i = [0]
                st_i = [0]
                pending_tr = []
                bg = []
                for g in range(8):
                    slot = g % 2
                    dil = g < 4
                    if g + 1 < 8:
                        load_group_weights(g + 1, 1 - slot)
                    heads = (2 * g, 2 * g + 1) if dil else (8 + g - 4,)
                    for u, hd in enumerate(heads):
                        S.dma("pool", stripH[u], bass.AP(f_dram, hd * WF, [[1, 128], [1, W_STRIP]]),
                              rd=(B_f,), wr=(stripHB[u],), chan=ch_w[6 + u], max_dma_last_dim=4352)
                    EV = 65 if dil else 129
                    Vv = Vt[:, 0:32 * 130].rearrange("p (t n) -> p t n", t=32) if dil else \
                        Vt[:, 0:32 * 129].rearrange("p (t n) -> p t n", t=32)
                    if dil:
                        S.op("dve", lambda e, Vv=Vv: e.memset(Vv[:, :, 64:65], 1.0), wr=tuple(VB))
                        S.op("dve", lambda e, Vv=Vv: e.memset(Vv[:, :, 129:130], 1.0), wr=tuple(VB))
                    else:
                        S.op("dve", lambda e, Vv=Vv: e.memset(Vv[:, :, 128:129], 1.0), wr=tuple(VB))
                    for tw in range(NW):
                        tsl = slice(tw * 512, (tw + 1) * 512)
                        for _ in range(3):
                            if bg:
                                bg.pop(0)()
                        bk = 6
                        for kc in range(8):
                            S.op("pe", lambda e, kc=kc, tsl=tsl, bk=bk: e.matmul(
                                PS[bk], lhsT=Wg[slot][:, kc, 0:128], rhs=hT[:, kc, tsl], start=(kc == 0), stop=(kc == 7)),
                                rd=(WgB[slot], hTB[kc][tw]), wr=(PSB[bk],), sig=(kc == 7))
                        S.op("dve", lambda e, tsl=tsl, bk=bk: e.tensor_scalar(
                            out=QZ[0][0:64, tsl], in0=PS[bk][0:64, :], scalar1=0.125, scalar2=None, op0=ALU.mult),
                            rd=(PSB[bk],), wr=(QZB[0][tw],))
                        S.op("act", lambda e, tsl=tsl, bk=bk: e.activation(
                            out=QZ[1][64:128, tsl], in_=PS[bk][64:128, :], func=AF.Copy, scale=0.125),
                            rd=(PSB[bk],), wr=(QZB[1][tw],))
                        bk = 7
                        for kc in range(8):
                            S.op("pe", lambda e, kc=kc, tsl=tsl, bk=bk: e.matmul(
                                PS[bk], lhsT=Wg[slot][:, kc, 128:256], rhs=hT[:, kc, tsl], start=(kc == 0), stop=(kc == 7)),
                                rd=(WgB[slot], hTB[kc][tw]), wr=(PSB[bk],), sig=(kc == 7))
                        S.op("dve", lambda e, tsl=tsl, bk=bk: e.tensor_copy(out=KT[:, tsl], in_=PS[bk]),
                             rd=(PSB[bk],), wr=(KTB[tw],))
                        bk = 3 + (tw % 2)
                        for ss in range(4):
                            t = tw * 4 + ss
                            for kc in range(8):
                                S.op("pe", lambda e, kc=kc, t=t, ss=ss, bk=bk: e.matmul(
                                    PS[bk][:, ss * 128:(ss + 1) * 128], lhsT=hT[:, kc, t * 128:(t + 1) * 128],
                                    rhs=Wg[slot][:, kc, 256:384], start=(kc == 0), stop=(kc == 7)),
                                    rd=(WgB[slot], hTB[kc][tw]), wr=(PSB[bk],), sig=(kc == 7 and ss == 3))
                        pv = PS[bk].rearrange("p (s n) -> p s n", s=4)
                        if dil:
                            S.op("dve", lambda e, tw=tw, pv=pv, Vv=Vv: e.tensor_copy(
                                out=Vv[:, tw * 4:(tw + 1) * 4, 0:64], in_=pv[:, :, 0:64]), rd=(PSB[bk],), wr=(VB[tw],))
                            S.op("act", lambda e, tw=tw, pv=pv, Vv=Vv: e.activation(
                                out=Vv[:, tw * 4:(tw + 1) * 4, 65:129], in_=pv[:, :, 64:128], func=AF.Copy),
                                rd=(PSB[bk],), wr=(VB[tw],))
                        else:
                            S.op("dve", lambda e, tw=tw, pv=pv, Vv=Vv: e.tensor_copy(
                                out=Vv[:, tw * 4:(tw + 1) * 4, 0:128], in_=pv), rd=(PSB[bk],), wr=(VB[tw],))
                    for u, hd in enumerate(heads):
                        for ci, cc in enumerate(range(0, W_STRIP, 512)):
                            n = min(512, W_STRIP - cc)
                            bk = 6 + ci % 2
                            S.op("pe", lambda e, cc=cc, n=n, bk=bk, u=u: e.matmul(PS[bk][:, 0:n], lhsT=flip_b, rhs=stripH[u][:, cc:cc + n],
                                                                               start=True, stop=True),
                                 rd=(stripHB[u], B_const), wr=(PSB[bk],))
                            S.op("dve", lambda e, cc=cc, n=n, bk=bk, u=u: e.tensor_copy(out=strip[u][:, cc:cc + n], in_=PS[bk][:, 0:n]),
                                 rd=(PSB[bk],), wr=(stripB[u],))
                    if True:
                        if dil:
                            accb = [3, 4]
                            reg = lambda u, i: PS[3 + u][:, i * 65:(i + 1) * 65]
                            regb = lambda u, i: PSB[3 + u]
                        else:
                            accb = [3, 4, 5]
                            reg = lambda u, i: PS[3 + (u * 4 + i) // 3][:, ((u * 4 + i) % 3) * 129:((u * 4 + i) % 3 + 1) * 129]
                            regb = lambda u, i: PSB[3 + (u * 4 + i) // 3]
                        items = []
                        for qw in range(NW):
                            kts = list(range(max(0, 4 * qw - 16), 4 * qw + 4)) if dil else list(range(0, 4 * qw + 4))
                            w_items = []
                            for kt in kts:
                                for u in range(2):
                                    q_lo = max(kt, 4 * qw)
                                    q_hi = min(4 * qw + 3, kt + 16) if dil else 4 * qw + 3
                                    if q_hi - q_lo + 1 > 0:
                                        w_items.append([qw, kt, u, q_lo, q_hi, False, False])
                            w_items[0][5] = True
                            w_items[-1][6] = True
                            items += w_items

                        def emit_st(it):
                            qw, kt, u, q_lo, q_hi, _, _ = it
                            nv = q_hi - q_lo + 1
                            N = nv * 128
                            offq = (q_lo - kt) * 128
                            offs = min(offq, W_STRIP - N)
                            sb_ = st_i[0] % 3
                            st_i[0] += 1
                            su = u if dil else 0
                            S.op("pe", lambda e: e.matmul(
                                PS[sb_][:, 0:N], lhsT=KT[:, kt * 128:(kt + 1) * 128],
                                rhs=QZ[u][:, q_lo * 128:q_lo * 128 + N], start=True, stop=True),
                                rd=(KTB[kt // 4],) + tuple(QZB[u][q_lo // 4:q_hi // 4 + 1]), wr=(PSB[sb_],))
                            pi = pt_i[0] % 6
                            pt_i[0] += 1
                            S.op("act", lambda e: e.activation(
                                out=PT[pi][:, 0:N], in_=PS[sb_][:, 0:N], func=AF.Exp), rd=(PSB[sb_],), wr=(PTB[pi],))
                            S.op("dve", lambda e: e.tensor_tensor(
                                out=PT[pi][:, 0:N], in0=PT[pi][:, 0:N], in1=strip[su][:, offs:offs + N], op=ALU.mult),
                                rd=(PTB[pi], stripB[su]), wr=(PTB[pi],))
                            return pi

                        def emit_pv(it, pi):
                            qw, kt, u, q_lo, q_hi, first, last = it
                            if first:
                                for b_ in accb:
                                    S.op("pe", lambda e, b_=b_: e.matmul(PS[b_], lhsT=zeros_b[:, 0:128], rhs=zeros_b,
                                                                        start=True, stop=True, skip_group_check=True),
                                         rd=(B_const,), wr=(PSB[b_],))
                            nv = q_hi - q_lo + 1
                            for i in range(nv):
                                qt = q_lo + i
                                qi = qt - 4 * qw
                                vr = Vv[:, kt, u * 65:(u + 1) * 65] if dil else Vv[:, kt, 0:129]
                                S.op("pe", lambda e, i=i, qi=qi, vr=vr, qt=qt: e.matmul(
                                    reg(u, qi), lhsT=PT[pi][:, i * 128:(i + 1) * 128], rhs=vr,
                                    start=False, stop=(kt == qt), skip_group_check=True),
                                    rd=(PTB[pi], VB[kt // 4]), wr=(regb(u, qi),), sig=(i == nv - 1))
                            if last:
                                win_end(qw)

                        def win_end(qw):
                            while bg:
                                bg.pop(0)()
                            if dil:
                                for u in range(2):
                                    if u == 0:
                                        S.op("dve", lambda e, u=u: e.tensor_copy(out=accS[:, u * 260:(u + 1) * 260], in_=PS[3 + u][:, 0:260]),
                                             rd=(PSB[3 + u],), wr=(accSB,))
                                    else:
                                        S.op("act", lambda e, u=u: e.activation(out=accS[:, u * 260:(u + 1) * 260], in_=PS[3 + u][:, 0:260],
                                                                                func=AF.Copy), rd=(PSB[3 + u],), wr=(accSB,))
                                a3 = accS[:, 0:520].rearrange("p (r n) -> p r n", r=8)
                                rr = small[:, 0:8]
                                bg.append(lambda: S.op("dve", lambda e: e.reciprocal(out=rr.unsqueeze(2), in_=a3[:, :, 64:65]),
                                                       rd=(accSB,), wr=(smallB,)))
                                for u in range(2):
                                    bg.append(lambda u=u: S.op("dve", lambda e: e.tensor_tensor(
                                        out=Otok[:, :, u * 64:(u + 1) * 64], in0=a3[:, u * 4:(u + 1) * 4, 0:64],
                                        in1=rr[:, u * 4:(u + 1) * 4].unsqueeze(2).to_broadcast([128, 4, 64]), op=ALU.mult),
                                        rd=(accSB, smallB), wr=(OtokB,)))
                            else:
                                a3 = accS[:, 0:1032].rearrange("p (r n) -> p r n", r=8)
                                for b_ in range(3):
                                    nr = 3 if b_ < 2 else 2
                                    src = PS[3 + b_][:, 0:nr * 129].rearrange("p (r n) -> p r n", r=nr)
                                    dst = a3[:, 3 * b_:3 * b_ + nr, :]
                                    if b_ == 1:
                                        S.op("act", lambda e, src=src, dst=dst: e.activation(out=dst, in_=src, func=AF.Copy),
                                             rd=(PSB[3 + b_],), wr=(accSB,))
                                    else:
                                        S.op("dve", lambda e, src=src, dst=dst: e.tensor_copy(out=dst, in_=src),
                                             rd=(PSB[3 + b_],), wr=(accSB,))
                                rr = small[:, 0:8]
                                r2n = small[:, 8:12]
                                ss_ = small[:, 12:16]
                                o1 = a3[:, 0:4, 0:128]
                                o2 = a3[:, 4:8, 0:128]
                                A = lambda fn, extra=(): S.op("dve", fn, rd=(accSB, smallB) + extra, wr=(accSB, smallB))
                                bg.append(lambda: A(lambda e: e.reciprocal(out=rr.unsqueeze(2), in_=a3[:, :, 128:129])))
                                bg.append(lambda: A(lambda e: e.tensor_scalar(out=r2n, in0=rr[:, 4:8], scalar1=neg_lam[:, l:l + 1], scalar2=None,
                                                                              op0=ALU.mult), (B_const,)))
                                bg.append(lambda: A(lambda e: e.tensor_tensor(out=o2, in0=o2, in1=r2n.unsqueeze(2).to_broadcast([128, 4, 128]), op=ALU.mult)))
                                bg.append(lambda: A(lambda e: e.tensor_tensor(out=o1, in0=o1, in1=rr[:, 0:4].unsqueeze(2).to_broadcast([128, 4, 128]), op=ALU.mult)))
                                bg.append(lambda: A(lambda e: e.tensor_tensor(out=o1, in0=o1, in1=o2, op=ALU.add)))
                                bg.append(lambda: A(lambda e: e.tensor_tensor(out=o2, in0=o1, in1=o1, op=ALU.mult)))
                                bg.append(lambda: A(lambda e: e.tensor_reduce(out=ss_, in_=o2, axis=AX.X, op=ALU.add)))

                                def rstd_pool():
                                    S.op("pool", lambda e: e.tensor_scalar(out=ss_, in0=ss_, scalar1=1.0 / 128, scalar2=LN_EPS, op0=ALU.mult, op1=ALU.add),
                                         rd=(smallB,), wr=(smallB,))
                                    S.op("pool", lambda e: e.tensor_tensor(out=ss_, in0=ss_, in1=neghalf[:, 0:4], op=ALU.pow),
                                         rd=(smallB, B_const), wr=(smallB,))
                                bg.append(rstd_pool)
                                bg.append(lambda: None)
                                bg.append(lambda: None)
                                bg.append(lambda: A(lambda e: e.tensor_tensor(out=o1, in0=o1, in1=ss_.unsqueeze(2).to_broadcast([128, 4, 128]), op=ALU.mult)))
                                bg.append(lambda: S.op("dve", lambda e: e.tensor_tensor(
                                    out=Otok, in0=o1, in1=gsub[:, l, :].unsqueeze(1).to_broadcast([128, 4, 128]), op=ALU.mult),
                                    rd=(accSB, B_const), wr=(OtokB,)))

                            def tr_out(g=g, qw=qw):
                                bk = 6 + (qw % 2)
                                for qi in range(4):
                                    S.op("pe", lambda e, qi=qi, bk=bk: e.matmul(
                                        PS[bk][:, qi * 128:(qi + 1) * 128], lhsT=Otok[:, qi, :], rhs=ident_b, start=True, stop=True),
                                        rd=(OtokB, B_const), wr=(PSB[bk],), sig=(qi == 3))
                                S.op("act", lambda e, bk=bk: e.activation(out=OT[:, g, qw * 512:(qw + 1) * 512], in_=PS[bk], func=AF.Copy),
                                     rd=(PSB[bk],), wr=(OTB[g][qw], xbB[0], xbB[1]))
                            bg.append(lambda: None)
                            bg.append(lambda: None)
                            bg.append(tr_out)

                        LOOK = 3
                        pis = []
                        for idx in range(len(items) + LOOK):
                            if idx < len(items):
                                pis.append(emit_st(items[idx]))
                                if bg:
                                    bg.pop(0)()
                            if idx - LOOK >= 0:
                                emit_pv(items[idx - LOOK], pis[idx - LOOK])
                    if g == 7:
                        while bg:
                            bg.pop(0)()
                    while pending_tr:
                        pending_tr.pop(0)()
                S.fence(Buf.REG[reg0:], fscr)
            with nc.sbuf_tensor(f"wo{li}", [128, 8, D], BF16) as Wo_h, \
                    nc.sbuf_tensor(f"wk4{li}", [128, 11264], F32) as wk4_h:
                Wo = Wo_h.ap()
                work = wk4_h.ap()
                WoB = Buf()
                for kc in range(8):
                    S.dma("pool", Wo[:, kc, :], w_o[l, kc * 128:(kc + 1) * 128, :], wr=(WoB,), chan=ch_w[kc % 6])
                g_b = work[:, 0:1024]
                lg_b = work[:, 1024:2048]
                lb_b = work[:, 2048:3072]
                tmpd = work[:, 3072:3200]
                bcB = Buf()
                bcast_rows(g_b, l, 16, tmpd, bcB)
                S.dma("sp", lg_b, ln1_g[l:l + 1, :].rearrange("o d -> (o d)").partition_broadcast(128), wr=(bcB,), chan=ch_bc)
                S.dma("sp", lb_b, ln1_b[l:l + 1, :].rearrange("o d -> (o d)").partition_broadcast(128), wr=(bcB,), chan=ch_bc)
                ys = [work[:, 4096 + i * 1024:4096 + (i + 1) * 1024] for i in range(3)]
                ysB = [Buf() for _ in range(3)]
                t1s = [work[:, 8192 + i * 1024:8192 + (i + 1) * 1024] for i in range(2)]
                t1B = [Buf() for _ in range(2)]
                st_ = [work[:, 10240 + i * 32:10240 + i * 32 + 12].rearrange("p (a b) -> p a b", a=2) for i in range(3)]
                mv_ = [work[:, 10400 + i * 8:10400 + i * 8 + 2] for i in range(3)]
                rs_ = [work[:, 10440 + i * 8:10440 + i * 8 + 1] for i in range(3)]
                def ld_x(t):
                    S.dma("sp", ys[t % 3], x_src[t * 128:(t + 1) * 128, :], rd=(B_xdst[t],), wr=(ysB[t % 3],), chan=ch_xl[t % 3])
                ld_x(0)
                ld_x(1)
                for t in range(NT):
                    s3 = t % 3
                    s2 = t % 2
                    if t + 2 < NT:
                        ld_x(t + 2)
                    for hh in range(2):
                        bk = (t % 2) * 2 + hh
                        for kc in range(8):
                            S.op("pe", lambda e, kc=kc, t=t, hh=hh, bk=bk: e.matmul(
                                PS[bk], lhsT=OT[:, kc, t * 128:(t + 1) * 128], rhs=Wo[:, kc, hh * 512:(hh + 1) * 512],
                                start=(kc == 0), stop=(kc == 7)), rd=(OTB[kc][t // 4], WoB), wr=(PSB[bk],), sig=(kc == 7))
                        S.op("dve", lambda e, hh=hh, bk=bk, s2=s2: e.tensor_tensor(
                            out=t1s[s2][:, hh * 512:(hh + 1) * 512], in0=PS[bk], in1=g_b[:, hh * 512:(hh + 1) * 512], op=ALU.mult),
                            rd=(PSB[bk], bcB), wr=(t1B[s2],))
                    S.op("dve", lambda e, s3=s3, s2=s2: e.scalar_tensor_tensor(
                        out=ys[s3], in0=ys[s3], scalar=ALPHA, in1=t1s[s2], op0=ALU.mult, op1=ALU.add),
                        rd=(ysB[s3], t1B[s2]), wr=(ysB[s3],))
                    ln_tail(ys[s3], st_[s3], mv_[s3], rs_[s3], lg_b, lb_b, ysB[s3], x_mid[t * 128:(t + 1) * 128, :], ch_xs[s3], eps_t,
                            B_xmid[t], bcB, use_pool=True)
                S.fence(Buf.REG[reg0:], fscr)
        reg0 = len(Buf.REG)
        if "B" in skip:
            pass
        else:
          with nc.sbuf_tensor(f"h2T{li}", [128, 2, 8, 1024], BF16) as h2_h, \
                nc.sbuf_tensor(f"acc{li}", [128, 2, 8, D], F32) as acc_h, \
                nc.sbuf_tensor(f"ew{li}", [128, 2, 3, 4096], BF16) as ew_h, \
                nc.sbuf_tensor(f"actb{li}", [128, 2, 4, 512], BF16) as act_h, \
                nc.sbuf_tensor(f"xb2{li}", [128, 2, 1024], BF16) as xb2_h, \
                nc.sbuf_tensor(f"bw{li}", [128, 10624], F32) as bw_h:
            h2Tall = h2_h.ap()
            accall = acc_h.ap()
            ew = ew_h.ap()
            actb = act_h.ap()
            bw = bw_h.ap()
            xb2 = xb2_h.ap()
            g_b = bw[:, 0:1024]
            lg_b = bw[:, 1024:2048]
            lb_b = bw[:, 2048:3072]
            tmpd = bw[:, 3072:3200]
            bcB = Buf()
            bcast_rows(g_b, l, 40, tmpd, bcB)
            S.dma("sp", lg_b, ln2_g[l:l + 1, :].rearrange("o d -> (o d)").partition_broadcast(128), wr=(bcB,), chan=ch_bc)
            S.dma("sp", lb_b, ln2_b[l:l + 1, :].rearrange("o d -> (o d)").partition_broadcast(128), wr=(bcB,), chan=ch_bc)
            xstage = [bw[:, 4096 + i * 1024:4096 + (i + 1) * 1024] for i in range(3)]
            xsB = [Buf() for _ in range(3)]
            sgs = [bw[:, 7168 + i * 512:7168 + (i + 1) * 512] for i in range(2)]
            sgB = [Buf(), Buf()]
            rt = bw[:, 8192:8192 + 1024]
            rtB = Buf()
            st_ = [bw[:, 9216 + i * 32:9216 + i * 32 + 12].rearrange("p (a b) -> p a b", a=2) for i in range(3)]
            mv_ = [bw[:, 9344 + i * 8:9344 + i * 8 + 2] for i in range(3)]
            rs_ = [bw[:, 9376 + i * 8:9376 + i * 8 + 1] for i in range(3)]
            gates2 = [bw[:, 9472 + i * 128:9472 + (i + 1) * 128].rearrange("p (s n) -> p s n", s=8) for i in range(2)]
            gatesB = [Buf(), Buf()]
            h2B = [[Buf() for _ in range(2)] for _ in range(2)]
            accB = [[Buf() for _ in range(8)] for _ in range(2)]
            ewguB = [Buf(), Buf()]
            ewdB = [Buf(), Buf()]
            actB = [Buf(), Buf()]
            xb2B = [Buf(), Buf()]
            xctr = [0]

            def load_gu(e_, slot):
                S.dma("pool", ew[:, slot, 0, :].rearrange("p (c n) -> p c n", c=8),
                      w_gate[l, e_].rearrange("(c p) n -> p c n", p=128), wr=(ewguB[slot],), chan=ch_w[slot * 3 + 0])
                S.dma("pool", ew[:, slot, 1, :].rearrange("p (c n) -> p c n", c=8),
                      w_up[l, e_].rearrange("(c p) n -> p c n", p=128), wr=(ewguB[slot],), chan=ch_w[slot * 3 + 1])

            def load_d(e_, slot):
                S.dma("pool", ew[:, slot, 2, :].rearrange("p (c n) -> p c n", c=4),
                      w_down[l, e_].rearrange("(c p) n -> p c n", p=128), wr=(ewdB[slot],), chan=ch_w[slot * 3 + 2])

            def b1_sub(T, s):
                par = T % 2
                h2T = h2Tall[:, par]
                t = T * 8 + s
                s3 = xctr[0] % 3
                s2 = xctr[0] % 2
                xctr[0] += 1
                S.dma("sp", xstage[s3], x_mid[t * 128:(t + 1) * 128, :], rd=(B_xmid[t],), wr=(xsB[s3],), chan=ch_xl[s3])
                S.op("dve", lambda e: e.tensor_copy(out=xb2[:, s2, :], in_=xstage[s3]), rd=(xsB[s3],), wr=(xb2B[s2],))
                for c in range(8):
                    bk = 6 + (c // 4) % 2
                    S.op("pe", lambda e, c=c, bk=bk: e.matmul(
                        PS[bk][:, (c % 4) * 128:(c % 4 + 1) * 128], lhsT=xb2[:, s2, c * 128:(c + 1) * 128],
                        rhs=ident_b, start=True, stop=True), rd=(xb2B[s2], B_const), wr=(PSB[bk],), sig=(c % 4 == 3))
                    if c % 4 == 3:
                        c0 = c - 3
                        for cc in range(4):
                            S.op("act", lambda e, c0=c0, cc=cc, bk=bk: e.activation(
                                out=h2T[:, c0 + cc, s * 128:(s + 1) * 128], in_=PS[bk][:, cc * 128:(cc + 1) * 128],
                                func=AF.Identity, bias=modT[:, l, 24 + c0 + cc:24 + c0 + cc + 1],
                                scale=sc2p[:, l, c0 + cc:c0 + cc + 1]),
                                rd=(PSB[bk], B_const), wr=(h2B[par][s // 4],))

            def router(T):
                par = T % 2
                h2T = h2Tall[:, par]
                gates = gates2[par]
                RB = 7
                for s in range(8):
                    for kc in range(8):
                        S.op("pe", lambda e, s=s, kc=kc: e.matmul(
                            PS[RB][:, s * 16:(s + 1) * 16], lhsT=h2T[:, kc, s * 128:(s + 1) * 128], rhs=wr_b[:, kc, :],
                            start=(kc == 0), stop=(kc == 7)), rd=(h2B[par][s // 4], B_const), wr=(PSB[RB],), sig=(kc == 7 and s == 7))
                v3 = lambda a: a.rearrange("p (s n) -> p s n", s=8)
                sc = v3(rt[:, 0:128])
                sel = v3(rt[:, 128:256])
                sel2 = v3(rt[:, 256:384])
                eq = v3(rt[:, 384:512])
                eq2 = v3(rt[:, 512:640])
                m1 = rt[:, 640:672]
                m2 = rt[:, 672:704]
                gs = rt[:, 704:736]
                gm = rt[:, 736:744]
                pen = rt[:, 744:776]
                t1_ = rt[:, 776:784]
                t2_ = rt[:, 784:792]
                ws_ = rt[:, 792:800]
                g4 = lambda a: a.rearrange("p s (g k) -> p (s g) k", g=4)
                R = lambda fn, **kw: S.op("dve", fn, rd=(rtB,) + kw.get("rd", ()), wr=(rtB,) + kw.get("wr", ()))
                S.op("act", lambda e: e.activation(out=sc, in_=v3(PS[RB][:, 0:128]), func=AF.Sigmoid), rd=(PSB[RB],), wr=(rtB,))
                R(lambda e: e.tensor_tensor(out=sel, in0=sc, in1=br_b.unsqueeze(1).to_broadcast([128, 8, 16]), op=ALU.add), rd=(B_const,))
                R(lambda e: e.tensor_reduce(out=m1, in_=g4(sel), axis=AX.X, op=ALU.max))
                R(lambda e: e.tensor_tensor(out=g4(eq), in0=g4(sel), in1=m1.unsqueeze(2).to_broadcast([128, 32, 4]), op=ALU.is_equal))
                R(lambda e: e.scalar_tensor_tensor(out=sel2, in0=eq, scalar=-BIG, in1=sel, op0=ALU.mult, op1=ALU.add))
                R(lambda e: e.tensor_reduce(out=m2, in_=g4(sel2), axis=AX.X, op=ALU.max))
                R(lambda e: e.tensor_tensor(out=gs, in0=m1, in1=m2, op=ALU.add))
                R(lambda e: e.tensor_reduce(out=gm, in_=gs.rearrange("p (s g) -> p s g", g=4), axis=AX.X, op=ALU.max))
                R(lambda e: e.tensor_tensor(out=pen.rearrange("p (s g) -> p s g", g=4), in0=gs.rearrange("p (s g) -> p s g", g=4),
                                            in1=gm.unsqueeze(2).to_broadcast([128, 8, 4]), op=ALU.is_equal))
                R(lambda e: e.tensor_scalar(out=pen, in0=pen, scalar1=-1.0, scalar2=BIG, op0=ALU.add, op1=ALU.mult))
                R(lambda e: e.tensor_tensor(out=g4(sel2), in0=g4(sel), in1=pen.unsqueeze(2).to_broadcast([128, 32, 4]), op=ALU.add))
                R(lambda e: e.tensor_reduce(out=t1_, in_=sel2, axis=AX.X, op=ALU.max))
                R(lambda e: e.tensor_tensor(out=eq, in0=sel2, in1=t1_.unsqueeze(2).to_broadcast([128, 8, 16]), op=ALU.is_equal))
                R(lambda e: e.scalar_tensor_tensor(out=sel, in0=eq, scalar=-BIG, in1=sel2, op0=ALU.mult, op1=ALU.add))
                R(lambda e: e.tensor_reduce(out=t2_, in_=sel, axis=AX.X, op=ALU.max))
                R(lambda e: e.tensor_tensor(out=eq2, in0=sel, in1=t2_.unsqueeze(2).to_broadcast([128, 8, 16]), op=ALU.is_equal))
                R(lambda e: e.tensor_tensor(out=eq, in0=eq, in1=eq2, op=ALU.add))
                R(lambda e: e.tensor_tensor(out=eq, in0=eq, in1=sc, op=ALU.mult))
                R(lambda e: e.tensor_reduce(out=ws_, in_=eq, axis=AX.X, op=ALU.add))
                R(lambda e: e.reciprocal(out=ws_, in_=ws_))
                S.op("dve", lambda e: e.tensor_tensor(out=gates, in0=eq, in1=ws_.unsqueeze(2).to_broadcast([128, 8, 16]), op=ALU.mult),
                     rd=(rtB,), wr=(gatesB[par],))

            def b4_load(T, s):
                t = T * 8 + s
                s3 = xctr[0] % 3
                xctr[0] += 1
                S.dma("sp", xstage[s3], x_mid[t * 128:(t + 1) * 128, :], rd=(B_xmid[t],), wr=(xsB[s3],), chan=ch_xl[s3])
                return s3

            def b4_sub(T, s, s3=None):
                par = T % 2
                acc = accall[:, par]
                t = T * 8 + s
                if s3 is None:
                    s3 = b4_load(T, s)
                S.op("dve", lambda e: e.tensor_tensor(out=acc[:, s, :], in0=acc[:, s, :], in1=g_b, op=ALU.mult),
                     rd=(accB[par][s], bcB), wr=(accB[par][s],))
                S.op("dve", lambda e: e.scalar_tensor_tensor(
                    out=xstage[s3], in0=xstage[s3], scalar=ALPHA, in1=acc[:, s, :], op0=ALU.mult, op1=ALU.add),
                    rd=(xsB[s3], accB[par][s]), wr=(xsB[s3],))
                ln_tail(xstage[s3], st_[s3], mv_[s3], rs_[s3], lg_b, lb_b, xsB[s3], x_dst[t * 128:(t + 1) * 128, :], ch_xs[s3], eps_t,
                        B_xdst[t], bcB, act_norm=False)

            def gu(T, e_, w_, slot):
                par = T % 2
                h2T = h2Tall[:, par]
                wg = ew[:, slot, 0, :].rearrange("p (c n) -> p c n", c=8)
                wu = ew[:, slot, 1, :].rearrange("p (c n) -> p c n", c=8)
                tsl = slice(w_ * 512, (w_ + 1) * 512)
                for fc in range(4):
                    pb = (fc % 2) * 2
                    for kc in range(8):
                        S.op("pe", lambda e, kc=kc, fc=fc, pb=pb: e.matmul(
                            PS[pb], lhsT=wg[:, kc, fc * 128:(fc + 1) * 128], rhs=h2T[:, kc, tsl],
                            start=(kc == 0), stop=(kc == 7)), rd=(ewguB[slot], h2B[par][w_]), wr=(PSB[pb],), sig=(kc == 7))
                    for kc in range(8):
                        S.op("pe", lambda e, kc=kc, fc=fc, pb=pb: e.matmul(
                            PS[pb + 1], lhsT=wu[:, kc, fc * 128:(fc + 1) * 128], rhs=h2T[:, kc, tsl],
                            start=(kc == 0), stop=(kc == 7)), rd=(ewguB[slot], h2B[par][w_]), wr=(PSB[pb + 1],), sig=(kc == 7))
                    si = fc % 2
                    S.op("act", lambda e, si=si, pb=pb: e.activation(out=sgs[si], in_=PS[pb], func=AF.Silu),
                         rd=(PSB[pb],), wr=(sgB[si],))
                    S.op("dve", lambda e, si=si, pb=pb, fc=fc: e.tensor_tensor(
                        out=actb[:, w_, fc, :], in0=sgs[si], in1=PS[pb + 1], op=ALU.mult),
                        rd=(sgB[si], PSB[pb + 1]), wr=(actB[w_],))

            def down(T, e_, w_, slot):
                par = T % 2
                acc = accall[:, par]
                gates = gates2[par]
                wd = ew[:, slot, 2, :].rearrange("p (c n) -> p c n", c=4)
                for ss in range(4):
                    s = w_ * 4 + ss
                    for hh in range(2):
                        yb = 4 + ((ss * 2 + hh) % 2)
                        for fc in range(4):
                            S.op("pe", lambda e, fc=fc, ss=ss, hh=hh, yb=yb: e.matmul(
                                PS[yb], lhsT=actb[:, w_, fc, ss * 128:(ss + 1) * 128], rhs=wd[:, fc, hh * 512:(hh + 1) * 512],
                                start=(fc == 0), stop=(fc == 3)), rd=(actB[w_], ewdB[slot]), wr=(PSB[yb],), sig=(fc == 3))
                        if e_ == 0:
                            S.op("dve", lambda e, s=s, hh=hh, yb=yb: e.tensor_scalar(
                                out=acc[:, s, hh * 512:(hh + 1) * 512], in0=PS[yb], scalar1=gates[:, s, e_:e_ + 1],
                                scalar2=None, op0=ALU.mult), rd=(PSB[yb], gatesB[par]), wr=(accB[par][s],))
                        else:
                            S.op("dve", lambda e, s=s, hh=hh, yb=yb: e.scalar_tensor_tensor(
                                out=acc[:, s, hh * 512:(hh + 1) * 512], in0=PS[yb], scalar=gates[:, s, e_:e_ + 1],
                                in1=acc[:, s, hh * 512:(hh + 1) * 512], op0=ALU.mult, op1=ALU.add),
                                rd=(PSB[yb], gatesB[par], accB[par][s]), wr=(accB[par][s],))

            load_gu(0, 0)
            load_d(0, 0)
            for s in range(8):
                b1_sub(0, s)
            router(0)
            eidx = 0
            pendD = None
            NTT = 4
            for T in range(NTT):
                extras = []
                if T > 0:
                    extras += [(lambda T=T, s=s: b4_sub(T - 1, s)) for s in range(8)]
                extras_late = []
                if T + 1 < NTT:
                    extras_late += [(lambda T=T, s=s: b1_sub(T + 1, s)) for s in range(8)]
                    extras_late.append(lambda T=T: router(T + 1))
                step = 0
                for e_ in range(NE):
                    slot = eidx % 2
                    eidx += 1
                    nxt = (T, e_ + 1) if e_ + 1 < NE else ((T + 1, 0) if T + 1 < NTT else None)
                    if nxt is not None:
                        load_gu(nxt[1], 1 - slot)
                    for w_ in range(2):
                        gu(T, e_, w_, slot)
                        if pendD is not None:
                            pendD()
                            pendD = None
                        if w_ == 0 and nxt is not None:
                            load_d(nxt[1], 1 - slot)
                        pendD = (lambda T=T, e_=e_, w_=w_, slot=slot: down(T, e_, w_, slot))
                        if extras:
                            extras.pop(0)()
                        elif step >= 14 and extras_late:
                            extras_late.pop(0)()
                        step += 1
                pendD()
                pendD = None
                while extras:
                    extras.pop(0)()
                while extras_late:
                    extras_late.pop(0)()
            slots = [b4_load(NTT - 1, 0), b4_load(NTT - 1, 1)]
            for s in range(8):
                if s + 2 < 8:
                    slots.append(b4_load(NTT - 1, s + 2))
                b4_sub(NTT - 1, s, slots[s])
            S.fence(Buf.REG[reg0:], fscr)
    for ch in ch_xs:
        if ch.last is not None:
            S.wait_event("sp", ch.last)
    return nc, S


_CACHE = {}


def kernel(**inputs):
    if "nc" not in _CACHE:
        _CACHE["nc"] = build()[0]
    nc = _CACHE["nc"]
    consts = _host_consts()
    shared = {}
    for k, v in inputs.items():
        if k in ("x", "c"):
            continue
        a = np.ascontiguousarray(np.asarray(v, dtype=np.float32))
        if k == "b_router":
            a = a.reshape(1, NE)
        shared[k] = a
    shared.update(consts)
    x = np.asarray(inputs["x"], dtype=np.float32)
    c = np.asarray(inputs["c"], dtype=np.float32)
    in_maps = []
    for b in range(8):
        m = dict(shared)
        m["x"] = np.ascontiguousarray(x[b])
        m["c"] = np.ascontiguousarray(c[b:b + 1])
        in_maps.append(m)
    res = run_bass_kernel_spmd(nc, in_maps, core_ids=list(range(8)))
    return np.stack([np.asarray(r["out"]) for r in res.results], axis=0).astype(np.float32)
```

```python
import math
import numpy as np
import concourse.bass as bass
import concourse.mybir as mybir
from concourse.bass_utils import run_bass_kernel_spmd

F32 = mybir.dt.float32
BF16 = mybir.dt.bfloat16
AF = mybir.ActivationFunctionType
ALU = mybir.AluOpType
AX = mybir.AxisListType

D = 1024
SEQ = 4096
DEPTH = 4
NT = SEQ // 128
NW = SEQ // 512
NE = 16
DFF = 512
ALPHA = (2.0 * DEPTH) ** 0.25
LN_EPS = 1e-5
W_STRIP = 2176
WF = 2304
NEG = -30000.0
BIG = 100.0
SEM_LIMIT = 30000


def _t5_bucket_np(d):
    n = np.maximum(d, 0)
    nf = np.maximum(n, 16).astype(np.float32)
    large = 16 + (np.log(nf / np.float32(16)) / np.float32(math.log(2048 / 16)) * np.float32(16)).astype(np.int32)
    large = np.minimum(large, 31)
    return np.where(n < 16, n, large)


def _host_consts():
    m = np.arange(WF)
    d = m - 127
    bucket = _t5_bucket_np(d)
    valid_c = d >= 0
    mult = ((d <= 128).astype(np.int32) + ((d % 4 == 0) & (d <= 512)).astype(np.int32)
            + ((d % 16 == 0) & (d <= 2048)).astype(np.int32))
    mult = np.where(valid_c, mult, 0)
    oh = np.zeros((2, 32, WF), np.float32)
    add = np.zeros((12, WF), np.float32)
    for b in range(32):
        oh[0, b] = ((bucket == b) & (mult > 0)).astype(np.float32)
        oh[1, b] = ((bucket == b) & valid_c).astype(np.float32)
    add_dil = np.where(mult > 0, np.log(np.maximum(mult, 1)).astype(np.float32), np.float32(NEG))
    add_dif = np.where(valid_c, np.float32(0), np.float32(NEG))
    add[:8] = add_dil[None]
    add[8:] = add_dif[None]
    ident = np.eye(128, dtype=np.float32)
    flip = ident[::-1].copy()
    return {"c_oh": oh, "c_add": add, "c_ident": ident, "c_flip": flip}


class Buf:
    __slots__ = ("w", "rd")
    EPOCH = None
    REG = []

    def __init__(self):
        self.w = Buf.EPOCH
        self.rd = {}
        Buf.REG.append(self)


class _Eng:
    def __init__(self, S, name, h, compute):
        self.S, self.name, self.h, self.compute = S, name, h, compute
        self.sem = None
        self.count = 0
        self.own = set()
        self.waited = {}
        self.nsem = 0
        self.new_sem()

    def new_sem(self):
        self.sem = self.S.nc.alloc_semaphore(f"s_{self.name}_{self.nsem}")
        self.nsem += 1
        self.count = 0
        self.own.add(id(self.sem))


class Chan:
    def __init__(self, S, name):
        self.S, self.name = S, name
        self.n = 0
        self.sem = S.nc.alloc_semaphore(f"c_{name}_0")
        self.count = 0
        self.last = None


class Sched:
    def __init__(self, nc):
        self.nc = nc
        self.e = {
            "pe": _Eng(self, "pe", nc.tensor, True),
            "act": _Eng(self, "act", nc.scalar, True),
            "dve": _Eng(self, "dve", nc.vector, True),
            "pool": _Eng(self, "pool", nc.gpsimd, True),
            "sp": _Eng(self, "sp", nc.sync, False),
        }
        self.sems = {}
        self.ninst = 0
        self.log = {k: [] for k in self.e}

    def _wait(self, X, evs):
        best = {}
        for sem, v in evs:
            k = id(sem)
            self.sems[k] = sem
            if v > best.get(k, 0):
                best[k] = v
        for k, v in best.items():
            if X.waited.get(k, 0) < v:
                X.h.wait_ge(self.sems[k], v)
                X.waited[k] = v
                self.log[X.name].append(("w", k, v, self.ninst))

    def _deps(self, X, rd, wr, is_dma):
        evs = []
        for b in rd:
            if b.w is not None:
                sem, v = b.w
                if is_dma or not (X.name == "pe" and id(sem) in X.own):
                    evs.append(b.w)
        for b in wr:
            if b.w is not None:
                sem, v = b.w
                if is_dma or id(sem) not in X.own or (b in rd and X.name != "pe"):
                    evs.append(b.w)
            for k, v in b.rd.items():
                if is_dma or k not in X.own:
                    evs.append((self.sems[k], v))
        return evs

    def _commit(self, ev, rd, wr):
        sem, v = ev
        k = id(sem)
        self.sems[k] = sem
        for b in rd:
            if b.rd.get(k, 0) < v:
                b.rd[k] = v
        for b in wr:
            b.w = ev
            b.rd = {}

    def op(self, eng, fn, rd=(), wr=(), sig=True):
        X = self.e[eng]
        self._wait(X, self._deps(X, rd, wr, False))
        ins = fn(X.h)
        self.ninst += 1
        if sig:
            ins.then_inc(X.sem, 1)
            X.count += 1
            ev = (X.sem, X.count)
            self.log[X.name].append(("i", id(X.sem), 1, self.ninst))
        else:
            ev = (X.sem, X.count + 1)
        self._commit(ev, rd, wr)
        self.last_ev = ev
        if sig and X.count >= SEM_LIMIT:
            X.new_sem()
        return ins

    def dma(self, q, out, in_, rd=(), wr=(), chan=None, **kw):
        X = self.e[q]
        evs = self._deps(X, rd, wr, True)
        if chan.last is not None:
            evs.append(chan.last)
        self._wait(X, evs)
        if chan.count >= SEM_LIMIT:
            chan.n += 1
            chan.sem = self.nc.alloc_semaphore(f"c_{chan.name}_{chan.n}")
            chan.count = 0
        ins = X.h.dma_start(out=out, in_=in_, **kw)
        ins.then_inc(chan.sem, 16)
        chan.count += 16
        self.log[X.name].append(("i", id(chan.sem), 16, self.ninst))
        ev = (chan.sem, chan.count)
        chan.last = ev
        self._commit(ev, rd, wr)
        self.ninst += 1
        return ev

    def fence(self, bufs, scratch):
        bufs = [b for b in bufs]
        self.op("dve", lambda e: e.memset(scratch, 0.0), rd=tuple(bufs), wr=tuple(bufs))
        Buf.EPOCH = self.last_ev

    def check_deadlock(self):
        val = {}
        pos = {k: 0 for k in self.log}
        progress = True
        while progress:
            progress = False
            for k, lg in self.log.items():
                while pos[k] < len(lg):
                    t, sem, v, n = lg[pos[k]]
                    if t == "w":
                        if val.get(sem, 0) >= v:
                            pos[k] += 1
                            progress = True
                        else:
                            break
                    else:
                        val[sem] = val.get(sem, 0) + v
                        pos[k] += 1
                        progress = True
        stuck = {k: (pos[k], len(lg), lg[pos[k]] if pos[k] < len(lg) else None) for k, lg in self.log.items()}
        ok = all(pos[k] == len(lg) for k, lg in self.log.items())
        return ok, stuck, val

    def wait_event(self, eng, ev):
        self._wait(self.e[eng], [ev])


def build(layers=(0, 1, 2, 3), dbg=None, skip=""):
    nc = bass.Bass("TRN2", target_bir_lowering=False)
    S = Sched(nc)
    Buf.EPOCH = None
    Buf.REG = []
    dt_in = lambda name, shape: nc.dram_tensor(name, list(shape), F32, kind="ExternalInput").ap()
    x_in = dt_in("x", (SEQ, D))
    c_in = dt_in("c", (1, D))
    rel_bias = dt_in("rel_bias", (32, 12))
    w_in = dt_in("w_in", (DEPTH, D, 3072))
    w_o = dt_in("w_o", (DEPTH, D, D))
    lam_q1 = dt_in("lam_q1", (DEPTH, 64))
    lam_k1 = dt_in("lam_k1", (DEPTH, 64))
    lam_q2 = dt_in("lam_q2", (DEPTH, 64))
    lam_k2 = dt_in("lam_k2", (DEPTH, 64))
    subln_g = dt_in("subln_g", (DEPTH, 128))
    w_ada = dt_in("w_ada", (DEPTH, D, 6 * D))
    b_ada = dt_in("b_ada", (DEPTH, 6 * D))
    ln1_g = dt_in("ln1_g", (DEPTH, D))
    ln1_b = dt_in("ln1_b", (DEPTH, D))
    ln2_g = dt_in("ln2_g", (DEPTH, D))
    ln2_b = dt_in("ln2_b", (DEPTH, D))
    w_router = dt_in("w_router", (D, NE))
    b_router = dt_in("b_router", (1, NE))
    w_gate = dt_in("w_gate", (DEPTH, NE, D, DFF))
    w_up = dt_in("w_up", (DEPTH, NE, D, DFF))
    w_down = dt_in("w_down", (DEPTH, NE, DFF, D))
    c_oh = dt_in("c_oh", (2, 32, WF))
    c_add = dt_in("c_add", (12, WF))
    c_ident = dt_in("c_ident", (128, 128))
    c_flip = dt_in("c_flip", (128, 128))
    out = nc.dram_tensor("out", [SEQ, D], F32, kind="ExternalOutput").ap()
    xs_a = nc.dram_tensor("xs_a", [SEQ, D], F32, kind=("ExternalOutput" if dbg else "Internal")).ap()
    xs_b = nc.dram_tensor("xs_b", [SEQ, D], F32, kind="Internal").ap()
    f_dram = nc.dram_tensor("f_dram", [12, WF], F32, kind="Internal")

    sb = lambda name, shape, dt: nc.alloc_sbuf_tensor(name, list(shape), dt).ap()
    PS = [nc.alloc_psum_tensor(f"ps{i}", [128, 512], F32).ap() for i in range(8)]
    PSB = [Buf() for _ in range(8)]

    ident_f = sb("ident_f", (128, 128), F32)
    ident_b = sb("ident_b", (128, 128), BF16)
    flip_b = sb("flip_b", (128, 128), BF16)
    ones_f = sb("ones_f", (128, 128), F32)
    zeros_b = sb("zeros_b", (128, 512), BF16)
    modT = sb("modT", (128, DEPTH, 48), F32)
    sc1p = sb("sc1p", (128, DEPTH, 8), F32)
    sc2p = sb("sc2p", (128, DEPTH, 8), F32)
    neg_lam = sb("neg_lam", (128, DEPTH), F32)
    gsub = sb("gsub", (128, DEPTH, 128), F32)
    wr_b = sb("wr_b", (128, 8, NE), BF16)
    br_b = sb("br_b", (128, NE), F32)
    fscr = sb("fscr", (128, 1), F32)
    B_const = Buf()

    ch_const = Chan(S, "const")
    ch_constp = Chan(S, "constp")

    def cdma(out_ap, in_ap, q="sp", **kw):
        return S.dma(q, out_ap, in_ap, rd=(), wr=(B_const,), chan=(ch_const if q == "sp" else ch_constp), **kw)

    with nc.sbuf_tensor("pro_region", [128, 128 * 1024 // 4], F32) as pro_h, \
            nc.sbuf_tensor("cTb", [128, 8], BF16) as cTb_h, \
            nc.sbuf_tensor("wblk", [128, 4, 8, 512], BF16) as wblk_h:
        pro = pro_h.ap()
        off = [0]

        def carve(n_f32):
            a = pro[:, off[0]:off[0] + n_f32]
            off[0] += n_f32
            return a

        cdma(ident_f, c_ident)
        cdma(ident_b, c_ident, q="pool")
        cdma(flip_b, c_flip, q="pool")
        S.op("dve", lambda e: e.memset(ones_f, 1.0), wr=(B_const,))
        S.op("dve", lambda e: e.memset(zeros_b, 0.0), wr=(B_const,))
        cdma(wr_b, w_router.rearrange("(c p) n -> p c n", p=128), q="pool")
        cdma(br_b, b_router.partition_broadcast(128))
        c_rows = carve(128)[0:8, :]
        cdma(c_rows, c_in.rearrange("o (j p) -> (o j) p", p=128))
        ca_rows = carve(128)[0:8, :]
        S.op("act", lambda e: e.activation(out=ca_rows, in_=c_rows, func=AF.Silu), rd=(B_const,), wr=(B_const,))
        cT = carve(8)
        S.op("pe", lambda e: e.matmul(PS[0][:, 0:8], lhsT=ca_rows, rhs=ident_f[0:8, 0:8], start=True, stop=True),
             rd=(B_const,), wr=(PSB[0],))
        S.op("dve", lambda e: e.tensor_copy(out=cT, in_=PS[0][:, 0:8]), rd=(PSB[0],), wr=(B_const,))
        cTb_t = cTb_h.ap()
        S.op("dve", lambda e: e.tensor_copy(out=cTb_t, in_=cT), rd=(B_const,), wr=(B_const,))
        brow = carve(6 * D)[0:1, :]
        mrow = carve(6 * D)[0:1, :]
        rowB = Buf()
        wblk_t = wblk_h.ap()
        wblkB = [Buf() for _ in range(4)]
        wch = [Chan(S, f"wb{i}") for i in range(4)]
        it = 0
        for l in range(DEPTH):
            cdma(brow, b_ada[l:l + 1, :])
            for cb in range(12):
                s_ = it % 4
                it += 1
                S.dma("pool", wblk_t[:, s_], w_ada[l, :, cb * 512:(cb + 1) * 512].rearrange("(c p) n -> p c n", p=128),
                      wr=(wblkB[s_],), chan=wch[s_])
                bk = 3 + (cb % 2)
                for kc in range(8):
                    S.op("pe", lambda e, s_=s_, kc=kc, bk=bk: e.matmul(
                        PS[bk][0:1, :], lhsT=cTb_t[:, kc:kc + 1], rhs=wblk_t[:, s_, kc, :], start=(kc == 0), stop=(kc == 7)),
                        rd=(wblkB[s_], B_const), wr=(PSB[bk],), sig=(kc == 7))
                S.op("dve", lambda e, cb=cb, bk=bk: e.tensor_tensor(
                    out=mrow[:, cb * 512:(cb + 1) * 512], in0=PS[bk][0:1, :], in1=brow[:, cb * 512:(cb + 1) * 512], op=ALU.add),
                    rd=(PSB[bk], B_const), wr=(rowB,))
            for j in range(48):
                S.op("pe", lambda e, j=j, l=l: e.matmul(
                    PS[2][:, l * 48 + j:l * 48 + j + 1], lhsT=mrow[:, j * 128:(j + 1) * 128], rhs=ones_f[0:1, 0:1],
                    start=True, stop=True), rd=(rowB, B_const), wr=(PSB[2],), sig=(j == 47))
        modT_flat = modT.rearrange("p l j -> p (l j)")
        S.op("dve", lambda e: e.tensor_copy(out=modT_flat, in_=PS[2][:, 0:192]), rd=(PSB[2],), wr=(B_const,))
        S.op("dve", lambda e: e.tensor_scalar(out=sc1p, in0=modT[:, :, 8:16], scalar1=1.0, scalar2=None, op0=ALU.add),
             rd=(B_const,), wr=(B_const,))
        S.op("dve", lambda e: e.tensor_scalar(out=sc2p, in0=modT[:, :, 32:40], scalar1=1.0, scalar2=None, op0=ALU.add),
             rd=(B_const,), wr=(B_const,))
        lam_t = [carve(DEPTH * 64) for _ in range(4)]
        for t_, src in zip(lam_t, (lam_q1, lam_k1, lam_q2, lam_k2)):
            cdma(t_, src.rearrange("l e -> (l e)").partition_broadcast(128))
        prod = carve(DEPTH * 64)
        ssum = [carve(DEPTH), carve(DEPTH)]
        for i in range(2):
            S.op("dve", lambda e, i=i: e.tensor_tensor(out=prod, in0=lam_t[2 * i], in1=lam_t[2 * i + 1], op=ALU.mult),
                 rd=(B_const,), wr=(B_const,))
            S.op("dve", lambda e, i=i: e.tensor_reduce(out=ssum[i], in_=prod.rearrange("p (l e) -> p l e", l=DEPTH),
                                                    axis=AX.X, op=ALU.add), rd=(B_const,), wr=(B_const,))
            S.op("act", lambda e, i=i: e.activation(out=ssum[i], in_=ssum[i], func=AF.Exp), rd=(B_const,), wr=(B_const,))
        S.op("dve", lambda e: e.tensor_tensor(out=neg_lam, in0=ssum[1], in1=ssum[0], op=ALU.subtract),
             rd=(B_const,), wr=(B_const,))
        cdma(gsub, subln_g.rearrange("l e -> (l e)").partition_broadcast(128))
        for l in range(DEPTH):
            lam_init = 0.8 - 0.6 * math.exp(-0.3 * l)
            S.op("dve", lambda e, l=l, li=lam_init: e.tensor_scalar(out=neg_lam[:, l:l + 1], in0=neg_lam[:, l:l + 1],
                                                                  scalar1=-li, scalar2=None, op0=ALU.add),
                 rd=(B_const,), wr=(B_const,))
            S.op("dve", lambda e, l=l, li=lam_init: e.tensor_scalar(out=gsub[:, l, :], in0=gsub[:, l, :],
                                                                  scalar1=1.0 - li, scalar2=None, op0=ALU.mult),
                 rd=(B_const,), wr=(B_const,))
        rb = carve(12)[0:32, :]
        cdma(rb, rel_bias)
        oh_sb = carve(2 * WF).rearrange("p (k m) -> p k m", k=2)[0:32]
        cdma(oh_sb, c_oh.rearrange("k b m -> b k m"))
        add_sb = carve(WF)[0:12, :]
        cdma(add_sb, c_add)
        f_sb = carve(WF)[0:12, :]
        fd_sb = carve(WF)[0:12, :]
        for kind, dst in ((0, f_sb), (1, fd_sb)):
            for cc in range(0, WF, 512):
                n = min(512, WF - cc)
                bk = 3 + (cc // 512) % 2
                S.op("pe", lambda e, kind=kind, cc=cc, n=n, bk=bk: e.matmul(
                    PS[bk][0:12, 0:n], lhsT=rb, rhs=oh_sb[:, kind, cc:cc + n], start=True, stop=True),
                    rd=(B_const,), wr=(PSB[bk],))
                S.op("dve", lambda e, dst=dst, cc=cc, n=n, bk=bk: e.tensor_tensor(
                    out=dst[:, cc:cc + n], in0=PS[bk][0:12, 0:n], in1=add_sb[:, cc:cc + n], op=ALU.add),
                    rd=(PSB[bk], B_const), wr=(B_const,))
        S.op("act", lambda e: e.activation(out=f_sb, in_=f_sb, func=AF.Exp), rd=(B_const,), wr=(B_const,))
        S.op("act", lambda e: e.activation(out=fd_sb, in_=fd_sb, func=AF.Exp), rd=(B_const,), wr=(B_const,))
        B_f = Buf()
        ch_f = Chan(S, "fdram")
        fd_ap = f_dram.ap()
        S.dma("sp", fd_ap[0:8, :], f_sb[0:8, :], rd=(B_const,), wr=(B_f,), chan=ch_f)
        S.dma("sp", fd_ap[8:12, :], fd_sb[8:12, :], rd=(B_const,), wr=(B_f,), chan=ch_f)
        S.fence(list(Buf.REG), fscr)

    ch_xl = [Chan(S, f"xl{i}") for i in range(4)]
    ch_xs = [Chan(S, f"xs{i}") for i in range(4)]
    ch_w = [Chan(S, f"w{i}") for i in range(8)]
    ch_bc = Chan(S, "bc")

    def bcast_rows(dst, l, col0, tmp, tmpB):
        for j in range(8):
            bk = 6 + (j % 2)
            S.op("dve", lambda e, j=j: e.tensor_scalar(out=tmp, in0=ident_f, scalar1=modT[:, l, col0 + j:col0 + j + 1],
                                                     scalar2=None, op0=ALU.mult), rd=(B_const,), wr=(tmpB,))
            S.op("pe", lambda e, bk=bk: e.matmul(PS[bk][:, 0:128], lhsT=ones_f, rhs=tmp, start=True, stop=True),
                 rd=(tmpB, B_const), wr=(PSB[bk],))
            S.op("dve", lambda e, j=j, bk=bk: e.tensor_copy(out=dst[:, j * 128:(j + 1) * 128], in_=PS[bk][:, 0:128]),
                 rd=(PSB[bk],), wr=(tmpB,))

    def ln_tail(y, stats, mv, rstd, g_b, b_b, yB, dst_dram, ch, eps_t, dstB, lnB, use_pool=False, act_norm=True):
        for hh in range(2):
            S.op("dve", lambda e, hh=hh: e.bn_stats(out=stats[:, hh, :], in_=y[:, hh * 512:(hh + 1) * 512]),
                 rd=(yB,), wr=(yB,))
        S.op("dve", lambda e: e.bn_aggr(out=mv, in_=stats.rearrange("p a b -> p (a b)")), rd=(yB,), wr=(yB,))
        S.op("act", lambda e: e.activation(out=rstd, in_=mv[:, 1:2], func=AF.Ln, bias=eps_t, scale=1.0), rd=(yB, B_const), wr=(yB,))
        S.op("act", lambda e: e.activation(out=rstd, in_=rstd, func=AF.Exp, scale=-0.5), rd=(yB,), wr=(yB,))
        if act_norm:
            S.op("dve", lambda e: e.tensor_scalar(out=mv[:, 1:2], in0=mv[:, 0:1], scalar1=rstd, scalar2=-1.0, op0=ALU.mult, op1=ALU.mult),
                 rd=(yB,), wr=(yB,))
            S.op("act", lambda e: e.activation(out=y, in_=y, func=AF.Identity, bias=mv[:, 1:2], scale=rstd), rd=(yB,), wr=(yB,))
        else:
            S.op("dve", lambda e: e.tensor_scalar(out=y, in0=y, scalar1=mv[:, 0:1], scalar2=rstd, op0=ALU.subtract, op1=ALU.mult),
                 rd=(yB,), wr=(yB,))
        eng = "pool" if use_pool else "dve"
        S.op(eng, lambda e: e.tensor_tensor(out=y, in0=y, in1=g_b, op=ALU.mult), rd=(yB, lnB), wr=(yB,))
        S.op(eng, lambda e: e.tensor_tensor(out=y, in0=y, in1=b_b, op=ALU.add), rd=(yB, lnB), wr=(yB,))
        S.dma("sp", dst_dram, y, rd=(yB,), wr=(dstB,), chan=ch)

    eps_t = sb("eps_t", (128, 1), F32)
    S.op("dve", lambda e: e.memset(eps_t, LN_EPS), wr=(B_const,))
    neghalf = sb("neghalf", (128, 8), F32)
    S.op("dve", lambda e: e.memset(neghalf, -0.5), wr=(B_const,))

    def stream_bufs(li, n_layers):
        src = x_in if li == 0 else xs_b
        dst = out if li == n_layers - 1 else xs_b
        return src, xs_a, dst

    B_xmid = [Buf() for _ in range(NT)]
    B_xdst = [Buf() for _ in range(NT)]

    for li, l in enumerate(layers):
        x_src, x_mid, x_dst = stream_bufs(li, len(layers))
        reg0 = len(Buf.REG)
        if "A" in skip:
            pass
        else:
          with nc.sbuf_tensor(f"hT{li}", [128, 8, SEQ], BF16) as hT_h, \
                nc.sbuf_tensor(f"rOT{li}", [128, 8, SEQ], BF16) as OT_h:
            hT = hT_h.ap()
            OT = OT_h.ap()
            hTB = [[Buf() for _ in range(NW)] for _ in range(8)]
            OTB = [[Buf() for _ in range(NW)] for _ in range(8)]
            with nc.sbuf_tensor(f"xst{li}", [128, 2, 4, D], F32) as xst_h:
                otf = OT.rearrange("p c t -> p (c t)")
                xbf = [otf[:, i * 4096:(i + 1) * 4096].rearrange("p (s d) -> p s d", s=4) for i in range(2)]
                xstage = [xst_h.ap()[:, i] for i in range(2)]
                xsB = [Buf(), Buf()]
                xbB = [Buf(), Buf()]
                for tw in range(NW):
                    s = tw % 2
                    S.dma("sp", xstage[s], x_src[tw * 512:(tw + 1) * 512, :].rearrange("(s p) d -> p s d", p=128),
                          rd=tuple(B_xdst[tw * 4:tw * 4 + 4]), wr=(xsB[s],), chan=ch_xl[s])
                    for ss in range(4):
                        S.op("dve", lambda e, s=s, ss=ss: e.tensor_copy(out=xbf[s][:, ss, :], in_=xstage[s][:, ss, :]),
                             rd=(xsB[s],), wr=(xbB[s],))
                    for c in range(8):
                        bk = 6 + (c % 2)
                        for ss in range(4):
                            S.op("pe", lambda e, s=s, ss=ss, c=c, bk=bk: e.matmul(
                                PS[bk][:, ss * 128:(ss + 1) * 128], lhsT=xbf[s][:, ss, c * 128:(c + 1) * 128],
                                rhs=ident_b, start=True, stop=True), rd=(xbB[s], B_const), wr=(PSB[bk],), sig=(ss == 3))
                        S.op("act", lambda e, c=c, tw=tw, bk=bk: e.activation(
                            out=hT[:, c, tw * 512:(tw + 1) * 512], in_=PS[bk], func=AF.Identity,
                            bias=modT[:, l, c:c + 1], scale=sc1p[:, l, c:c + 1]),
                            rd=(PSB[bk], B_const), wr=(hTB[c][tw],))
                S.fence(Buf.REG[reg0:], fscr)
            with nc.sbuf_tensor(f"grp{li}", [128, 4096 * 3 + 32 * 130 + 4 * W_STRIP + 2 * 8 * 384 + 6 * 512 + 4 * 128], BF16) as G_h, \
                    nc.sbuf_tensor(f"accS{li}", [128, 1040], F32) as accS_h, \
                    nc.sbuf_tensor(f"sml{li}", [128, 64], F32) as sml_h:
                G = G_h.ap()
                go = [0]

                def gcarve(n):
                    a = G[:, go[0]:go[0] + n]
                    go[0] += n
                    return a

                KT = gcarve(4096)
                QZ = [gcarve(4096), gcarve(4096)]
                Vt = gcarve(32 * 130)
                strip = [gcarve(W_STRIP), gcarve(W_STRIP)]
                stripH = [gcarve(W_STRIP), gcarve(W_STRIP)]
                stripHB = [Buf(), Buf()]
                Wg = [gcarve(8 * 384).rearrange("p (c n) -> p c n", c=8) for _ in range(2)]
                PT = [gcarve(512) for _ in range(6)]
                Otok = gcarve(4 * 128).rearrange("p (s n) -> p s n", s=4)
                KTB = [Buf() for _ in range(NW)]
                QZB = [[Buf() for _ in range(NW)] for _ in range(2)]
                VB = [Buf() for _ in range(NW)]
                stripB = [Buf(), Buf()]
                WgB = [Buf(), Buf()]
                PTB = [Buf() for _ in range(6)]
                accS = accS_h.ap()
                accSB = Buf()
                OtokB = Buf()
                small = sml_h.ap()
                smallB = Buf()
                S.op("dve", lambda e: e.memset(QZ[0][64:128, :], 0.0), wr=tuple(QZB[0]))
                S.op("dve", lambda e: e.memset(QZ[1][0:64, :], 0.0), wr=tuple(QZB[1]))

                def load_group_weights(g, slot):
                    if g < 4:
                        cols = (g * 128, 512 + g * 128, 1024 + g * 128)
                    else:
                        hh = g - 4
                        cols = (1536 + hh * 128, 2048 + hh * 128, 2560 + hh * 128)
                    for i, c0 in enumerate(cols):
                        S.dma("pool", Wg[slot][:, :, i * 128:(i + 1) * 128],
                              w_in[l, :, c0:c0 + 128].rearrange("(c p) n -> p c n", p=128),
                              wr=(WgB[slot],), chan=ch_w[slot * 3 + i] if slot == 0 else ch_w[3 + i])

                load_group_weights(0, 0)
                pt_i = [0]
                st_i = [0]
                pending_tr = []
                bg = []
                for g in range(8):
                    slot = g % 2
                    dil = g < 4
                    if g + 1 < 8:
                        load_group_weights(g + 1, 1 - slot)
                    heads = (2 * g, 2 * g + 1) if dil else (8 + g - 4,)
                    for u, hd in enumerate(heads):
                        S.dma("pool", stripH[u], bass.AP(f_dram, hd * WF, [[1, 128], [1, W_STRIP]]),
                              rd=(B_f,), wr=(stripHB[u],), chan=ch_w[6 + u], max_dma_last_dim=4352)
                    EV = 65 if dil else 129
                    Vv = Vt[:, 0:32 * 130].rearrange("p (t n) -> p t n", t=32) if dil else \
                        Vt[:, 0:32 * 129].rearrange("p (t n) -> p t n", t=32)
                    if dil:
                        S.op("dve", lambda e, Vv=Vv: e.memset(Vv[:, :, 64:65], 1.0), wr=tuple(VB))
                        S.op("dve", lambda e, Vv=Vv: e.memset(Vv[:, :, 129:130], 1.0), wr=tuple(VB))
                    else:
                        S.op("dve", lambda e, Vv=Vv: e.memset(Vv[:, :, 128:129], 1.0), wr=tuple(VB))
                    for tw in range(NW):
                        tsl = slice(tw * 512, (tw + 1) * 512)
                        for _ in range(3):
                            if bg:
                                bg.pop(0)()
                        bk = 6
                        for kc in range(8):
                            S.op("pe", lambda e, kc=kc, tsl=tsl, bk=bk: e.matmul(
                                PS[bk], lhsT=Wg[slot][:, kc, 0:128], rhs=hT[:, kc, tsl], start=(kc == 0), stop=(kc == 7)),
                                rd=(WgB[slot], hTB[kc][tw]), wr=(PSB[bk],), sig=(kc == 7))
                        S.op("dve", lambda e, tsl=tsl, bk=bk: e.tensor_scalar(
                            out=QZ[0][0:64, tsl], in0=PS[bk][0:64, :], scalar1=0.125, scalar2=None, op0=ALU.mult),
                            rd=(PSB[bk],), wr=(QZB[0][tw],))
                        S.op("act", lambda e, tsl=tsl, bk=bk: e.activation(
                            out=QZ[1][64:128, tsl], in_=PS[bk][64:128, :], func=AF.Copy, scale=0.125),
                            rd=(PSB[bk],), wr=(QZB[1][tw],))
                        bk = 7
                        for kc in range(8):
                            S.op("pe", lambda e, kc=kc, tsl=tsl, bk=bk: e.matmul(
                                PS[bk], lhsT=Wg[slot][:, kc, 128:256], rhs=hT[:, kc, tsl], start=(kc == 0), stop=(kc == 7)),
                                rd=(WgB[slot], hTB[kc][tw]), wr=(PSB[bk],), sig=(kc == 7))
                        S.op("dve", lambda e, tsl=tsl, bk=bk: e.tensor_copy(out=KT[:, tsl], in_=PS[bk]),
                             rd=(PSB[bk],), wr=(KTB[tw],))
                        bk = 3 + (tw % 2)
                        for ss in range(4):
                            t = tw * 4 + ss
                            for kc in range(8):
                                S.op("pe", lambda e, kc=kc, t=t, ss=ss, bk=bk: e.matmul(
                                    PS[bk][:, ss * 128:(ss + 1) * 128], lhsT=hT[:, kc, t * 128:(t + 1) * 128],
                                    rhs=Wg[slot][:, kc, 256:384], start=(kc == 0), stop=(kc == 7)),
                                    rd=(WgB[slot], hTB[kc][tw]), wr=(PSB[bk],), sig=(kc == 7 and ss == 3))
                        pv = PS[bk].rearrange("p (s n) -> p s n", s=4)
                        if dil:
                            S.op("dve", lambda e, tw=tw, pv=pv, Vv=Vv: e.tensor_copy(
                                out=Vv[:, tw * 4:(tw + 1) * 4, 0:64], in_=pv[:, :, 0:64]), rd=(PSB[bk],), wr=(VB[tw],))
                            S.op("act", lambda e, tw=tw, pv=pv, Vv=Vv: e.activation(
                                out=Vv[:, tw * 4:(tw + 1) * 4, 65:129], in_=pv[:, :, 64:128], func=AF.Copy),
                                rd=(PSB[bk],), wr=(VB[tw],))
                        else:
                            S.op("dve", lambda e, tw=tw, pv=pv, Vv=Vv: e.tensor_copy(
                                out=Vv[:, tw * 4:(tw + 1) * 4, 0:128], in_=pv), rd=(PSB[bk],), wr=(VB[tw],))
                    for u, hd in enumerate(heads):
                        for ci, cc in enumerate(range(0, W_STRIP, 512)):
                            n = min(512, W_STRIP - cc)
                            bk = 6 + ci % 2
                            S.op("pe", lambda e, cc=cc, n=n, bk=bk, u=u: e.matmul(PS[bk][:, 0:n], lhsT=flip_b, rhs=stripH[u][:, cc:cc + n],
                                                                               start=True, stop=True),
                                 rd=(stripHB[u], B_const), wr=(PSB[bk],))
                            S.op("dve", lambda e, cc=cc, n=n, bk=bk, u=u: e.tensor_copy(out=strip[u][:, cc:cc + n], in_=PS[bk][:, 0:n]),
                                 rd=(PSB[bk],), wr=(stripB[u],))
                    if True:
                        if dil:
                            accb = [3, 4]
                            reg = lambda u, i: PS[3 + u][:, i * 65:(i + 1) * 65]
                            regb = lambda u, i: PSB[3 + u]
                        else:
                            accb = [3, 4, 5]
                            reg = lambda u, i: PS[3 + (u * 4 + i) // 3][:, ((u * 4 + i) % 3) * 129:((u * 4 + i) % 3 + 1) * 129]
                            regb = lambda u, i: PSB[3 + (u * 4 + i) // 3]
                        items = []
                        for qw in range(NW):
                            kts = list(range(max(0, 4 * qw - 16), 4 * qw + 4)) if dil else list(range(0, 4 * qw + 4))
                            w_items = []
                            for kt in kts:
                                for u in range(2):
                                    q_lo = max(kt, 4 * qw)
                                    q_hi = min(4 * qw + 3, kt + 16) if dil else 4 * qw + 3
                                    if q_hi - q_lo + 1 > 0:
                                        w_items.append([qw, kt, u, q_lo, q_hi, False, False])
                            w_items[0][5] = True
                            w_items[-1][6] = True
                            items += w_items

                        def emit_st(it):
                            qw, kt, u, q_lo, q_hi, _, _ = it
                            nv = q_hi - q_lo + 1
                            N = nv * 128
                            offq = (q_lo - kt) * 128
                            offs = min(offq, W_STRIP - N)
                            sb_ = (0, 1, 2, 7)[st_i[0] % 4]
                            st_i[0] += 1
                            su = u if dil else 0
                            S.op("pe", lambda e: e.matmul(
                                PS[sb_][:, 0:N], lhsT=KT[:, kt * 128:(kt + 1) * 128],
                                rhs=QZ[u][:, q_lo * 128:q_lo * 128 + N], start=True, stop=True),
                                rd=(KTB[kt // 4],) + tuple(QZB[u][q_lo // 4:q_hi // 4 + 1]), wr=(PSB[sb_],))
                            pi = pt_i[0] % 6
                            pt_i[0] += 1
                            S.op("act", lambda e: e.activation(
                                out=PT[pi][:, 0:N], in_=PS[sb_][:, 0:N], func=AF.Exp), rd=(PSB[sb_],), wr=(PTB[pi],))
                            S.op("dve", lambda e: e.tensor_tensor(
                                out=PT[pi][:, 0:N], in0=PT[pi][:, 0:N], in1=strip[su][:, offs:offs + N], op=ALU.mult),
                                rd=(PTB[pi], stripB[su]), wr=(PTB[pi],))
                            return pi

                        def emit_pv(it, pi):
                            qw, kt, u, q_lo, q_hi, first, last = it
                            if first:
                                for b_ in accb:
                                    S.op("pe", lambda e, b_=b_: e.matmul(PS[b_], lhsT=zeros_b[:, 0:128], rhs=zeros_b,
                                                                        start=True, stop=True, skip_group_check=True),
                                         rd=(B_const,), wr=(PSB[b_],))
                            nv = q_hi - q_lo + 1
                            for i in range(nv):
                                qt = q_lo + i
                                qi = qt - 4 * qw
                                vr = Vv[:, kt, u * 65:(u + 1) * 65] if dil else Vv[:, kt, 0:129]
                                S.op("pe", lambda e, i=i, qi=qi, vr=vr, qt=qt: e.matmul(
                                    reg(u, qi), lhsT=PT[pi][:, i * 128:(i + 1) * 128], rhs=vr,
                                    start=False, stop=(kt == qt), skip_group_check=True),
                                    rd=(PTB[pi], VB[kt // 4]), wr=(regb(u, qi),), sig=(i == nv - 1))
                            if last:
                                win_end(qw)

                        def win_end(qw):
                            while bg:
                                bg.pop(0)()
                            if dil:
                                for u in range(2):
                                    if u == 0:
                                        S.op("dve", lambda e, u=u: e.tensor_copy(out=accS[:, u * 260:(u + 1) * 260], in_=PS[3 + u][:, 0:260]),
                                             rd=(PSB[3 + u],), wr=(accSB,))
                                    else:
                                        S.op("act", lambda e, u=u: e.activation(out=accS[:, u * 260:(u + 1) * 260], in_=PS[3 + u][:, 0:260],
                                                                                func=AF.Copy), rd=(PSB[3 + u],), wr=(accSB,))
                                a3 = accS[:, 0:520].rearrange("p (r n) -> p r n", r=8)
                                rr = small[:, 0:8]
                                bg.append(lambda: S.op("dve", lambda e: e.reciprocal(out=rr.unsqueeze(2), in_=a3[:, :, 64:65]),
                                                       rd=(accSB,), wr=(smallB,)))
                                for u in range(2):
                                    bg.append(lambda u=u: S.op("dve", lambda e: e.tensor_tensor(
                                        out=Otok[:, :, u * 64:(u + 1) * 64], in0=a3[:, u * 4:(u + 1) * 4, 0:64],
                                        in1=rr[:, u * 4:(u + 1) * 4].unsqueeze(2).to_broadcast([128, 4, 64]), op=ALU.mult),
                                        rd=(accSB, smallB), wr=(OtokB,)))
                            else:
                                a3 = accS[:, 0:1032].rearrange("p (r n) -> p r n", r=8)
                                for b_ in range(3):
                                    nr = 3 if b_ < 2 else 2
                                    src = PS[3 + b_][:, 0:nr * 129].rearrange("p (r n) -> p r n", r=nr)
                                    dst = a3[:, 3 * b_:3 * b_ + nr, :]
                                    if b_ == 1:
                                        S.op("act", lambda e, src=src, dst=dst: e.activation(out=dst, in_=src, func=AF.Copy),
                                             rd=(PSB[3 + b_],), wr=(accSB,))
                                    else:
                                        S.op("dve", lambda e, src=src, dst=dst: e.tensor_copy(out=dst, in_=src),
                                             rd=(PSB[3 + b_],), wr=(accSB,))
                                rr = small[:, 0:8]
                                r2n = small[:, 8:12]
                                ss_ = small[:, 12:16]
                                o1 = a3[:, 0:4, 0:128]
                                o2 = a3[:, 4:8, 0:128]
                                A = lambda fn, extra=(): S.op("dve", fn, rd=(accSB, smallB) + extra, wr=(accSB, smallB))
                                bg.append(lambda: A(lambda e: e.reciprocal(out=rr.unsqueeze(2), in_=a3[:, :, 128:129])))
                                bg.append(lambda: A(lambda e: e.tensor_scalar(out=r2n, in0=rr[:, 4:8], scalar1=neg_lam[:, l:l + 1], scalar2=None,
                                                                              op0=ALU.mult), (B_const,)))
                                bg.append(lambda: A(lambda e: e.tensor_tensor(out=o2, in0=o2, in1=r2n.unsqueeze(2).to_broadcast([128, 4, 128]), op=ALU.mult)))
                                bg.append(lambda: A(lambda e: e.tensor_tensor(out=o1, in0=o1, in1=rr[:, 0:4].unsqueeze(2).to_broadcast([128, 4, 128]), op=ALU.mult)))
                                bg.append(lambda: A(lambda e: e.tensor_tensor(out=o1, in0=o1, in1=o2, op=ALU.add)))
                                bg.append(lambda: A(lambda e: e.tensor_tensor(out=o2, in0=o1, in1=o1, op=ALU.mult)))
                                bg.append(lambda: A(lambda e: e.tensor_reduce(out=ss_, in_=o2, axis=AX.X, op=ALU.add)))

                                def rstd_pool():
                                    S.op("pool", lambda e: e.tensor_scalar(out=ss_, in0=ss_, scalar1=1.0 / 128, scalar2=LN_EPS, op0=ALU.mult, op1=ALU.add),
                                         rd=(smallB,), wr=(smallB,))
                                    S.op("pool", lambda e: e.tensor_tensor(out=ss_, in0=ss_, in1=neghalf[:, 0:4], op=ALU.pow),
                                         rd=(smallB, B_const), wr=(smallB,))
                                bg.append(rstd_pool)
                                bg.append(lambda: None)
                                bg.append(lambda: None)
                                bg.append(lambda: A(lambda e: e.tensor_tensor(out=o1, in0=o1, in1=ss_.unsqueeze(2).to_broadcast([128, 4, 128]), op=ALU.mult)))
                                bg.append(lambda: S.op("dve", lambda e: e.tensor_tensor(
                                    out=Otok, in0=o1, in1=gsub[:, l, :].unsqueeze(1).to_broadcast([128, 4, 128]), op=ALU.mult),
                                    rd=(accSB, B_const), wr=(OtokB,)))

                            def tr_out(g=g, qw=qw):
                                bk = 6
                                for qi in range(4):
                                    S.op("pe", lambda e, qi=qi, bk=bk: e.matmul(
                                        PS[bk][:, qi * 128:(qi + 1) * 128], lhsT=Otok[:, qi, :], rhs=ident_b, start=True, stop=True),
                                        rd=(OtokB, B_const), wr=(PSB[bk],), sig=(qi == 3))
                                S.op("act", lambda e, bk=bk: e.activation(out=OT[:, g, qw * 512:(qw + 1) * 512], in_=PS[bk], func=AF.Copy),
                                     rd=(PSB[bk],), wr=(OTB[g][qw], xbB[0], xbB[1]))
                            bg.append(lambda: None)
                            bg.append(lambda: None)
                            bg.append(tr_out)

                        LOOK = 4
                        pis = []
                        for idx in range(len(items) + LOOK):
                            if idx < len(items):
                                pis.append(emit_st(items[idx]))
                                if bg:
                                    bg.pop(0)()
                            if idx - LOOK >= 0:
                                emit_pv(items[idx - LOOK], pis[idx - LOOK])
                    if g == 7:
                        while bg:
                            bg.pop(0)()
                    while pending_tr:
                        pending_tr.pop(0)()
                S.fence(Buf.REG[reg0:], fscr)
            with nc.sbuf_tensor(f"wo{li}", [128, 8, D], BF16) as Wo_h, \
                    nc.sbuf_tensor(f"wk4{li}", [128, 11264], F32) as wk4_h:
                Wo = Wo_h.ap()
                work = wk4_h.ap()
                WoB = Buf()
                for kc in range(8):
                    S.dma("pool", Wo[:, kc, :], w_o[l, kc * 128:(kc + 1) * 128, :], wr=(WoB,), chan=ch_w[kc % 6])
                g_b = work[:, 0:1024]
                lg_b = work[:, 1024:2048]
                lb_b = work[:, 2048:3072]
                tmpd = work[:, 3072:3200]
                bcB = Buf()
                bcast_rows(g_b, l, 16, tmpd, bcB)
                S.dma("sp", lg_b, ln1_g[l:l + 1, :].rearrange("o d -> (o d)").partition_broadcast(128), wr=(bcB,), chan=ch_bc)
                S.dma("sp", lb_b, ln1_b[l:l + 1, :].rearrange("o d -> (o d)").partition_broadcast(128), wr=(bcB,), chan=ch_bc)
                ys = [work[:, 4096 + i * 1024:4096 + (i + 1) * 1024] for i in range(3)]
                ysB = [Buf() for _ in range(3)]
                t1s = [work[:, 8192 + i * 1024:8192 + (i + 1) * 1024] for i in range(2)]
                t1B = [Buf() for _ in range(2)]
                st_ = [work[:, 10240 + i * 32:10240 + i * 32 + 12].rearrange("p (a b) -> p a b", a=2) for i in range(3)]
                mv_ = [work[:, 10400 + i * 8:10400 + i * 8 + 2] for i in range(3)]
                rs_ = [work[:, 10440 + i * 8:10440 + i * 8 + 1] for i in range(3)]
                def ld_x(t):
                    S.dma("sp", ys[t % 3], x_src[t * 128:(t + 1) * 128, :], rd=(B_xdst[t],), wr=(ysB[t % 3],), chan=ch_xl[t % 3])
                ld_x(0)
                ld_x(1)
                for t in range(NT):
                    s3 = t % 3
                    s2 = t % 2
                    if t + 2 < NT:
                        ld_x(t + 2)
                    for hh in range(2):
                        bk = (t % 2) * 2 + hh
                        for kc in range(8):
                            S.op("pe", lambda e, kc=kc, t=t, hh=hh, bk=bk: e.matmul(
                                PS[bk], lhsT=OT[:, kc, t * 128:(t + 1) * 128], rhs=Wo[:, kc, hh * 512:(hh + 1) * 512],
                                start=(kc == 0), stop=(kc == 7)), rd=(OTB[kc][t // 4], WoB), wr=(PSB[bk],), sig=(kc == 7))
                        S.op("dve", lambda e, hh=hh, bk=bk, s2=s2: e.tensor_tensor(
                            out=t1s[s2][:, hh * 512:(hh + 1) * 512], in0=PS[bk], in1=g_b[:, hh * 512:(hh + 1) * 512], op=ALU.mult),
                            rd=(PSB[bk], bcB), wr=(t1B[s2],))
                    S.op("dve", lambda e, s3=s3, s2=s2: e.scalar_tensor_tensor(
                        out=ys[s3], in0=ys[s3], scalar=ALPHA, in1=t1s[s2], op0=ALU.mult, op1=ALU.add),
                        rd=(ysB[s3], t1B[s2]), wr=(ysB[s3],))
                    ln_tail(ys[s3], st_[s3], mv_[s3], rs_[s3], lg_b, lb_b, ysB[s3], x_mid[t * 128:(t + 1) * 128, :], ch_xs[s3], eps_t,
                            B_xmid[t], bcB, use_pool=True)
                S.fence(Buf.REG[reg0:], fscr)
        reg0 = len(Buf.REG)
        if "B" in skip:
            pass
        else:
          with nc.sbuf_tensor(f"h2T{li}", [128, 2, 8, 1024], BF16) as h2_h, \
                nc.sbuf_tensor(f"acc{li}", [128, 2, 8, D], F32) as acc_h, \
                nc.sbuf_tensor(f"ew{li}", [128, 2, 3, 4096], BF16) as ew_h, \
                nc.sbuf_tensor(f"actb{li}", [128, 2, 4, 512], BF16) as act_h, \
                nc.sbuf_tensor(f"xb2{li}", [128, 2, 1024], BF16) as xb2_h, \
                nc.sbuf_tensor(f"bw{li}", [128, 10624], F32) as bw_h:
            h2Tall = h2_h.ap()
            accall = acc_h.ap()
            ew = ew_h.ap()
            actb = act_h.ap()
            bw = bw_h.ap()
            xb2 = xb2_h.ap()
            g_b = bw[:, 0:1024]
            lg_b = bw[:, 1024:2048]
            lb_b = bw[:, 2048:3072]
            tmpd = bw[:, 3072:3200]
            bcB = Buf()
            bcast_rows(g_b, l, 40, tmpd, bcB)
            S.dma("sp", lg_b, ln2_g[l:l + 1, :].rearrange("o d -> (o d)").partition_broadcast(128), wr=(bcB,), chan=ch_bc)
            S.dma("sp", lb_b, ln2_b[l:l + 1, :].rearrange("o d -> (o d)").partition_broadcast(128), wr=(bcB,), chan=ch_bc)
            xstage = [bw[:, 4096 + i * 1024:4096 + (i + 1) * 1024] for i in range(3)]
            xsB = [Buf() for _ in range(3)]
            sgs = [bw[:, 7168 + i * 512:7168 + (i + 1) * 512] for i in range(2)]
            sgB = [Buf(), Buf()]
            rt = bw[:, 8192:8192 + 1024]
            rtB = Buf()
            st_ = [bw[:, 9216 + i * 32:9216 + i * 32 + 12].rearrange("p (a b) -> p a b", a=2) for i in range(3)]
            mv_ = [bw[:, 9344 + i * 8:9344 + i * 8 + 2] for i in range(3)]
            rs_ = [bw[:, 9376 + i * 8:9376 + i * 8 + 1] for i in range(3)]
            gates2 = [bw[:, 9472 + i * 128:9472 + (i + 1) * 128].rearrange("p (s n) -> p s n", s=8) for i in range(2)]
            gatesB = [Buf(), Buf()]
            h2B = [[Buf() for _ in range(2)] for _ in range(2)]
            accB = [[Buf() for _ in range(8)] for _ in range(2)]
            ewguB = [Buf(), Buf()]
            ewdB = [Buf(), Buf()]
            actB = [Buf(), Buf()]
            xb2B = [Buf(), Buf()]
            xctr = [0]

            def load_gu(e_, slot):
                S.dma("pool", ew[:, slot, 0, :].rearrange("p (c n) -> p c n", c=8),
                      w_gate[l, e_].rearrange("(c p) n -> p c n", p=128), wr=(ewguB[slot],), chan=ch_w[slot * 3 + 0])
                S.dma("pool", ew[:, slot, 1, :].rearrange("p (c n) -> p c n", c=8),
                      w_up[l, e_].rearrange("(c p) n -> p c n", p=128), wr=(ewguB[slot],), chan=ch_w[slot * 3 + 1])

            def load_d(e_, slot):
                S.dma("pool", ew[:, slot, 2, :].rearrange("p (c n) -> p c n", c=4),
                      w_down[l, e_].rearrange("(c p) n -> p c n", p=128), wr=(ewdB[slot],), chan=ch_w[slot * 3 + 2])

            def b1_sub(T, s):
                par = T % 2
                h2T = h2Tall[:, par]
                t = T * 8 + s
                s3 = xctr[0] % 3
                s2 = xctr[0] % 2
                xctr[0] += 1
                S.dma("sp", xstage[s3], x_mid[t * 128:(t + 1) * 128, :], rd=(B_xmid[t],), wr=(xsB[s3],), chan=ch_xl[s3])
                S.op("dve", lambda e: e.tensor_copy(out=xb2[:, s2, :], in_=xstage[s3]), rd=(xsB[s3],), wr=(xb2B[s2],))
                for c in range(8):
                    bk = 6 + (c // 4) % 2
                    S.op("pe", lambda e, c=c, bk=bk: e.matmul(
                        PS[bk][:, (c % 4) * 128:(c % 4 + 1) * 128], lhsT=xb2[:, s2, c * 128:(c + 1) * 128],
                        rhs=ident_b, start=True, stop=True), rd=(xb2B[s2], B_const), wr=(PSB[bk],), sig=(c % 4 == 3))
                    if c % 4 == 3:
                        c0 = c - 3
                        for cc in range(4):
                            S.op("act", lambda e, c0=c0, cc=cc, bk=bk: e.activation(
                                out=h2T[:, c0 + cc, s * 128:(s + 1) * 128], in_=PS[bk][:, cc * 128:(cc + 1) * 128],
                                func=AF.Identity, bias=modT[:, l, 24 + c0 + cc:24 + c0 + cc + 1],
                                scale=sc2p[:, l, c0 + cc:c0 + cc + 1]),
                                rd=(PSB[bk], B_const), wr=(h2B[par][s // 4],))

            def router(T):
                par = T % 2
                h2T = h2Tall[:, par]
                gates = gates2[par]
                RB = 7
                for s in range(8):
                    for kc in range(8):
                        S.op("pe", lambda e, s=s, kc=kc: e.matmul(
                            PS[RB][:, s * 16:(s + 1) * 16], lhsT=h2T[:, kc, s * 128:(s + 1) * 128], rhs=wr_b[:, kc, :],
                            start=(kc == 0), stop=(kc == 7)), rd=(h2B[par][s // 4], B_const), wr=(PSB[RB],), sig=(kc == 7 and s == 7))
                v3 = lambda a: a.rearrange("p (s n) -> p s n", s=8)
                sc = v3(rt[:, 0:128])
                sel = v3(rt[:, 128:256])
                sel2 = v3(rt[:, 256:384])
                eq = v3(rt[:, 384:512])
                eq2 = v3(rt[:, 512:640])
                m1 = rt[:, 640:672]
                m2 = rt[:, 672:704]
                gs = rt[:, 704:736]
                gm = rt[:, 736:744]
                pen = rt[:, 744:776]
                t1_ = rt[:, 776:784]
                t2_ = rt[:, 784:792]
                ws_ = rt[:, 792:800]
                g4 = lambda a: a.rearrange("p s (g k) -> p (s g) k", g=4)
                R = lambda fn, **kw: S.op("dve", fn, rd=(rtB,) + kw.get("rd", ()), wr=(rtB,) + kw.get("wr", ()))
                S.op("act", lambda e: e.activation(out=sc, in_=v3(PS[RB][:, 0:128]), func=AF.Sigmoid), rd=(PSB[RB],), wr=(rtB,))
                R(lambda e: e.tensor_tensor(out=sel, in0=sc, in1=br_b.unsqueeze(1).to_broadcast([128, 8, 16]), op=ALU.add), rd=(B_const,))
                R(lambda e: e.tensor_reduce(out=m1, in_=g4(sel), axis=AX.X, op=ALU.max))
                R(lambda e: e.tensor_tensor(out=g4(eq), in0=g4(sel), in1=m1.unsqueeze(2).to_broadcast([128, 32, 4]), op=ALU.is_equal))
                R(lambda e: e.scalar_tensor_tensor(out=sel2, in0=eq, scalar=-BIG, in1=sel, op0=ALU.mult, op1=ALU.add))
                R(lambda e: e.tensor_reduce(out=m2, in_=g4(sel2), axis=AX.X, op=ALU.max))
                R(lambda e: e.tensor_tensor(out=gs, in0=m1, in1=m2, op=ALU.add))
                R(lambda e: e.tensor_reduce(out=gm, in_=gs.rearrange("p (s g) -> p s g", g=4), axis=AX.X, op=ALU.max))
                R(lambda e: e.tensor_tensor(out=pen.rearrange("p (s g) -> p s g", g=4), in0=gs.rearrange("p (s g) -> p s g", g=4),
                                            in1=gm.unsqueeze(2).to_broadcast([128, 8, 4]), op=ALU.is_equal))
                R(lambda e: e.tensor_scalar(out=pen, in0=pen, scalar1=-1.0, scalar2=BIG, op0=ALU.add, op1=ALU.mult))
                R(lambda e: e.tensor_tensor(out=g4(sel2), in0=g4(sel), in1=pen.unsqueeze(2).to_broadcast([128, 32, 4]), op=ALU.add))
                R(lambda e: e.tensor_reduce(out=t1_, in_=sel2, axis=AX.X, op=ALU.max))
                R(lambda e: e.tensor_tensor(out=eq, in0=sel2, in1=t1_.unsqueeze(2).to_broadcast([128, 8, 16]), op=ALU.is_equal))
                R(lambda e: e.scalar_tensor_tensor(out=sel, in0=eq, scalar=-BIG, in1=sel2, op0=ALU.mult, op1=ALU.add))
                R(lambda e: e.tensor_reduce(out=t2_, in_=sel, axis=AX.X, op=ALU.max))
                R(lambda e: e.tensor_tensor(out=eq2, in0=sel, in1=t2_.unsqueeze(2).to_broadcast([128, 8, 16]), op=ALU.is_equal))
                R(lambda e: e.tensor_tensor(out=eq, in0=eq, in1=eq2, op=ALU.add))
                R(lambda e: e.tensor_tensor(out=eq, in0=eq, in1=sc, op=ALU.mult))
                R(lambda e: e.tensor_reduce(out=ws_, in_=eq, axis=AX.X, op=ALU.add))
                R(lambda e: e.reciprocal(out=ws_, in_=ws_))
                S.op("dve", lambda e: e.tensor_tensor(out=gates, in0=eq, in1=ws_.unsqueeze(2).to_broadcast([128, 8, 16]), op=ALU.mult),
                     rd=(rtB,), wr=(gatesB[par],))

            def b4_load(T, s):
                t = T * 8 + s
                s3 = xctr[0] % 3
                xctr[0] += 1
                S.dma("sp", xstage[s3], x_mid[t * 128:(t + 1) * 128, :], rd=(B_xmid[t],), wr=(xsB[s3],), chan=ch_xl[s3])
                return s3

            def b4_sub(T, s, s3=None):
                par = T % 2
                acc = accall[:, par]
                t = T * 8 + s
                if s3 is None:
                    s3 = b4_load(T, s)
                S.op("dve", lambda e: e.tensor_tensor(out=acc[:, s, :], in0=acc[:, s, :], in1=g_b, op=ALU.mult),
                     rd=(accB[par][s], bcB), wr=(accB[par][s],))
                S.op("dve", lambda e: e.scalar_tensor_tensor(
                    out=xstage[s3], in0=xstage[s3], scalar=ALPHA, in1=acc[:, s, :], op0=ALU.mult, op1=ALU.add),
                    rd=(xsB[s3], accB[par][s]), wr=(xsB[s3],))
                ln_tail(xstage[s3], st_[s3], mv_[s3], rs_[s3], lg_b, lb_b, xsB[s3], x_dst[t * 128:(t + 1) * 128, :], ch_xs[s3], eps_t,
                        B_xdst[t], bcB, act_norm=False)

            def gu(T, e_, w_, slot):
                par = T % 2
                h2T = h2Tall[:, par]
                wg = ew[:, slot, 0, :].rearrange("p (c n) -> p c n", c=8)
                wu = ew[:, slot, 1, :].rearrange("p (c n) -> p c n", c=8)
                tsl = slice(w_ * 512, (w_ + 1) * 512)
                for fc in range(4):
                    pb = (fc % 2) * 2
                    for kc in range(8):
                        S.op("pe", lambda e, kc=kc, fc=fc, pb=pb: e.matmul(
                            PS[pb], lhsT=wg[:, kc, fc * 128:(fc + 1) * 128], rhs=h2T[:, kc, tsl],
                            start=(kc == 0), stop=(kc == 7)), rd=(ewguB[slot], h2B[par][w_]), wr=(PSB[pb],), sig=(kc == 7))
                    for kc in range(8):
                        S.op("pe", lambda e, kc=kc, fc=fc, pb=pb: e.matmul(
                            PS[pb + 1], lhsT=wu[:, kc, fc * 128:(fc + 1) * 128], rhs=h2T[:, kc, tsl],
                            start=(kc == 0), stop=(kc == 7)), rd=(ewguB[slot], h2B[par][w_]), wr=(PSB[pb + 1],), sig=(kc == 7))
                    si = fc % 2
                    S.op("act", lambda e, si=si, pb=pb: e.activation(out=sgs[si], in_=PS[pb], func=AF.Silu),
                         rd=(PSB[pb],), wr=(sgB[si],))
                    S.op("dve", lambda e, si=si, pb=pb, fc=fc: e.tensor_tensor(
                        out=actb[:, w_, fc, :], in0=sgs[si], in1=PS[pb + 1], op=ALU.mult),
                        rd=(sgB[si], PSB[pb + 1]), wr=(actB[w_],))

            def down(T, e_, w_, slot):
                par = T % 2
                acc = accall[:, par]
                gates = gates2[par]
                wd = ew[:, slot, 2, :].rearrange("p (c n) -> p c n", c=4)
                for ss in range(4):
                    s = w_ * 4 + ss
                    for hh in range(2):
                        yb = 4 + ((ss * 2 + hh) % 2)
                        for fc in range(4):
                            S.op("pe", lambda e, fc=fc, ss=ss, hh=hh, yb=yb: e.matmul(
                                PS[yb], lhsT=actb[:, w_, fc, ss * 128:(ss + 1) * 128], rhs=wd[:, fc, hh * 512:(hh + 1) * 512],
                                start=(fc == 0), stop=(fc == 3)), rd=(actB[w_], ewdB[slot]), wr=(PSB[yb],), sig=(fc == 3))
                        if e_ == 0:
                            S.op("dve", lambda e, s=s, hh=hh, yb=yb: e.tensor_scalar(
                                out=acc[:, s, hh * 512:(hh + 1) * 512], in0=PS[yb], scalar1=gates[:, s, e_:e_ + 1],
                                scalar2=None, op0=ALU.mult), rd=(PSB[yb], gatesB[par]), wr=(accB[par][s],))
                        else:
                            S.op("dve", lambda e, s=s, hh=hh, yb=yb: e.scalar_tensor_tensor(
                                out=acc[:, s, hh * 512:(hh + 1) * 512], in0=PS[yb], scalar=gates[:, s, e_:e_ + 1],
                                in1=acc[:, s, hh * 512:(hh + 1) * 512], op0=ALU.mult, op1=ALU.add),
                                rd=(PSB[yb], gatesB[par], accB[par][s]), wr=(accB[par][s],))

            load_gu(0, 0)
            load_d(0, 0)
            for s in range(8):
                b1_sub(0, s)
            router(0)
            eidx = 0
            pendD = None
            NTT = 4
            for T in range(NTT):
                extras = []
                if T > 0:
                    extras += [(lambda T=T, s=s: b4_sub(T - 1, s)) for s in range(8)]
                extras_late = []
                if T + 1 < NTT:
                    extras_late += [(lambda T=T, s=s: b1_sub(T + 1, s)) for s in range(8)]
                    extras_late.append(lambda T=T: router(T + 1))
                step = 0
                for e_ in range(NE):
                    slot = eidx % 2
                    eidx += 1
                    nxt = (T, e_ + 1) if e_ + 1 < NE else ((T + 1, 0) if T + 1 < NTT else None)
                    if nxt is not None:
                        load_gu(nxt[1], 1 - slot)
                    for w_ in range(2):
                        gu(T, e_, w_, slot)
                        if pendD is not None:
                            pendD()
                            pendD = None
                        if w_ == 0 and nxt is not None:
                            load_d(nxt[1], 1 - slot)
                        pendD = (lambda T=T, e_=e_, w_=w_, slot=slot: down(T, e_, w_, slot))
                        if extras:
                            extras.pop(0)()
                        elif step >= 14 and extras_late:
                            extras_late.pop(0)()
                        step += 1
                pendD()
                pendD = None
                while extras:
                    extras.pop(0)()
                while extras_late:
                    extras_late.pop(0)()
            slots = [b4_load(NTT - 1, 0), b4_load(NTT - 1, 1)]
            for s in range(8):
                if s + 2 < 8:
                    slots.append(b4_load(NTT - 1, s + 2))
                b4_sub(NTT - 1, s, slots[s])
            S.fence(Buf.REG[reg0:], fscr)
    for ch in ch_xs:
        if ch.last is not None:
            S.wait_event("sp", ch.last)
    return nc, S


_CACHE = {}


def kernel(**inputs):
    if "nc" not in _CACHE:
        _CACHE["nc"] = build()[0]
    nc = _CACHE["nc"]
    consts = _host_consts()
    shared = {}
    for k, v in inputs.items():
        if k in ("x", "c"):
            continue
        a = np.ascontiguousarray(np.asarray(v, dtype=np.float32))
        if k == "b_router":
            a = a.reshape(1, NE)
        shared[k] = a
    shared.update(consts)
    x = np.asarray(inputs["x"], dtype=np.float32)
    c = np.asarray(inputs["c"], dtype=np.float32)
    in_maps = []
    for b in range(8):
        m = dict(shared)
        m["x"] = np.ascontiguousarray(x[b])
        m["c"] = np.ascontiguousarray(c[b:b + 1])
        in_maps.append(m)
    res = run_bass_kernel_spmd(nc, in_maps, core_ids=list(range(8)))
    return np.stack([np.asarray(r["out"]) for r in res.results], axis=0).astype(np.float32)
```

```python
import math
import numpy as np
import concourse.bass as bass
import concourse.mybir as mybir
from concourse.bass_utils import run_bass_kernel_spmd

F32 = mybir.dt.float32
BF16 = mybir.dt.bfloat16
AF = mybir.ActivationFunctionType
ALU = mybir.AluOpType
AX = mybir.AxisListType

D = 1024
SEQ = 4096
DEPTH = 4
NT = SEQ // 128
NW = SEQ // 512
NE = 16
DFF = 512
ALPHA = (2.0 * DEPTH) ** 0.25
LN_EPS = 1e-5
W_STRIP = 2176
WF = 2304
NEG = -30000.0
BIG = 100.0
SEM_LIMIT = 30000


def _t5_bucket_np(d):
    n = np.maximum(d, 0)
    nf = np.maximum(n, 16).astype(np.float32)
    large = 16 + (np.log(nf / np.float32(16)) / np.float32(math.log(2048 / 16)) * np.float32(16)).astype(np.int32)
    large = np.minimum(large, 31)
    return np.where(n < 16, n, large)


def _host_consts():
    m = np.arange(WF)
    d = m - 127
    bucket = _t5_bucket_np(d)
    valid_c = d >= 0
    mult = ((d <= 128).astype(np.int32) + ((d % 4 == 0) & (d <= 512)).astype(np.int32)
            + ((d % 16 == 0) & (d <= 2048)).astype(np.int32))
    mult = np.where(valid_c, mult, 0)
    oh = np.zeros((2, 32, WF), np.float32)
    add = np.zeros((12, WF), np.float32)
    for b in range(32):
        oh[0, b] = ((bucket == b) & (mult > 0)).astype(np.float32)
        oh[1, b] = ((bucket == b) & valid_c).astype(np.float32)
    add_dil = np.where(mult > 0, np.log(np.maximum(mult, 1)).astype(np.float32), np.float32(NEG))
    add_dif = np.where(valid_c, np.float32(0), np.float32(NEG))
    add[:8] = add_dil[None]
    add[8:] = add_dif[None]
    ident = np.eye(128, dtype=np.float32)
    flip = ident[::-1].copy()
    return {"c_oh": oh, "c_add": add, "c_ident": ident, "c_flip": flip}


class Buf:
    __slots__ = ("w", "rd")
    EPOCH = None
    REG = []

    def __init__(self):
        self.w = Buf.EPOCH
        self.rd = {}
        Buf.REG.append(self)


class _Eng:
    def __init__(self, S, name, h, compute):
        self.S, self.name, self.h, self.compute = S, name, h, compute
        self.sem = None
        self.count = 0
        self.own = set()
        self.waited = {}
        self.nsem = 0
        self.new_sem()

    def new_sem(self):
        self.sem = self.S.nc.alloc_semaphore(f"s_{self.name}_{self.nsem}")
        self.nsem += 1
        self.count = 0
        self.own.add(id(self.sem))


class Chan:
    def __init__(self, S, name):
        self.S, self.name = S, name
        self.n = 0
        self.sem = S.nc.alloc_semaphore(f"c_{name}_0")
        self.count = 0
        self.last = None


class Sched:
    def __init__(self, nc):
        self.nc = nc
        self.e = {
            "pe": _Eng(self, "pe", nc.tensor, True),
            "act": _Eng(self, "act", nc.scalar, True),
            "dve": _Eng(self, "dve", nc.vector, True),
            "pool": _Eng(self, "pool", nc.gpsimd, True),
            "sp": _Eng(self, "sp", nc.sync, False),
        }
        self.sems = {}
        self.ninst = 0
        self.log = {k: [] for k in self.e}

    def _wait(self, X, evs):
        best = {}
        for sem, v in evs:
            k = id(sem)
            self.sems[k] = sem
            if v > best.get(k, 0):
                best[k] = v
        for k, v in best.items():
            if X.waited.get(k, 0) < v:
                X.h.wait_ge(self.sems[k], v)
                X.waited[k] = v
                self.log[X.name].append(("w", k, v, self.ninst))

    def _deps(self, X, rd, wr, is_dma):
        evs = []
        for b in rd:
            if b.w is not None:
                sem, v = b.w
                if is_dma or not (X.name == "pe" and id(sem) in X.own):
                    evs.append(b.w)
        for b in wr:
            if b.w is not None:
                sem, v = b.w
                if is_dma or id(sem) not in X.own or (b in rd and X.name != "pe"):
                    evs.append(b.w)
            for k, v in b.rd.items():
                if is_dma or k not in X.own:
                    evs.append((self.sems[k], v))
        return evs

    def _commit(self, ev, rd, wr):
        sem, v = ev
        k = id(sem)
        self.sems[k] = sem
        for b in rd:
            if b.rd.get(k, 0) < v:
                b.rd[k] = v
        for b in wr:
            b.w = ev
            b.rd = {}

    def op(self, eng, fn, rd=(), wr=(), sig=True):
        X = self.e[eng]
        self._wait(X, self._deps(X, rd, wr, False))
        ins = fn(X.h)
        self.ninst += 1
        if sig:
            ins.then_inc(X.sem, 1)
            X.count += 1
            ev = (X.sem, X.count)
            self.log[X.name].append(("i", id(X.sem), 1, self.ninst))
        else:
            ev = (X.sem, X.count + 1)
        self._commit(ev, rd, wr)
        self.last_ev = ev
        if sig and X.count >= SEM_LIMIT:
            X.new_sem()
        return ins

    def dma(self, q, out, in_, rd=(), wr=(), chan=None, **kw):
        X = self.e[q]
        evs = self._deps(X, rd, wr, True)
        if chan.last is not None:
            evs.append(chan.last)
        self._wait(X, evs)
        if chan.count >= SEM_LIMIT:
            chan.n += 1
            chan.sem = self.nc.alloc_semaphore(f"c_{chan.name}_{chan.n}")
            chan.count = 0
        ins = X.h.dma_start(out=out, in_=in_, **kw)
        ins.then_inc(chan.sem, 16)
        chan.count += 16
        self.log[X.name].append(("i", id(chan.sem), 16, self.ninst))
        ev = (chan.sem, chan.count)
        chan.last = ev
        self._commit(ev, rd, wr)
        self.ninst += 1
        return ev

    def fence(self, bufs, scratch):
        bufs = [b for b in bufs]
        self.op("dve", lambda e: e.memset(scratch, 0.0), rd=tuple(bufs), wr=tuple(bufs))
        Buf.EPOCH = self.last_ev

    def check_deadlock(self):
        val = {}
        pos = {k: 0 for k in self.log}
        progress = True
        while progress:
            progress = False
            for k, lg in self.log.items():
                while pos[k] < len(lg):
                    t, sem, v, n = lg[pos[k]]
                    if t == "w":
                        if val.get(sem, 0) >= v:
                            pos[k] += 1
                            progress = True
                        else:
                            break
                    else:
                        val[sem] = val.get(sem, 0) + v
                        pos[k] += 1
                        progress = True
        stuck = {k: (pos[k], len(lg), lg[pos[k]] if pos[k] < len(lg) else None) for k, lg in self.log.items()}
        ok = all(pos[k] == len(lg) for k, lg in self.log.items())
        return ok, stuck, val

    def wait_event(self, eng, ev):
        self._wait(self.e[eng], [ev])


def build(layers=(0, 1, 2, 3), dbg=None, skip=""):
    nc = bass.Bass("TRN2", target_bir_lowering=False)
    S = Sched(nc)
    Buf.EPOCH = None
    Buf.REG = []
    dt_in = lambda name, shape: nc.dram_tensor(name, list(shape), F32, kind="ExternalInput").ap()
    x_in = dt_in("x", (SEQ, D))
    c_in = dt_in("c", (1, D))
    rel_bias = dt_in("rel_bias", (32, 12))
    w_in = dt_in("w_in", (DEPTH, D, 3072))
    w_o = dt_in("w_o", (DEPTH, D, D))
    lam_q1 = dt_in("lam_q1", (DEPTH, 64))
    lam_k1 = dt_in("lam_k1", (DEPTH, 64))
    lam_q2 = dt_in("lam_q2", (DEPTH, 64))
    lam_k2 = dt_in("lam_k2", (DEPTH, 64))
    subln_g = dt_in("subln_g", (DEPTH, 128))
    w_ada = dt_in("w_ada", (DEPTH, D, 6 * D))
    b_ada = dt_in("b_ada", (DEPTH, 6 * D))
    ln1_g = dt_in("ln1_g", (DEPTH, D))
    ln1_b = dt_in("ln1_b", (DEPTH, D))
    ln2_g = dt_in("ln2_g", (DEPTH, D))
    ln2_b = dt_in("ln2_b", (DEPTH, D))
    w_router = dt_in("w_router", (D, NE))
    b_router = dt_in("b_router", (1, NE))
    w_gate = dt_in("w_gate", (DEPTH, NE, D, DFF))
    w_up = dt_in("w_up", (DEPTH, NE, D, DFF))
    w_down = dt_in("w_down", (DEPTH, NE, DFF, D))
    c_oh = dt_in("c_oh", (2, 32, WF))
    c_add = dt_in("c_add", (12, WF))
    c_ident = dt_in("c_ident", (128, 128))
    c_flip = dt_in("c_flip", (128, 128))
    out = nc.dram_tensor("out", [SEQ, D], F32, kind="ExternalOutput").ap()
    xs_a = nc.dram_tensor("xs_a", [SEQ, D], F32, kind=("ExternalOutput" if dbg else "Internal")).ap()
    xs_b = nc.dram_tensor("xs_b", [SEQ, D], F32, kind="Internal").ap()
    f_dram = nc.dram_tensor("f_dram", [12, WF], F32, kind="Internal")

    sb = lambda name, shape, dt: nc.alloc_sbuf_tensor(name, list(shape), dt).ap()
    PS = [nc.alloc_psum_tensor(f"ps{i}", [128, 512], F32).ap() for i in range(8)]
    PSB = [Buf() for _ in range(8)]

    ident_f = sb("ident_f", (128, 128), F32)
    ident_b = sb("ident_b", (128, 128), BF16)
    flip_b = sb("flip_b", (128, 128), BF16)
    ones_f = sb("ones_f", (128, 128), F32)
    zeros_b = sb("zeros_b", (128, 512), BF16)
    modT = sb("modT", (128, DEPTH, 48), F32)
    sc1p = sb("sc1p", (128, DEPTH, 8), F32)
    sc2p = sb("sc2p", (128, DEPTH, 8), F32)
    neg_lam = sb("neg_lam", (128, DEPTH), F32)
    gsub = sb("gsub", (128, DEPTH, 128), F32)
    wr_b = sb("wr_b", (128, 8, NE), BF16)
    br_b = sb("br_b", (128, NE), F32)
    fscr = sb("fscr", (128, 1), F32)
    lnc = sb("lnc", (128, 4), F32)
    B_const = Buf()

    ch_const = Chan(S, "const")
    ch_constp = Chan(S, "constp")

    def cdma(out_ap, in_ap, q="sp", **kw):
        return S.dma(q, out_ap, in_ap, rd=(), wr=(B_const,), chan=(ch_const if q == "sp" else ch_constp), **kw)

    with nc.sbuf_tensor("pro_region", [128, 128 * 1024 // 4], F32) as pro_h, \
            nc.sbuf_tensor("cTb", [128, 8], BF16) as cTb_h, \
            nc.sbuf_tensor("wblk", [128, 4, 8, 512], BF16) as wblk_h:
        pro = pro_h.ap()
        off = [0]

        def carve(n_f32):
            a = pro[:, off[0]:off[0] + n_f32]
            off[0] += n_f32
            return a

        cdma(ident_f, c_ident)
        cdma(ident_b, c_ident, q="pool")
        cdma(flip_b, c_flip, q="pool")
        S.op("dve", lambda e: e.memset(ones_f, 1.0), wr=(B_const,))
        S.op("dve", lambda e: e.memset(zeros_b, 0.0), wr=(B_const,))
        cdma(wr_b, w_router.rearrange("(c p) n -> p c n", p=128), q="pool")
        cdma(br_b, b_router.partition_broadcast(128))
        cdma(lnc, rel_bias[31:32, 8:12].rearrange("o h -> (o h)").partition_broadcast(128))
        c_rows = carve(128)[0:8, :]
        cdma(c_rows, c_in.rearrange("o (j p) -> (o j) p", p=128))
        ca_rows = carve(128)[0:8, :]
        S.op("act", lambda e: e.activation(out=ca_rows, in_=c_rows, func=AF.Silu), rd=(B_const,), wr=(B_const,))
        cT = carve(8)
        S.op("pe", lambda e: e.matmul(PS[0][:, 0:8], lhsT=ca_rows, rhs=ident_f[0:8, 0:8], start=True, stop=True),
             rd=(B_const,), wr=(PSB[0],))
        S.op("dve", lambda e: e.tensor_copy(out=cT, in_=PS[0][:, 0:8]), rd=(PSB[0],), wr=(B_const,))
        cTb_t = cTb_h.ap()
        S.op("dve", lambda e: e.tensor_copy(out=cTb_t, in_=cT), rd=(B_const,), wr=(B_const,))
        brow = carve(6 * D)[0:1, :]
        mrow = carve(6 * D)[0:1, :]
        rowB = Buf()
        wblk_t = wblk_h.ap()
        wblkB = [Buf() for _ in range(4)]
        wch = [Chan(S, f"wb{i}") for i in range(4)]
        it = 0
        for l in range(DEPTH):
            cdma(brow, b_ada[l:l + 1, :])
            for cb in range(12):
                s_ = it % 4
                it += 1
                S.dma("pool", wblk_t[:, s_], w_ada[l, :, cb * 512:(cb + 1) * 512].rearrange("(c p) n -> p c n", p=128),
                      wr=(wblkB[s_],), chan=wch[s_])
                bk = 3 + (cb % 2)
                for kc in range(8):
                    S.op("pe", lambda e, s_=s_, kc=kc, bk=bk: e.matmul(
                        PS[bk][0:1, :], lhsT=cTb_t[:, kc:kc + 1], rhs=wblk_t[:, s_, kc, :], start=(kc == 0), stop=(kc == 7)),
                        rd=(wblkB[s_], B_const), wr=(PSB[bk],), sig=(kc == 7))
                S.op("dve", lambda e, cb=cb, bk=bk: e.tensor_tensor(
                    out=mrow[:, cb * 512:(cb + 1) * 512], in0=PS[bk][0:1, :], in1=brow[:, cb * 512:(cb + 1) * 512], op=ALU.add),
                    rd=(PSB[bk], B_const), wr=(rowB,))
            for j in range(48):
                S.op("pe", lambda e, j=j, l=l: e.matmul(
                    PS[2][:, l * 48 + j:l * 48 + j + 1], lhsT=mrow[:, j * 128:(j + 1) * 128], rhs=ones_f[0:1, 0:1],
                    start=True, stop=True), rd=(rowB, B_const), wr=(PSB[2],), sig=(j == 47))
        modT_flat = modT.rearrange("p l j -> p (l j)")
        S.op("dve", lambda e: e.tensor_copy(out=modT_flat, in_=PS[2][:, 0:192]), rd=(PSB[2],), wr=(B_const,))
        S.op("dve", lambda e: e.tensor_scalar(out=sc1p, in0=modT[:, :, 8:16], scalar1=1.0, scalar2=None, op0=ALU.add),
             rd=(B_const,), wr=(B_const,))
        S.op("dve", lambda e: e.tensor_scalar(out=sc2p, in0=modT[:, :, 32:40], scalar1=1.0, scalar2=None, op0=ALU.add),
             rd=(B_const,), wr=(B_const,))
        lam_t = [carve(DEPTH * 64) for _ in range(4)]
        for t_, src in zip(lam_t, (lam_q1, lam_k1, lam_q2, lam_k2)):
            cdma(t_, src.rearrange("l e -> (l e)").partition_broadcast(128))
        prod = carve(DEPTH * 64)
        ssum = [carve(DEPTH), carve(DEPTH)]
        for i in range(2):
            S.op("dve", lambda e, i=i: e.tensor_tensor(out=prod, in0=lam_t[2 * i], in1=lam_t[2 * i + 1], op=ALU.mult),
                 rd=(B_const,), wr=(B_const,))
            S.op("dve", lambda e, i=i: e.tensor_reduce(out=ssum[i], in_=prod.rearrange("p (l e) -> p l e", l=DEPTH),
                                                    axis=AX.X, op=ALU.add), rd=(B_const,), wr=(B_const,))
            S.op("act", lambda e, i=i: e.activation(out=ssum[i], in_=ssum[i], func=AF.Exp), rd=(B_const,), wr=(B_const,))
        S.op("dve", lambda e: e.tensor_tensor(out=neg_lam, in0=ssum[1], in1=ssum[0], op=ALU.subtract),
             rd=(B_const,), wr=(B_const,))
        cdma(gsub, subln_g.rearrange("l e -> (l e)").partition_broadcast(128))
        for l in range(DEPTH):
            lam_init = 0.8 - 0.6 * math.exp(-0.3 * l)
            S.op("dve", lambda e, l=l, li=lam_init: e.tensor_scalar(out=neg_lam[:, l:l + 1], in0=neg_lam[:, l:l + 1],
                                                                  scalar1=-li, scalar2=None, op0=ALU.add),
                 rd=(B_const,), wr=(B_const,))
            S.op("dve", lambda e, l=l, li=lam_init: e.tensor_scalar(out=gsub[:, l, :], in0=gsub[:, l, :],
                                                                  scalar1=1.0 - li, scalar2=None, op0=ALU.mult),
                 rd=(B_const,), wr=(B_const,))
        rb = carve(12)[0:32, :]
        cdma(rb, rel_bias)
        oh_sb = carve(2 * WF).rearrange("p (k m) -> p k m", k=2)[0:32]
        cdma(oh_sb, c_oh.rearrange("k b m -> b k m"))
        add_sb = carve(WF)[0:12, :]
        cdma(add_sb, c_add)
        f_sb = carve(WF)[0:12, :]
        fd_sb = carve(WF)[0:12, :]
        for kind, dst in ((0, f_sb), (1, fd_sb)):
            for cc in range(0, WF, 512):
                n = min(512, WF - cc)
                bk = 3 + (cc // 512) % 2
                S.op("pe", lambda e, kind=kind, cc=cc, n=n, bk=bk: e.matmul(
                    PS[bk][0:12, 0:n], lhsT=rb, rhs=oh_sb[:, kind, cc:cc + n], start=True, stop=True),
                    rd=(B_const,), wr=(PSB[bk],))
                S.op("dve", lambda e, dst=dst, cc=cc, n=n, bk=bk: e.tensor_tensor(
                    out=dst[:, cc:cc + n], in0=PS[bk][0:12, 0:n], in1=add_sb[:, cc:cc + n], op=ALU.add),
                    rd=(PSB[bk], B_const), wr=(B_const,))
        S.op("act", lambda e: e.activation(out=f_sb, in_=f_sb, func=AF.Exp), rd=(B_const,), wr=(B_const,))
        S.op("act", lambda e: e.activation(out=fd_sb, in_=fd_sb, func=AF.Exp), rd=(B_const,), wr=(B_const,))
        B_f = Buf()
        ch_f = Chan(S, "fdram")
        fd_ap = f_dram.ap()
        S.dma("sp", fd_ap[0:8, :], f_sb[0:8, :], rd=(B_const,), wr=(B_f,), chan=ch_f)
        S.dma("sp", fd_ap[8:12, :], fd_sb[8:12, :], rd=(B_const,), wr=(B_f,), chan=ch_f)
        S.fence(list(Buf.REG), fscr)

    ch_xl = [Chan(S, f"xl{i}") for i in range(4)]
    ch_xs = [Chan(S, f"xs{i}") for i in range(4)]
    ch_w = [Chan(S, f"w{i}") for i in range(8)]
    ch_bc = Chan(S, "bc")

    def bcast_rows(dst, l, col0, tmp, tmpB):
        for j in range(8):
            bk = 6 + (j % 2)
            S.op("dve", lambda e, j=j: e.tensor_scalar(out=tmp, in0=ident_f, scalar1=modT[:, l, col0 + j:col0 + j + 1],
                                                     scalar2=None, op0=ALU.mult), rd=(B_const,), wr=(tmpB,))
            S.op("pe", lambda e, bk=bk: e.matmul(PS[bk][:, 0:128], lhsT=ones_f, rhs=tmp, start=True, stop=True),
                 rd=(tmpB, B_const), wr=(PSB[bk],))
            S.op("dve", lambda e, j=j, bk=bk: e.tensor_copy(out=dst[:, j * 128:(j + 1) * 128], in_=PS[bk][:, 0:128]),
                 rd=(PSB[bk],), wr=(tmpB,))

    def ln_tail(y, stats, mv, rstd, g_b, b_b, yB, dst_dram, ch, eps_t, dstB, lnB, use_pool=False, act_norm=True):
        for hh in range(2):
            S.op("dve", lambda e, hh=hh: e.bn_stats(out=stats[:, hh, :], in_=y[:, hh * 512:(hh + 1) * 512]),
                 rd=(yB,), wr=(yB,))
        S.op("dve", lambda e: e.bn_aggr(out=mv, in_=stats.rearrange("p a b -> p (a b)")), rd=(yB,), wr=(yB,))
        S.op("act", lambda e: e.activation(out=rstd, in_=mv[:, 1:2], func=AF.Ln, bias=eps_t, scale=1.0), rd=(yB, B_const), wr=(yB,))
        S.op("act", lambda e: e.activation(out=rstd, in_=rstd, func=AF.Exp, scale=-0.5), rd=(yB,), wr=(yB,))
        if act_norm:
            S.op("dve", lambda e: e.tensor_scalar(out=mv[:, 1:2], in0=mv[:, 0:1], scalar1=rstd, scalar2=-1.0, op0=ALU.mult, op1=ALU.mult),
                 rd=(yB,), wr=(yB,))
            S.op("act", lambda e: e.activation(out=y, in_=y, func=AF.Identity, bias=mv[:, 1:2], scale=rstd), rd=(yB,), wr=(yB,))
        else:
            S.op("dve", lambda e: e.tensor_scalar(out=y, in0=y, scalar1=mv[:, 0:1], scalar2=rstd, op0=ALU.subtract, op1=ALU.mult),
                 rd=(yB,), wr=(yB,))
        eng = "pool" if use_pool else "dve"
        S.op(eng, lambda e: e.tensor_tensor(out=y, in0=y, in1=g_b, op=ALU.mult), rd=(yB, lnB), wr=(yB,))
        S.op(eng, lambda e: e.tensor_tensor(out=y, in0=y, in1=b_b, op=ALU.add), rd=(yB, lnB), wr=(yB,))
        S.dma("sp", dst_dram, y, rd=(yB,), wr=(dstB,), chan=ch)

    eps_t = sb("eps_t", (128, 1), F32)
    S.op("dve", lambda e: e.memset(eps_t, LN_EPS), wr=(B_const,))
    neghalf = sb("neghalf", (128, 8), F32)
    S.op("dve", lambda e: e.memset(neghalf, -0.5), wr=(B_const,))

    def stream_bufs(li, n_layers):
        src = x_in if li == 0 else xs_b
        dst = out if li == n_layers - 1 else xs_b
        return src, xs_a, dst

    B_xmid = [Buf() for _ in range(NT)]
    B_xdst = [Buf() for _ in range(NT)]

    for li, l in enumerate(layers):
        x_src, x_mid, x_dst = stream_bufs(li, len(layers))
        reg0 = len(Buf.REG)
        if "A" in skip:
            pass
        else:
          with nc.sbuf_tensor(f"hT{li}", [128, 8, SEQ], BF16) as hT_h, \
                nc.sbuf_tensor(f"rOT{li}", [128, 8, SEQ], BF16) as OT_h:
            hT = hT_h.ap()
            OT = OT_h.ap()
            hTB = [[Buf() for _ in range(NW)] for _ in range(8)]
            OTB = [[Buf() for _ in range(NW)] for _ in range(8)]
            with nc.sbuf_tensor(f"xst{li}", [128, 2, 4, D], F32) as xst_h:
                otf = OT.rearrange("p c t -> p (c t)")
                xbf = [otf[:, i * 4096:(i + 1) * 4096].rearrange("p (s d) -> p s d", s=4) for i in range(2)]
                xstage = [xst_h.ap()[:, i] for i in range(2)]
                xsB = [Buf(), Buf()]
                xbB = [Buf(), Buf()]
                for tw in range(NW):
                    s = tw % 2
                    S.dma("sp", xstage[s], x_src[tw * 512:(tw + 1) * 512, :].rearrange("(s p) d -> p s d", p=128),
                          rd=tuple(B_xdst[tw * 4:tw * 4 + 4]), wr=(xsB[s],), chan=ch_xl[s])
                    for ss in range(4):
                        S.op("dve", lambda e, s=s, ss=ss: e.tensor_copy(out=xbf[s][:, ss, :], in_=xstage[s][:, ss, :]),
                             rd=(xsB[s],), wr=(xbB[s],))
                    for c in range(8):
                        bk = 6 + (c % 2)
                        for ss in range(4):
                            S.op("pe", lambda e, s=s, ss=ss, c=c, bk=bk: e.matmul(
                                PS[bk][:, ss * 128:(ss + 1) * 128], lhsT=xbf[s][:, ss, c * 128:(c + 1) * 128],
                                rhs=ident_b, start=True, stop=True), rd=(xbB[s], B_const), wr=(PSB[bk],), sig=(ss == 3))
                        S.op("act", lambda e, c=c, tw=tw, bk=bk: e.activation(
                            out=hT[:, c, tw * 512:(tw + 1) * 512], in_=PS[bk], func=AF.Identity,
                            bias=modT[:, l, c:c + 1], scale=sc1p[:, l, c:c + 1]),
                            rd=(PSB[bk], B_const), wr=(hTB[c][tw],))
                S.fence(Buf.REG[reg0:], fscr)
            with nc.sbuf_tensor(f"grp{li}", [128, 4096 * 3 + 32 * 130 + 4 * W_STRIP + 2 * 8 * 384 + 6 * 512 + 4 * 128], BF16) as G_h, \
                    nc.sbuf_tensor(f"accS{li}", [128, 1040], F32) as accS_h, \
                    nc.sbuf_tensor(f"sml{li}", [128, 64], F32) as sml_h:
                G = G_h.ap()
                go = [0]

                def gcarve(n):
                    a = G[:, go[0]:go[0] + n]
                    go[0] += n
                    return a

                KT = gcarve(4096)
                QZ = [gcarve(4096), gcarve(4096)]
                Vt = gcarve(32 * 130)
                strip = [gcarve(W_STRIP), gcarve(W_STRIP)]
                stripH = [gcarve(W_STRIP), gcarve(W_STRIP)]
                stripHB = [Buf(), Buf()]
                Wg = [gcarve(8 * 384).rearrange("p (c n) -> p c n", c=8) for _ in range(2)]
                PT = [gcarve(512) for _ in range(6)]
                Otok = gcarve(4 * 128).rearrange("p (s n) -> p s n", s=4)
                KTB = [Buf() for _ in range(NW)]
                QZB = [[Buf() for _ in range(NW)] for _ in range(2)]
                VB = [Buf() for _ in range(NW)]
                stripB = [Buf(), Buf()]
                WgB = [Buf(), Buf()]
                PTB = [Buf() for _ in range(6)]
                accS = accS_h.ap()
                accSB = Buf()
                OtokB = Buf()
                small = sml_h.ap()
                smallB = Buf()
                S.op("dve", lambda e: e.memset(QZ[0][64:128, :], 0.0), wr=tuple(QZB[0]))
                S.op("dve", lambda e: e.memset(QZ[1][0:64, :], 0.0), wr=tuple(QZB[1]))

                def load_group_weights(g, slot):
                    if g < 4:
                        cols = (g * 128, 512 + g * 128, 1024 + g * 128)
                    else:
                        hh = g - 4
                        cols = (1536 + hh * 128, 2048 + hh * 128, 2560 + hh * 128)
                    for i, c0 in enumerate(cols):
                        S.dma("pool", Wg[slot][:, :, i * 128:(i + 1) * 128],
                              w_in[l, :, c0:c0 + 128].rearrange("(c p) n -> p c n", p=128),
                              wr=(WgB[slot],), chan=ch_w[slot * 3 + i] if slot == 0 else ch_w[3 + i])

                load_group_weights(0, 0)
                pt_i = [0]
                st_i = [0]
                pending_tr = []
                bg = []
                for g in range(8):
                    slot = g % 2
                    dil = g < 4
                    if g + 1 < 8:
                        load_group_weights(g + 1, 1 - slot)
                    heads = (2 * g, 2 * g + 1) if dil else (8 + g - 4,)
                    for u, hd in enumerate(heads):
                        S.dma("pool", stripH[u], bass.AP(f_dram, hd * WF, [[1, 128], [1, W_STRIP]]),
                              rd=(B_f,), wr=(stripHB[u],), chan=ch_w[6 + u], max_dma_last_dim=4352)
                    EV = 65 if dil else 129
                    Vv = Vt[:, 0:32 * 130].rearrange("p (t n) -> p t n", t=32) if dil else \
                        Vt[:, 0:32 * 129].rearrange("p (t n) -> p t n", t=32)
                    if dil:
                        S.op("dve", lambda e, Vv=Vv: e.memset(Vv[:, :, 64:65], 1.0), wr=tuple(VB))
                        S.op("dve", lambda e, Vv=Vv: e.memset(Vv[:, :, 129:130], 1.0), wr=tuple(VB))
                    else:
                        S.op("dve", lambda e, Vv=Vv: e.memset(Vv[:, :, 128:129], 1.0), wr=tuple(VB))
                    for tw in range(NW):
                        tsl = slice(tw * 512, (tw + 1) * 512)
                        for _ in range(3):
                            if bg:
                                bg.pop(0)()
                        bk = 6
                        for kc in range(8):
                            S.op("pe", lambda e, kc=kc, tsl=tsl, bk=bk: e.matmul(
                                PS[bk], lhsT=Wg[slot][:, kc, 0:128], rhs=hT[:, kc, tsl], start=(kc == 0), stop=(kc == 7)),
                                rd=(WgB[slot], hTB[kc][tw]), wr=(PSB[bk],), sig=(kc == 7))
                        S.op("dve", lambda e, tsl=tsl, bk=bk: e.tensor_scalar(
                            out=QZ[0][0:64, tsl], in0=PS[bk][0:64, :], scalar1=0.125, scalar2=None, op0=ALU.mult),
                            rd=(PSB[bk],), wr=(QZB[0][tw],))
                        S.op("act", lambda e, tsl=tsl, bk=bk: e.activation(
                            out=QZ[1][64:128, tsl], in_=PS[bk][64:128, :], func=AF.Copy, scale=0.125),
                            rd=(PSB[bk],), wr=(QZB[1][tw],))
                        bk = 7
                        for kc in range(8):
                            S.op("pe", lambda e, kc=kc, tsl=tsl, bk=bk: e.matmul(
                                PS[bk], lhsT=Wg[slot][:, kc, 128:256], rhs=hT[:, kc, tsl], start=(kc == 0), stop=(kc == 7)),
                                rd=(WgB[slot], hTB[kc][tw]), wr=(PSB[bk],), sig=(kc == 7))
                        S.op("dve", lambda e, tsl=tsl, bk=bk: e.tensor_copy(out=KT[:, tsl], in_=PS[bk]),
                             rd=(PSB[bk],), wr=(KTB[tw],))
                        bk = 3 + (tw % 2)
                        for ss in range(4):
                            t = tw * 4 + ss
                            for kc in range(8):
                                S.op("pe", lambda e, kc=kc, t=t, ss=ss, bk=bk: e.matmul(
                                    PS[bk][:, ss * 128:(ss + 1) * 128], lhsT=hT[:, kc, t * 128:(t + 1) * 128],
                                    rhs=Wg[slot][:, kc, 256:384], start=(kc == 0), stop=(kc == 7)),
                                    rd=(WgB[slot], hTB[kc][tw]), wr=(PSB[bk],), sig=(kc == 7 and ss == 3))
                        pv = PS[bk].rearrange("p (s n) -> p s n", s=4)
                        if dil:
                            S.op("dve", lambda e, tw=tw, pv=pv, Vv=Vv: e.tensor_copy(
                                out=Vv[:, tw * 4:(tw + 1) * 4, 0:64], in_=pv[:, :, 0:64]), rd=(PSB[bk],), wr=(VB[tw],))
                            S.op("act", lambda e, tw=tw, pv=pv, Vv=Vv: e.activation(
                                out=Vv[:, tw * 4:(tw + 1) * 4, 65:129], in_=pv[:, :, 64:128], func=AF.Copy),
                                rd=(PSB[bk],), wr=(VB[tw],))
                        else:
                            S.op("dve", lambda e, tw=tw, pv=pv, Vv=Vv: e.tensor_copy(
                                out=Vv[:, tw * 4:(tw + 1) * 4, 0:128], in_=pv), rd=(PSB[bk],), wr=(VB[tw],))
                    for u, hd in enumerate(heads):
                        for ci, cc in enumerate(range(0, W_STRIP, 512)):
                            n = min(512, W_STRIP - cc)
                            bk = 6 + ci % 2
                            S.op("pe", lambda e, cc=cc, n=n, bk=bk, u=u: e.matmul(PS[bk][:, 0:n], lhsT=flip_b, rhs=stripH[u][:, cc:cc + n],
                                                                               start=True, stop=True),
                                 rd=(stripHB[u], B_const), wr=(PSB[bk],))
                            S.op("dve", lambda e, cc=cc, n=n, bk=bk, u=u: e.tensor_copy(out=strip[u][:, cc:cc + n], in_=PS[bk][:, 0:n]),
                                 rd=(PSB[bk],), wr=(stripB[u],))
                    if True:
                        if dil:
                            accb = [3, 4]
                            reg = lambda u, i: PS[3 + u][:, i * 65:(i + 1) * 65]
                            regb = lambda u, i: PSB[3 + u]
                        else:
                            accb = [3, 4, 5]
                            reg = lambda u, i: PS[3 + (u * 4 + i) // 3][:, ((u * 4 + i) % 3) * 129:((u * 4 + i) % 3 + 1) * 129]
                            regb = lambda u, i: PSB[3 + (u * 4 + i) // 3]
                        items = []
                        for qw in range(NW):
                            kts = list(range(max(0, 4 * qw - 16), 4 * qw + 4)) if dil else list(range(0, 4 * qw + 4))
                            w_items = []
                            for kt in kts:
                                for u in range(2):
                                    q_lo = max(kt, 4 * qw)
                                    q_hi = min(4 * qw + 3, kt + 16) if dil else 4 * qw + 3
                                    if q_hi - q_lo + 1 > 0:
                                        w_items.append([qw, kt, u, q_lo, q_hi, False, False])
                            w_items[0][5] = True
                            w_items[-1][6] = True
                            items += w_items

                        def emit_st(it):
                            qw, kt, u, q_lo, q_hi, _, _ = it
                            nv = q_hi - q_lo + 1
                            N = nv * 128
                            offq = (q_lo - kt) * 128
                            offs = min(offq, W_STRIP - N)
                            sb_ = (0, 1, 2, 7)[st_i[0] % 4]
                            st_i[0] += 1
                            su = u if dil else 0
                            S.op("pe", lambda e: e.matmul(
                                PS[sb_][:, 0:N], lhsT=KT[:, kt * 128:(kt + 1) * 128],
                                rhs=QZ[u][:, q_lo * 128:q_lo * 128 + N], start=True, stop=True),
                                rd=(KTB[kt // 4],) + tuple(QZB[u][q_lo // 4:q_hi // 4 + 1]), wr=(PSB[sb_],))
                            pi = pt_i[0] % 6
                            pt_i[0] += 1
                            if (not dil) and (q_lo - kt) >= 13:
                                S.op("act", lambda e: e.activation(
                                    out=PT[pi][:, 0:N], in_=PS[sb_][:, 0:N], func=AF.Exp, bias=lnc[:, g - 4:g - 3]),
                                    rd=(PSB[sb_], B_const), wr=(PTB[pi],))
                                return pi
                            S.op("act", lambda e: e.activation(
                                out=PT[pi][:, 0:N], in_=PS[sb_][:, 0:N], func=AF.Exp), rd=(PSB[sb_],), wr=(PTB[pi],))
                            S.op("dve", lambda e: e.tensor_tensor(
                                out=PT[pi][:, 0:N], in0=PT[pi][:, 0:N], in1=strip[su][:, offs:offs + N], op=ALU.mult),
                                rd=(PTB[pi], stripB[su]), wr=(PTB[pi],))
                            return pi

                        def emit_pv(it, pi):
                            qw, kt, u, q_lo, q_hi, first, last = it
                            if first:
                                for b_ in accb:
                                    S.op("pe", lambda e, b_=b_: e.matmul(PS[b_], lhsT=zeros_b[:, 0:128], rhs=zeros_b,
                                                                        start=True, stop=True, skip_group_check=True),
                                         rd=(B_const,), wr=(PSB[b_],))
                            nv = q_hi - q_lo + 1
                            for i in range(nv):
                                qt = q_lo + i
                                qi = qt - 4 * qw
                                vr = Vv[:, kt, u * 65:(u + 1) * 65] if dil else Vv[:, kt, 0:129]
                                S.op("pe", lambda e, i=i, qi=qi, vr=vr, qt=qt: e.matmul(
                                    reg(u, qi), lhsT=PT[pi][:, i * 128:(i + 1) * 128], rhs=vr,
                                    start=False, stop=(kt == qt), skip_group_check=True),
                                    rd=(PTB[pi], VB[kt // 4]), wr=(regb(u, qi),), sig=(i == nv - 1))
                            if last:
                                win_end(qw)

                        def win_end(qw):
                            while bg:
                                bg.pop(0)()
                            if dil:
                                for u in range(2):
                                    if u == 0:
                                        S.op("dve", lambda e, u=u: e.tensor_copy(out=accS[:, u * 260:(u + 1) * 260], in_=PS[3 + u][:, 0:260]),
                                             rd=(PSB[3 + u],), wr=(accSB,))
                                    else:
                                        S.op("act", lambda e, u=u: e.activation(out=accS[:, u * 260:(u + 1) * 260], in_=PS[3 + u][:, 0:260],
                                                                                func=AF.Copy), rd=(PSB[3 + u],), wr=(accSB,))
                                a3 = accS[:, 0:520].rearrange("p (r n) -> p r n", r=8)
                                rr = small[:, 0:8]
                                bg.append(lambda: S.op("dve", lambda e: e.reciprocal(out=rr.unsqueeze(2), in_=a3[:, :, 64:65]),
                                                       rd=(accSB,), wr=(smallB,)))
                                for u in range(2):
                                    bg.append(lambda u=u: S.op("dve", lambda e: e.tensor_tensor(
                                        out=Otok[:, :, u * 64:(u + 1) * 64], in0=a3[:, u * 4:(u + 1) * 4, 0:64],
                                        in1=rr[:, u * 4:(u + 1) * 4].unsqueeze(2).to_broadcast([128, 4, 64]), op=ALU.mult),
                                        rd=(accSB, smallB), wr=(OtokB,)))
                            else:
                                a3 = accS[:, 0:1032].rearrange("p (r n) -> p r n", r=8)
                                for b_ in range(3):
                                    nr = 3 if b_ < 2 else 2
                                    src = PS[3 + b_][:, 0:nr * 129].rearrange("p (r n) -> p r n", r=nr)
                                    dst = a3[:, 3 * b_:3 * b_ + nr, :]
                                    if b_ == 1:
                                        S.op("act", lambda e, src=src, dst=dst: e.activation(out=dst, in_=src, func=AF.Copy),
                                             rd=(PSB[3 + b_],), wr=(accSB,))
                                    else:
                                        S.op("dve", lambda e, src=src, dst=dst: e.tensor_copy(out=dst, in_=src),
                                             rd=(PSB[3 + b_],), wr=(accSB,))
                                rr = small[:, 0:8]
                                r2n = small[:, 8:12]
                                ss_ = small[:, 12:16]
                                o1 = a3[:, 0:4, 0:128]
                                o2 = a3[:, 4:8, 0:128]
                                A = lambda fn, extra=(): S.op("dve", fn, rd=(accSB, smallB) + extra, wr=(accSB, smallB))
                                bg.append(lambda: A(lambda e: e.reciprocal(out=rr.unsqueeze(2), in_=a3[:, :, 128:129])))
                                bg.append(lambda: A(lambda e: e.tensor_scalar(out=r2n, in0=rr[:, 4:8], scalar1=neg_lam[:, l:l + 1], scalar2=None,
                                                                              op0=ALU.mult), (B_const,)))
                                bg.append(lambda: A(lambda e: e.tensor_tensor(out=o2, in0=o2, in1=r2n.unsqueeze(2).to_broadcast([128, 4, 128]), op=ALU.mult)))
                                bg.append(lambda: A(lambda e: e.tensor_tensor(out=o1, in0=o1, in1=rr[:, 0:4].unsqueeze(2).to_broadcast([128, 4, 128]), op=ALU.mult)))
                                bg.append(lambda: A(lambda e: e.tensor_tensor(out=o1, in0=o1, in1=o2, op=ALU.add)))
                                bg.append(lambda: A(lambda e: e.tensor_tensor(out=o2, in0=o1, in1=o1, op=ALU.mult)))
                                bg.append(lambda: A(lambda e: e.tensor_reduce(out=ss_, in_=o2, axis=AX.X, op=ALU.add)))

                                def rstd_pool():
                                    S.op("pool", lambda e: e.tensor_scalar(out=ss_, in0=ss_, scalar1=1.0 / 128, scalar2=LN_EPS, op0=ALU.mult, op1=ALU.add),
                                         rd=(smallB,), wr=(smallB,))
                                    S.op("pool", lambda e: e.tensor_tensor(out=ss_, in0=ss_, in1=neghalf[:, 0:4], op=ALU.pow),
                                         rd=(smallB, B_const), wr=(smallB,))
                                bg.append(rstd_pool)
                                bg.append(lambda: None)
                                bg.append(lambda: None)
                                bg.append(lambda: A(lambda e: e.tensor_tensor(out=o1, in0=o1, in1=ss_.unsqueeze(2).to_broadcast([128, 4, 128]), op=ALU.mult)))
                                bg.append(lambda: S.op("dve", lambda e: e.tensor_tensor(
                                    out=Otok, in0=o1, in1=gsub[:, l, :].unsqueeze(1).to_broadcast([128, 4, 128]), op=ALU.mult),
                                    rd=(accSB, B_const), wr=(OtokB,)))

                            def tr_out(g=g, qw=qw):
                                bk = 6
                                for qi in range(4):
                                    S.op("pe", lambda e, qi=qi, bk=bk: e.matmul(
                                        PS[bk][:, qi * 128:(qi + 1) * 128], lhsT=Otok[:, qi, :], rhs=ident_b, start=True, stop=True),
                                        rd=(OtokB, B_const), wr=(PSB[bk],), sig=(qi == 3))
                                S.op("act", lambda e, bk=bk: e.activation(out=OT[:, g, qw * 512:(qw + 1) * 512], in_=PS[bk], func=AF.Copy),
                                     rd=(PSB[bk],), wr=(OTB[g][qw], xbB[0], xbB[1]))
                            bg.append(lambda: None)
                            bg.append(lambda: None)
                            bg.append(tr_out)

                        LOOK = 4
                        pis = []
                        for idx in range(len(items) + LOOK):
                            if idx < len(items):
                                pis.append(emit_st(items[idx]))
                                if bg:
                                    bg.pop(0)()
                            if idx - LOOK >= 0:
                                emit_pv(items[idx - LOOK], pis[idx - LOOK])
                    if g == 7:
                        while bg:
                            bg.pop(0)()
                    while pending_tr:
                        pending_tr.pop(0)()
                S.fence(Buf.REG[reg0:], fscr)
            with nc.sbuf_tensor(f"wo{li}", [128, 8, D], BF16) as Wo_h, \
                    nc.sbuf_tensor(f"wk4{li}", [128, 11264], F32) as wk4_h:
                Wo = Wo_h.ap()
                work = wk4_h.ap()
                WoB = Buf()
                for kc in range(8):
                    S.dma("pool", Wo[:, kc, :], w_o[l, kc * 128:(kc + 1) * 128, :], wr=(WoB,), chan=ch_w[kc % 6])
                g_b = work[:, 0:1024]
                lg_b = work[:, 1024:2048]
                lb_b = work[:, 2048:3072]
                tmpd = work[:, 3072:3200]
                bcB = Buf()
                bcast_rows(g_b, l, 16, tmpd, bcB)
                S.dma("sp", lg_b, ln1_g[l:l + 1, :].rearrange("o d -> (o d)").partition_broadcast(128), wr=(bcB,), chan=ch_bc)
                S.dma("sp", lb_b, ln1_b[l:l + 1, :].rearrange("o d -> (o d)").partition_broadcast(128), wr=(bcB,), chan=ch_bc)
                ys = [work[:, 4096 + i * 1024:4096 + (i + 1) * 1024] for i in range(3)]
                ysB = [Buf() for _ in range(3)]
                t1s = [work[:, 8192 + i * 1024:8192 + (i + 1) * 1024] for i in range(2)]
                t1B = [Buf() for _ in range(2)]
                st_ = [work[:, 10240 + i * 32:10240 + i * 32 + 12].rearrange("p (a b) -> p a b", a=2) for i in range(3)]
                mv_ = [work[:, 10400 + i * 8:10400 + i * 8 + 2] for i in range(3)]
                rs_ = [work[:, 10440 + i * 8:10440 + i * 8 + 1] for i in range(3)]
                def ld_x(t):
                    S.dma("sp", ys[t % 3], x_src[t * 128:(t + 1) * 128, :], rd=(B_xdst[t],), wr=(ysB[t % 3],), chan=ch_xl[t % 3])
                ld_x(0)
                ld_x(1)
                for t in range(NT):
                    s3 = t % 3
                    s2 = t % 2
                    if t + 2 < NT:
                        ld_x(t + 2)
                    for hh in range(2):
                        bk = (t % 2) * 2 + hh
                        for kc in range(8):
                            S.op("pe", lambda e, kc=kc, t=t, hh=hh, bk=bk: e.matmul(
                                PS[bk], lhsT=OT[:, kc, t * 128:(t + 1) * 128], rhs=Wo[:, kc, hh * 512:(hh + 1) * 512],
                                start=(kc == 0), stop=(kc == 7)), rd=(OTB[kc][t // 4], WoB), wr=(PSB[bk],), sig=(kc == 7))
                        S.op("dve", lambda e, hh=hh, bk=bk, s2=s2: e.tensor_tensor(
                            out=t1s[s2][:, hh * 512:(hh + 1) * 512], in0=PS[bk], in1=g_b[:, hh * 512:(hh + 1) * 512], op=ALU.mult),
                            rd=(PSB[bk], bcB), wr=(t1B[s2],))
                    S.op("dve", lambda e, s3=s3, s2=s2: e.scalar_tensor_tensor(
                        out=ys[s3], in0=ys[s3], scalar=ALPHA, in1=t1s[s2], op0=ALU.mult, op1=ALU.add),
                        rd=(ysB[s3], t1B[s2]), wr=(ysB[s3],))
                    ln_tail(ys[s3], st_[s3], mv_[s3], rs_[s3], lg_b, lb_b, ysB[s3], x_mid[t * 128:(t + 1) * 128, :], ch_xs[s3], eps_t,
                            B_xmid[t], bcB, use_pool=True)
                S.fence(Buf.REG[reg0:], fscr)
        reg0 = len(Buf.REG)
        if "B" in skip:
            pass
        else:
          with nc.sbuf_tensor(f"h2T{li}", [128, 2, 8, 1024], BF16) as h2_h, \
                nc.sbuf_tensor(f"acc{li}", [128, 2, 8, D], F32) as acc_h, \
                nc.sbuf_tensor(f"ew{li}", [128, 2, 3, 4096], BF16) as ew_h, \
                nc.sbuf_tensor(f"actb{li}", [128, 2, 4, 512], BF16) as act_h, \
                nc.sbuf_tensor(f"xb2{li}", [128, 2, 1024], BF16) as xb2_h, \
                nc.sbuf_tensor(f"bw{li}", [128, 10624], F32) as bw_h:
            h2Tall = h2_h.ap()
            accall = acc_h.ap()
            ew = ew_h.ap()
            actb = act_h.ap()
            bw = bw_h.ap()
            xb2 = xb2_h.ap()
            g_b = bw[:, 0:1024]
            lg_b = bw[:, 1024:2048]
            lb_b = bw[:, 2048:3072]
            tmpd = bw[:, 3072:3200]
            bcB = Buf()
            bcast_rows(g_b, l, 40, tmpd, bcB)
            S.dma("sp", lg_b, ln2_g[l:l + 1, :].rearrange("o d -> (o d)").partition_broadcast(128), wr=(bcB,), chan=ch_bc)
            S.dma("sp", lb_b, ln2_b[l:l + 1, :].rearrange("o d -> (o d)").partition_broadcast(128), wr=(bcB,), chan=ch_bc)
            xstage = [bw[:, 4096 + i * 1024:4096 + (i + 1) * 1024] for i in range(3)]
            xsB = [Buf() for _ in range(3)]
            sgs = [bw[:, 7168 + i * 512:7168 + (i + 1) * 512] for i in range(2)]
            sgB = [Buf(), Buf()]
            rt = bw[:, 8192:8192 + 1024]
            rtB = Buf()
            st_ = [bw[:, 9216 + i * 32:9216 + i * 32 + 12].rearrange("p (a b) -> p a b", a=2) for i in range(3)]
            mv_ = [bw[:, 9344 + i * 8:9344 + i * 8 + 2] for i in range(3)]
            rs_ = [bw[:, 9376 + i * 8:9376 + i * 8 + 1] for i in range(3)]
            gates2 = [bw[:, 9472 + i * 128:9472 + (i + 1) * 128].rearrange("p (s n) -> p s n", s=8) for i in range(2)]
            gatesB = [Buf(), Buf()]
            h2B = [[Buf() for _ in range(2)] for _ in range(2)]
            accB = [[Buf() for _ in range(8)] for _ in range(2)]
            ewguB = [Buf(), Buf()]
            ewdB = [Buf(), Buf()]
            actB = [Buf(), Buf()]
            xb2B = [Buf(), Buf()]
            xctr = [0]

            def load_gu(e_, slot):
                S.dma("pool", ew[:, slot, 0, :].rearrange("p (c n) -> p c n", c=8),
                      w_gate[l, e_].rearrange("(c p) n -> p c n", p=128), wr=(ewguB[slot],), chan=ch_w[slot * 3 + 0])
                S.dma("pool", ew[:, slot, 1, :].rearrange("p (c n) -> p c n", c=8),
                      w_up[l, e_].rearrange("(c p) n -> p c n", p=128), wr=(ewguB[slot],), chan=ch_w[slot * 3 + 1])

            def load_d(e_, slot):
                S.dma("pool", ew[:, slot, 2, :].rearrange("p (c n) -> p c n", c=4),
                      w_down[l, e_].rearrange("(c p) n -> p c n", p=128), wr=(ewdB[slot],), chan=ch_w[slot * 3 + 2])

            def b1_sub(T, s):
                par = T % 2
                h2T = h2Tall[:, par]
                t = T * 8 + s
                s3 = xctr[0] % 3
                s2 = xctr[0] % 2
                xctr[0] += 1
                S.dma("sp", xstage[s3], x_mid[t * 128:(t + 1) * 128, :], rd=(B_xmid[t],), wr=(xsB[s3],), chan=ch_xl[s3])
                S.op("dve", lambda e: e.tensor_copy(out=xb2[:, s2, :], in_=xstage[s3]), rd=(xsB[s3],), wr=(xb2B[s2],))
                for c in range(8):
                    bk = 6 + (c // 4) % 2
                    S.op("pe", lambda e, c=c, bk=bk: e.matmul(
                        PS[bk][:, (c % 4) * 128:(c % 4 + 1) * 128], lhsT=xb2[:, s2, c * 128:(c + 1) * 128],
                        rhs=ident_b, start=True, stop=True), rd=(xb2B[s2], B_const), wr=(PSB[bk],), sig=(c % 4 == 3))
                    if c % 4 == 3:
                        c0 = c - 3
                        for cc in range(4):
                            S.op("act", lambda e, c0=c0, cc=cc, bk=bk: e.activation(
                                out=h2T[:, c0 + cc, s * 128:(s + 1) * 128], in_=PS[bk][:, cc * 128:(cc + 1) * 128],
                                func=AF.Identity, bias=modT[:, l, 24 + c0 + cc:24 + c0 + cc + 1],
                                scale=sc2p[:, l, c0 + cc:c0 + cc + 1]),
                                rd=(PSB[bk], B_const), wr=(h2B[par][s // 4],))

            def router(T):
                par = T % 2
                h2T = h2Tall[:, par]
                gates = gates2[par]
                RB = 7
                for s in range(8):
                    for kc in range(8):
                        S.op("pe", lambda e, s=s, kc=kc: e.matmul(
                            PS[RB][:, s * 16:(s + 1) * 16], lhsT=h2T[:, kc, s * 128:(s + 1) * 128], rhs=wr_b[:, kc, :],
                            start=(kc == 0), stop=(kc == 7)), rd=(h2B[par][s // 4], B_const), wr=(PSB[RB],), sig=(kc == 7 and s == 7))
                v3 = lambda a: a.rearrange("p (s n) -> p s n", s=8)
                sc = v3(rt[:, 0:128])
                sel = v3(rt[:, 128:256])
                sel2 = v3(rt[:, 256:384])
                eq = v3(rt[:, 384:512])
                eq2 = v3(rt[:, 512:640])
                m1 = rt[:, 640:672]
                m2 = rt[:, 672:704]
                gs = rt[:, 704:736]
                gm = rt[:, 736:744]
                pen = rt[:, 744:776]
                t1_ = rt[:, 776:784]
                t2_ = rt[:, 784:792]
                ws_ = rt[:, 792:800]
                g4 = lambda a: a.rearrange("p s (g k) -> p (s g) k", g=4)
                R = lambda fn, **kw: S.op("dve", fn, rd=(rtB,) + kw.get("rd", ()), wr=(rtB,) + kw.get("wr", ()))
                S.op("act", lambda e: e.activation(out=sc, in_=v3(PS[RB][:, 0:128]), func=AF.Sigmoid), rd=(PSB[RB],), wr=(rtB,))
                R(lambda e: e.tensor_tensor(out=sel, in0=sc, in1=br_b.unsqueeze(1).to_broadcast([128, 8, 16]), op=ALU.add), rd=(B_const,))
                R(lambda e: e.tensor_reduce(out=m1, in_=g4(sel), axis=AX.X, op=ALU.max))
                R(lambda e: e.tensor_tensor(out=g4(eq), in0=g4(sel), in1=m1.unsqueeze(2).to_broadcast([128, 32, 4]), op=ALU.is_equal))
                R(lambda e: e.scalar_tensor_tensor(out=sel2, in0=eq, scalar=-BIG, in1=sel, op0=ALU.mult, op1=ALU.add))
                R(lambda e: e.tensor_reduce(out=m2, in_=g4(sel2), axis=AX.X, op=ALU.max))
                R(lambda e: e.tensor_tensor(out=gs, in0=m1, in1=m2, op=ALU.add))
                R(lambda e: e.tensor_reduce(out=gm, in_=gs.rearrange("p (s g) -> p s g", g=4), axis=AX.X, op=ALU.max))
                R(lambda e: e.tensor_tensor(out=pen.rearrange("p (s g) -> p s g", g=4), in0=gs.rearrange("p (s g) -> p s g", g=4),
                                            in1=gm.unsqueeze(2).to_broadcast([128, 8, 4]), op=ALU.is_equal))
                R(lambda e: e.tensor_scalar(out=pen, in0=pen, scalar1=-1.0, scalar2=BIG, op0=ALU.add, op1=ALU.mult))
                R(lambda e: e.tensor_tensor(out=g4(sel2), in0=g4(sel), in1=pen.unsqueeze(2).to_broadcast([128, 32, 4]), op=ALU.add))
                R(lambda e: e.tensor_reduce(out=t1_, in_=sel2, axis=AX.X, op=ALU.max))
                R(lambda e: e.tensor_tensor(out=eq, in0=sel2, in1=t1_.unsqueeze(2).to_broadcast([128, 8, 16]), op=ALU.is_equal))
                R(lambda e: e.scalar_tensor_tensor(out=sel, in0=eq, scalar=-BIG, in1=sel2, op0=ALU.mult, op1=ALU.add))
                R(lambda e: e.tensor_reduce(out=t2_, in_=sel, axis=AX.X, op=ALU.max))
                R(lambda e: e.tensor_tensor(out=eq2, in0=sel, in1=t2_.unsqueeze(2).to_broadcast([128, 8, 16]), op=ALU.is_equal))
                R(lambda e: e.tensor_tensor(out=eq, in0=eq, in1=eq2, op=ALU.add))
                R(lambda e: e.tensor_tensor(out=eq, in0=eq, in1=sc, op=ALU.mult))
                R(lambda e: e.tensor_reduce(out=ws_, in_=eq, axis=AX.X, op=ALU.add))
                R(lambda e: e.reciprocal(out=ws_, in_=ws_))
                S.op("dve", lambda e: e.tensor_tensor(out=gates, in0=eq, in1=ws_.unsqueeze(2).to_broadcast([128, 8, 16]), op=ALU.mult),
                     rd=(rtB,), wr=(gatesB[par],))

            def b4_load(T, s):
                t = T * 8 + s
                s3 = xctr[0] % 3
                xctr[0] += 1
                S.dma("sp", xstage[s3], x_mid[t * 128:(t + 1) * 128, :], rd=(B_xmid[t],), wr=(xsB[s3],), chan=ch_xl[s3])
                return s3

            def b4_sub(T, s, s3=None, final=False):
                par = T % 2
                acc = accall[:, par]
                t = T * 8 + s
                if s3 is None:
                    s3 = b4_load(T, s)
                S.op("dve", lambda e: e.tensor_tensor(out=acc[:, s, :], in0=acc[:, s, :], in1=g_b, op=ALU.mult),
                     rd=(accB[par][s], bcB), wr=(accB[par][s],))
                S.op("dve", lambda e: e.scalar_tensor_tensor(
                    out=xstage[s3], in0=xstage[s3], scalar=ALPHA, in1=acc[:, s, :], op0=ALU.mult, op1=ALU.add),
                    rd=(xsB[s3], accB[par][s]), wr=(xsB[s3],))
                ln_tail(xstage[s3], st_[s3], mv_[s3], rs_[s3], lg_b, lb_b, xsB[s3], x_dst[t * 128:(t + 1) * 128, :], ch_xs[s3], eps_t,
                        B_xdst[t], bcB, act_norm=final, use_pool=final)

            def gu(T, e_, w_, slot):
                par = T % 2
                h2T = h2Tall[:, par]
                wg = ew[:, slot, 0, :].rearrange("p (c n) -> p c n", c=8)
                wu = ew[:, slot, 1, :].rearrange("p (c n) -> p c n", c=8)
                tsl = slice(w_ * 512, (w_ + 1) * 512)
                for fc in range(4):
                    pb = (fc % 2) * 2
                    for kc in range(8):
                        S.op("pe", lambda e, kc=kc, fc=fc, pb=pb: e.matmul(
                            PS[pb], lhsT=wg[:, kc, fc * 128:(fc + 1) * 128], rhs=h2T[:, kc, tsl],
                            start=(kc == 0), stop=(kc == 7)), rd=(ewguB[slot], h2B[par][w_]), wr=(PSB[pb],), sig=(kc == 7))
                    for kc in range(8):
                        S.op("pe", lambda e, kc=kc, fc=fc, pb=pb: e.matmul(
                            PS[pb + 1], lhsT=wu[:, kc, fc * 128:(fc + 1) * 128], rhs=h2T[:, kc, tsl],
                            start=(kc == 0), stop=(kc == 7)), rd=(ewguB[slot], h2B[par][w_]), wr=(PSB[pb + 1],), sig=(kc == 7))
                    si = fc % 2
                    S.op("act", lambda e, si=si, pb=pb: e.activation(out=sgs[si], in_=PS[pb], func=AF.Silu),
                         rd=(PSB[pb],), wr=(sgB[si],))
                    S.op("dve", lambda e, si=si, pb=pb, fc=fc: e.tensor_tensor(
                        out=actb[:, w_, fc, :], in0=sgs[si], in1=PS[pb + 1], op=ALU.mult),
                        rd=(sgB[si], PSB[pb + 1]), wr=(actB[w_],))

            def down(T, e_, w_, slot):
                par = T % 2
                acc = accall[:, par]
                gates = gates2[par]
                wd = ew[:, slot, 2, :].rearrange("p (c n) -> p c n", c=4)
                for ss in range(4):
                    s = w_ * 4 + ss
                    for hh in range(2):
                        yb = 4 + ((ss * 2 + hh) % 2)
                        for fc in range(4):
                            S.op("pe", lambda e, fc=fc, ss=ss, hh=hh, yb=yb: e.matmul(
                                PS[yb], lhsT=actb[:, w_, fc, ss * 128:(ss + 1) * 128], rhs=wd[:, fc, hh * 512:(hh + 1) * 512],
                                start=(fc == 0), stop=(fc == 3)), rd=(actB[w_], ewdB[slot]), wr=(PSB[yb],), sig=(fc == 3))
                        if e_ == 0:
                            S.op("dve", lambda e, s=s, hh=hh, yb=yb: e.tensor_scalar(
                                out=acc[:, s, hh * 512:(hh + 1) * 512], in0=PS[yb], scalar1=gates[:, s, e_:e_ + 1],
                                scalar2=None, op0=ALU.mult), rd=(PSB[yb], gatesB[par]), wr=(accB[par][s],))
                        else:
                            S.op("dve", lambda e, s=s, hh=hh, yb=yb: e.scalar_tensor_tensor(
                                out=acc[:, s, hh * 512:(hh + 1) * 512], in0=PS[yb], scalar=gates[:, s, e_:e_ + 1],
                                in1=acc[:, s, hh * 512:(hh + 1) * 512], op0=ALU.mult, op1=ALU.add),
                                rd=(PSB[yb], gatesB[par], accB[par][s]), wr=(accB[par][s],))

            load_gu(0, 0)
            load_d(0, 0)
            for s in range(8):
                b1_sub(0, s)
            router(0)
            eidx = 0
            pendD = None
            NTT = 4
            for T in range(NTT):
                extras = []
                if T > 0:
                    extras += [(lambda T=T, s=s: b4_sub(T - 1, s)) for s in range(8)]
                extras_late = []
                if T + 1 < NTT:
                    extras_late += [(lambda T=T, s=s: b1_sub(T + 1, s)) for s in range(8)]
                    extras_late.append(lambda T=T: router(T + 1))
                step = 0
                for e_ in range(NE):
                    slot = eidx % 2
                    eidx += 1
                    nxt = (T, e_ + 1) if e_ + 1 < NE else ((T + 1, 0) if T + 1 < NTT else None)
                    if nxt is not None:
                        load_gu(nxt[1], 1 - slot)
                    for w_ in range(2):
                        gu(T, e_, w_, slot)
                        if pendD is not None:
                            pendD()
                            pendD = None
                        if w_ == 0 and nxt is not None:
                            load_d(nxt[1], 1 - slot)
                        pendD = (lambda T=T, e_=e_, w_=w_, slot=slot: down(T, e_, w_, slot))
                        if extras:
                            extras.pop(0)()
                        elif step >= 14 and extras_late:
                            extras_late.pop(0)()
                        step += 1
                pendD()
                pendD = None
                while extras:
                    extras.pop(0)()
                while extras_late:
                    extras_late.pop(0)()
            slots = [b4_load(NTT - 1, 0), b4_load(NTT - 1, 1)]
            for s in range(8):
                if s + 2 < 8:
                    slots.append(b4_load(NTT - 1, s + 2))
                b4_sub(NTT - 1, s, slots[s], final=True)
            S.fence(Buf.REG[reg0:], fscr)
    for ch in ch_xs:
        if ch.last is not None:
            S.wait_event("sp", ch.last)
    return nc, S


_CACHE = {}


def kernel(**inputs):
    if "nc" not in _CACHE:
        _CACHE["nc"] = build()[0]
    nc = _CACHE["nc"]
    consts = _host_consts()
    shared = {}
    for k, v in inputs.items():
        if k in ("x", "c"):
            continue
        a = np.ascontiguousarray(np.asarray(v, dtype=np.float32))
        if k == "b_router":
            a = a.reshape(1, NE)
        shared[k] = a
    shared.update(consts)
    x = np.asarray(inputs["x"], dtype=np.float32)
    c = np.asarray(inputs["c"], dtype=np.float32)
    in_maps = []
    for b in range(8):
        m = dict(shared)
        m["x"] = np.ascontiguousarray(x[b])
        m["c"] = np.ascontiguousarray(c[b:b + 1])
        in_maps.append(m)
    res = run_bass_kernel_spmd(nc, in_maps, core_ids=list(range(8)))
    return np.stack([np.asarray(r["out"]) for r in res.results], axis=0).astype(np.float32)
```
